# Optimizing a Trainium2 kernel written in Bass

```python
import jax, jax.numpy as jnp
from jax import lax
import numpy as np

D_MODEL = 2048
BATCH = 2
SEQ = 16384
DEPTH = 1

CTX_LEN = 256
GRID_W = 64
D_MIX = D_MODEL
GLA_HEADS = 4
GLA_WIDTH = D_MIX // 2
GLA_DV = GLA_WIDTH // GLA_HEADS
GLA_DK = GLA_DV // 2
GLA_QK = GLA_HEADS * GLA_DK
GATE_RANK = 16
GATE_NORMALIZER = 16.0
CHUNK = 64
FOURIER_WIDTH = D_MIX - GLA_WIDTH
FOURIER_GROUPS = 8
FOURIER_GROUP_DIM = FOURIER_WIDTH // FOURIER_GROUPS
N_EXPERTS = 16
CAPACITY_FACTOR = 2
D_EXPERT = D_MODEL // 2
EPS = 1e-6
PROJ_WIDTH = 2 * GLA_QK + 2 * GLA_WIDTH + 2 * GATE_RANK + FOURIER_WIDTH
SPLIT_POINTS = (GLA_QK, 2 * GLA_QK, 2 * GLA_QK + GLA_WIDTH, 2 * GLA_QK + 2 * GLA_WIDTH,
                2 * GLA_QK + 2 * GLA_WIDTH + GATE_RANK, 2 * GLA_QK + 2 * GLA_WIDTH + 2 * GATE_RANK)

kernel_name = 'hybrid_gla_fnet_ec_moe_diffusion_block'


def rms_norm(x, g):
    xf = x.astype(jnp.float32)
    y = xf * lax.rsqrt(jnp.mean(xf * xf, axis=-1, keepdims=True) + EPS)
    return (y * g.astype(jnp.float32)).astype(x.dtype)


def modulate(h, shift, scale):
    return h * (1 + scale) + shift


def ada_params(cond, w_ada, b_ada):
    return jnp.split(jax.nn.silu(cond) @ w_ada + b_ada, 6, axis=-1)


def split_projection(p, w_a2_f, b_a2_f, w_a2_b, b_a2_b):
    q, k, v, g, z_f, z_b, u = jnp.split(p, SPLIT_POINTS, axis=-1)
    log_a_f = jax.nn.log_sigmoid((z_f @ w_a2_f + b_a2_f).astype(jnp.float32)) / GATE_NORMALIZER
    log_a_b = jax.nn.log_sigmoid((z_b @ w_a2_b + b_a2_b).astype(jnp.float32)) / GATE_NORMALIZER
    heads = lambda t: t.reshape(t.shape[0], t.shape[1], GLA_HEADS, -1).transpose(0, 2, 1, 3)
    return heads(q), heads(k), heads(v), g, heads(log_a_f), heads(log_a_b), u


def gla_chunked(q, k, v, log_a, s0):
    b, h, l, dk = q.shape
    dv = v.shape[-1]
    n = l // CHUNK
    q = (q.astype(jnp.float32) * dk ** -0.5).reshape(b, h, n, CHUNK, dk)
    k = k.astype(jnp.float32).reshape(b, h, n, CHUNK, dk)
    v = v.astype(jnp.float32).reshape(b, h, n, CHUNK, dv)
    cum = jnp.cumsum(log_a.astype(jnp.float32).reshape(b, h, n, CHUNK, dk), axis=3)
    cum_last = cum[:, :, :, -1:, :]
    q_dec = q * jnp.exp(cum)
    k_inv = k * jnp.exp(-cum)
    k_to_end = k * jnp.exp(cum_last - cum)
    in_chunk = jnp.tril(jnp.ones((CHUNK, CHUNK), dtype=bool))
    scores = jnp.where(in_chunk, jnp.einsum('bhnid,bhnjd->bhnij', q_dec, k_inv), 0.0)
    o_intra = jnp.einsum('bhnij,bhnjv->bhniv', scores, v)
    chunk_decay = jnp.exp(cum_last[:, :, :, 0, :])
    xs = tuple(jnp.moveaxis(t, 2, 0) for t in (q_dec, k_to_end, v, chunk_decay))

    def step(state, inp):
        q_n, k_n, v_n, d_n = inp
        o_n = jnp.einsum('bhid,bhdv->bhiv', q_n, state)
        state = d_n[..., None] * state + jnp.einsum('bhjd,bhjv->bhdv', k_n, v_n)
        return state, o_n

    s_final, o_inter = lax.scan(step, s0.astype(jnp.float32), xs)
    o = o_intra + jnp.moveaxis(o_inter, 0, 2)
    return o.reshape(b, h, l, dv), s_final


def gla_bidirectional(q, k, v, log_a_f, log_a_b, s0_f, s0_b):
    rev = lambda t: jnp.flip(t, axis=2)
    o_f, s_f = gla_chunked(q, k, v, log_a_f, s0_f)
    o_b, s_b = gla_chunked(rev(q), rev(k), rev(v), rev(log_a_b), s0_b)
    return o_f + rev(o_b), s_f, s_b


def fourier_mix(u):
    b, l, _ = u.shape
    uf = u.astype(jnp.float32).reshape(b, l, FOURIER_GROUPS, FOURIER_GROUP_DIM)
    y = jnp.fft.fft2(uf, axes=(1, 3), norm='ortho').real
    return y.reshape(b, l, FOURIER_WIDTH).astype(u.dtype)


def merge_heads(o, g, u, gla_norm_g, w_out):
    b, h, l, dv = o.shape
    o = o.transpose(0, 2, 1, 3)
    o = o * lax.rsqrt(jnp.mean(o * o, axis=-1, keepdims=True) + EPS)
    o = o * gla_norm_g.reshape(GLA_HEADS, GLA_DV).astype(jnp.float32)
    y_gla = (o.reshape(b, l, GLA_WIDTH) * jax.nn.silu(g.astype(jnp.float32))).astype(g.dtype)
    return jnp.concatenate([y_gla, fourier_mix(u)], axis=-1) @ w_out


def ec_moe(h, w_router, w_gate, w_up, w_down):
    b, l, d = h.shape
    cap = CAPACITY_FACTOR * l // N_EXPERTS
    aff = jax.nn.softmax(jnp.einsum('bld,de->ble', h, w_router).astype(jnp.float32), axis=-1)
    gate, idx = lax.top_k(jnp.swapaxes(aff, 1, 2), cap)
    xe = jax.vmap(lambda hb, ib: hb[ib])(h, idx)
    hid = jax.nn.silu(jnp.einsum('becd,edf->becf', xe, w_gate)) * jnp.einsum('becd,edf->becf', xe, w_up)
    ye = jnp.einsum('becf,efd->becd', hid, w_down) * gate[..., None].astype(h.dtype)
    combine = lambda yb, ib: jnp.zeros((l, d), yb.dtype).at[ib.reshape(-1)].add(yb.reshape(-1, d))
    return jax.vmap(combine)(ye, idx)


def setup_inputs(seed: int = 0) -> dict:
    key = jax.random.key(seed)
    ks = jax.random.split(key, 24)
    nrm = lambda k, shape, s: jax.random.normal(k, shape, jnp.float32) * s
    D, L = DEPTH, D_MODEL
    return {
        'x': nrm(ks[0], (BATCH, SEQ, D_MODEL), 1.0),
        'c': nrm(ks[1], (BATCH, D_MODEL), 1.0),
        'ctx': nrm(ks[2], (BATCH, CTX_LEN, D_MODEL), 1.0),
        'c_ctx': nrm(ks[3], (D_MODEL,), 1.0),
        'w_ada': nrm(ks[4], (D, D_MODEL, 6 * D_MODEL), D_MODEL ** -0.5),
        'b_ada': nrm(ks[5], (D, 6 * D_MODEL), 0.02),
        'norm1_g': 1.0 + nrm(ks[6], (D, D_MODEL), 0.02),
        'w_in': nrm(ks[7], (D, D_MODEL, PROJ_WIDTH), D_MODEL ** -0.5),
        'w_a2_f': nrm(ks[8], (D, GATE_RANK, GLA_QK), GATE_RANK ** -0.5),
        'b_a2_f': nrm(ks[9], (D, GLA_QK), 0.1),
        'w_a2_b': nrm(ks[10], (D, GATE_RANK, GLA_QK), GATE_RANK ** -0.5),
        'b_a2_b': nrm(ks[11], (D, GLA_QK), 0.1),
        'gla_norm_g': 1.0 + nrm(ks[12], (D, GLA_WIDTH), 0.02),
        'w_out': nrm(ks[13], (D, D_MIX, D_MODEL), D_MIX ** -0.5),
        'norm2_g': 1.0 + nrm(ks[14], (D, D_MODEL), 0.02),
        'w_router': nrm(ks[15], (D, D_MODEL, N_EXPERTS), D_MODEL ** -0.5),
        'w_e_gate': nrm(ks[16], (D, N_EXPERTS, D_MODEL, D_EXPERT), D_MODEL ** -0.5),
        'w_e_up': nrm(ks[17], (D, N_EXPERTS, D_MODEL, D_EXPERT), D_MODEL ** -0.5),
        'w_e_down': nrm(ks[18], (D, N_EXPERTS, D_EXPERT, D_MODEL), D_EXPERT ** -0.5),
        'final_norm_g': 1.0 + nrm(ks[19], (D_MODEL,), 0.02),
    }


def reference(x, c, ctx, c_ctx, w_ada, b_ada, norm1_g, w_in, w_a2_f, b_a2_f, w_a2_b, b_a2_b,
              gla_norm_g, w_out, norm2_g, w_router, w_e_gate, w_e_up, w_e_down, final_norm_g):
    b, l, _ = x.shape
    rows = l // GRID_W
    assert rows * GRID_W == l
    zero_state = jnp.zeros((b, GLA_HEADS, GLA_DK, GLA_DV), jnp.float32)
    x_lat, x_ctx = x, ctx
    for layer in range(DEPTH):
        is_last = layer == DEPTH - 1
        sh1_l, sc1_l, g1_l, sh2_l, sc2_l, g2_l = [m[:, None, :] for m in ada_params(c, w_ada[layer], b_ada[layer])]
        sh1_c, sc1_c, g1_c, sh2_c, sc2_c, g2_c = ada_params(c_ctx, w_ada[layer], b_ada[layer])
        gate_w = (w_a2_f[layer], b_a2_f[layer], w_a2_b[layer], b_a2_b[layer])

        h_c = modulate(rms_norm(x_ctx, norm1_g[layer]), sh1_c, sc1_c)
        q_c, k_c, v_c, gt_c, laf_c, lab_c, u_c = split_projection(h_c @ w_in[layer], *gate_w)
        o_c, s_f, s_b = gla_bidirectional(q_c, k_c, v_c, laf_c, lab_c, zero_state, zero_state)

        h_l = modulate(rms_norm(x_lat, norm1_g[layer]), sh1_l, sc1_l)
        q_l, k_l, v_l, gt_l, laf_l, lab_l, u_l = split_projection(h_l @ w_in[layer], *gate_w)
        o_l, _, _ = gla_bidirectional(q_l, k_l, v_l, laf_l, lab_l, s_f, s_b)
        x_lat = x_lat + g1_l * merge_heads(o_l, gt_l, u_l, gla_norm_g[layer], w_out[layer])

        if not is_last:
            x_ctx = x_ctx + g1_c * merge_heads(o_c, gt_c, u_c, gla_norm_g[layer], w_out[layer])
            hf_c = modulate(rms_norm(x_ctx, norm2_g[layer]), sh2_c, sc2_c)
            x_ctx = x_ctx + g2_c * ec_moe(hf_c, w_router[layer], w_e_gate[layer], w_e_up[layer], w_e_down[layer])

        hf_l = modulate(rms_norm(x_lat, norm2_g[layer]), sh2_l, sc2_l)
        x_lat = x_lat + g2_l * ec_moe(hf_l, w_router[layer], w_e_gate[layer], w_e_up[layer], w_e_down[layer])
    return rms_norm(x_lat, final_norm_g)
```

```python
import contextlib
import numpy as np
import concourse.bass as bass
import concourse.mybir as mybir
from concourse.bass_utils import run_bass_kernel_spmd

F32 = mybir.dt.float32
BF16 = mybir.dt.bfloat16
I32 = mybir.dt.int32
ALU = mybir.AluOpType
AF = mybir.ActivationFunctionType
AX = mybir.AxisListType

D = 2048
KC = 16
TB = 512
EPS = 1e-6
ENGS = ("sync", "scalar", "vector", "gpsimd", "tensor")


class Buf:
    __slots__ = ("name", "last_w", "readers", "dma_sem", "dma_cnt", "dma_writers")

    def __init__(self, name):
        self.name = name
        self.last_w = None
        self.readers = {}
        self.dma_sem = None
        self.dma_cnt = 0
        self.dma_writers = False


class Prog:
    def __init__(self, nc):
        self.nc = nc
        self.ins = []
        self.last_on = {e: None for e in ENGS}
        self.dma_bufs = []
        self.pending = {e: None for e in ENGS}

    def _emit(self, eng, fn, reads, writes, dma=False):
        iid = len(self.ins)
        deps = set()
        for b in reads:
            if b.dma_writers:
                deps.add(("dma", b, b.dma_cnt))
            elif b.last_w is not None:
                deps.add(("ins", b.last_w))
        for b in writes:
            if b.dma_writers:
                deps.add(("dma", b, b.dma_cnt))
            elif b.last_w is not None:
                deps.add(("ins", b.last_w))
            for r in b.readers.values():
                deps.add(("ins", r))
        if self.pending[eng] is not None:
            deps |= self.pending[eng]
            self.pending[eng] = None
        rec = dict(eng=eng, fn=fn, deps=deps, dma=dma, dst=None, signal=False, cnt=None)
        if dma:
            d = writes[0]
            rec["dst"] = d
            d.dma_cnt += 1
            rec["cnt"] = d.dma_cnt
            d.dma_writers = True
            d.last_w = None
            d.readers = {}
            if d not in self.dma_bufs:
                self.dma_bufs.append(d)
        else:
            for b in writes:
                b.last_w = iid
                b.dma_writers = False
                b.readers = {}
            self.last_on[eng] = iid
        rkey = ("dma", id(writes[0])) if dma else eng
        for b in reads:
            if b not in writes:
                b.readers[rkey] = iid
        self.ins.append(rec)
        return iid

    def op(self, eng, fn, reads=(), writes=()):
        return self._emit(eng, fn, list(reads), list(writes))

    def dma(self, eng, fn, reads, write):
        return self._emit(eng, fn, list(reads), [write], dma=True)

    def barrier(self):
        deps = set()
        for e in ENGS:
            if self.last_on[e] is not None:
                deps.add(("ins", self.last_on[e]))
        for b in self.dma_bufs:
            deps.add(("dma", b, b.dma_cnt))
        for e in ENGS:
            self.pending[e] = set(deps) | (self.pending[e] or set())

    def build(self, final_waits=()):
        nc = self.nc
        ins = self.ins
        for rec in ins:
            for d in rec["deps"]:
                if d[0] == "ins":
                    p = ins[d[1]]
                    if p["dma"]:
                        continue
                    if p["eng"] == "tensor" and rec["eng"] == "tensor":
                        continue
                    p["signal"] = True
        cnt = {e: 0 for e in ENGS}
        for rec in ins:
            if not rec["dma"] and rec["signal"]:
                cnt[rec["eng"]] += 1
                rec["cnt"] = cnt[rec["eng"]]
        self.cnt = cnt
        with contextlib.ExitStack() as st:
            esem = {e: st.enter_context(nc.semaphore("s_" + e)) for e in ENGS}
            for i, b in enumerate(self.dma_bufs):
                b.dma_sem = st.enter_context(nc.semaphore("d%d" % i))
            block = st.enter_context(nc.Block())
            per = {e: [r for r in ins if r["eng"] == e] for e in ENGS}

            def run(engname, eng):
                known = {}
                for rec in per[engname]:
                    waits = {}
                    for d in rec["deps"]:
                        if d[0] == "dma":
                            s, v = d[1].dma_sem, 16 * d[2]
                        else:
                            p = ins[d[1]]
                            if p["dma"]:
                                s, v = p["dst"].dma_sem, 16 * p["cnt"]
                            else:
                                if p["eng"] == "tensor" and engname == "tensor":
                                    continue
                                s, v = esem[p["eng"]], p["cnt"]
                        if known.get(id(s), 0) >= v:
                            continue
                        if waits.get(id(s), (None, 0))[1] < v:
                            waits[id(s)] = (s, v)
                    for s, v in waits.values():
                        eng.wait_ge(s, v)
                        known[id(s)] = v
                    r = rec["fn"](eng)
                    if rec["dma"]:
                        r.then_inc(rec["dst"].dma_sem, 16)
                    elif rec["signal"]:
                        r.then_inc(esem[engname], 1)
                if engname == "sync":
                    for b in final_waits:
                        eng.wait_ge(b.dma_sem, 16 * b.dma_cnt)

            @block.sync
            def _(e):
                run("sync", e)

            @block.scalar
            def _(e):
                run("scalar", e)

            @block.vector
            def _(e):
                run("vector", e)

            @block.gpsimd
            def _(e):
                run("gpsimd", e)

            @block.tensor
            def _(e):
                run("tensor", e)


class T:
    def __init__(self, t, name):
        self.t = t
        self.b = Buf(name)

    def __getitem__(self, k):
        return self.t[k]


class Cfg:
    def __init__(self, L=16384, CTX=256, E=16, DE=1024, dev=False):
        self.L, self.CTX, self.E, self.DE, self.dev = L, CTX, E, DE, dev
        self.stop = None
        self.noscope = False
        self.T1 = L // 128
        self.NB = L // TB
        self.OWN = L // 4
        self.NOB = self.OWN // TB
        self.TG = TB
        self.NG = self.OWN // self.TG
        self.FC = DE // 128
        self.CAP = 2 * L // E
        self.PW = 4128


def const_layout(cfg):
    lay = {}
    off = 0
    for name, w in (("ident", 128), ("ones", 128), ("maskf", 128), ("maskb", 128), ("c128", 128),
                    ("s128", 128), ("reset", TB), ("twc", cfg.T1), ("tws", cfg.T1),
                    ("f1a", 2 * cfg.T1), ("f1b", 2 * cfg.T1), ("sel", cfg.E * 128)):
        lay[name] = (off, w)
        off += w
    return lay, off


def make_consts(cfg):
    lay, ncol = const_layout(cfg)
    c = np.zeros((128, ncol), np.float64)

    def put(name, arr):
        o, w = lay[name]
        c[:arr.shape[0], o:o + w] = arr

    a = np.arange(128)
    put("ident", np.eye(128))
    put("ones", np.ones((128, 128)))
    put("maskf", (a[:, None] <= a[None, :]).astype(np.float64))
    put("maskb", (a[:, None] >= a[None, :]).astype(np.float64))
    ang = 2 * np.pi * np.outer(a, a) / 128.0
    put("c128", np.cos(ang))
    put("s128", np.sin(ang))
    r = np.ones((128, TB))
    r[:, ::128] = 0.0
    put("reset", r)
    T1, L = cfg.T1, cfg.L
    k1 = np.arange(T1)
    nrm = 1.0 / np.sqrt(L * 128.0)
    angt = 2 * np.pi * np.outer(a, k1) / L
    put("twc", np.cos(angt) * nrm)
    put("tws", np.sin(angt) * nrm)
    ang1 = 2 * np.pi * np.outer(k1, k1) / T1
    put("f1a", np.concatenate([np.cos(ang1), -np.sin(ang1)], axis=1))
    put("f1b", np.concatenate([-np.sin(ang1), -np.cos(ang1)], axis=1))
    sel = np.zeros((cfg.E, cfg.E * 128))
    for e in range(cfg.E):
        sel[e, e * 128:(e + 1) * 128] = 1.0
    put("sel", sel)
    return c.astype(np.float32)


def build(cfg):
    L, CTX, E, DE, T1, NB, OWN, NOB = cfg.L, cfg.CTX, cfg.E, cfg.DE, cfg.T1, cfg.NB, cfg.OWN, cfg.NOB
    TG, NG, FC, PW = cfg.TG, cfg.NG, cfg.FC, cfg.PW
    lay, ncol = const_layout(cfg)
    nc = bass.Bass("TRN2", target_bir_lowering=False)
    P = Prog(nc)

    def din(name, shape, dt=F32):
        return nc.dram_tensor(name, list(shape), dt, kind="ExternalInput")

    dbg = "ExternalOutput" if cfg.dev else None

    def dscr(name, shape, dt, out=False):
        if out and dbg:
            return T(nc.dram_tensor(name, list(shape), dt, kind=dbg), name)
        return T(nc.dram_tensor(name, list(shape), dt), name)

    dumps = []

    def dump(name, tl, shape, dt=F32):
        if not cfg.dev:
            return
        dd = T(nc.dram_tensor("dbg_" + name, list(shape), dt, kind="ExternalOutput"), "dbg_" + name)
        P.dma("gpsimd", lambda e: e.dma_start(out=dd.t.ap(), in_=tl[:]), [tl.b], dd.b)
        dumps.append(dd.b)

    xT = din("xT", [D, L])
    ctxT = din("ctxT", [D, CTX])
    cond = din("cond", [128, 32])
    w_ada = din("w_ada", [D, 6 * D])
    b_ada = din("b_ada", [1, 6 * D])
    ncols = din("ncols", [128, 48])
    glag = din("glag", [128, 8])
    w_in = din("w_in", [D, PW])
    wa2 = din("wa2", [16, 1024])
    ba2 = din("ba2", [128, 8])
    w_out = din("w_out", [D, D])
    w_router = din("w_router", [D, E])
    w_eg = din("w_eg", [E, D, DE])
    w_eu = din("w_eu", [E, D, DE])
    w_ed = din("w_ed", [E, DE, D])
    cst_d = din("cst", [128, ncol])
    idx_d = din("idx", [128, NOB], I32)
    outT = T(nc.dram_tensor("outT", [D, OWN], F32, kind="ExternalOutput"), "outT")

    ADA = dscr("ADA", [2, 6 * D], F32, out=True)
    HT = dscr("HT", [NB * 128, KC * TB], BF16, out=True)
    OB = dscr("OB", [NB * 128, 2 * TB], F32)
    YT = dscr("YT", [NB * 128, KC * TB], BF16, out=True)
    FA = dscr("FA", [128, L], BF16)
    FB = dscr("FB", [128, L], BF16)
    X1 = dscr("X1", [NB * 128, KC * TB], F32, out=True)
    HF = dscr("HF", [NB * 128, KC * TB], BF16, out=True)
    AFS = dscr("AFS", [NB * 128, 4 * E], F32, out=True)

    xT_v = xT.ap().rearrange("(k p) t -> p k t", p=128)
    ctxT_v = ctxT.ap().rearrange("(k p) t -> p k t", p=128)
    w_in_v = w_in.ap().rearrange("(k p) n -> p k n", p=128)
    w_out_v = w_out.ap().rearrange("(k p) n -> p k n", p=128)
    w_ada_v = w_ada.ap().rearrange("(k p) n -> p k n", p=128)
    wr_v = w_router.ap().rearrange("(k p) n -> p k n", p=128)

    top = contextlib.ExitStack()

    def sb(st, name, shape, dt=F32):
        return T(st.enter_context(nc.sbuf_tensor("sb_" + name, list(shape), dt)), name)

    def psb(st, name, dt=F32, n=512):
        return T(st.enter_context(nc.psum_tensor("ps_" + name, [128, n], dt)), name)

    def cs(name):
        o, w = lay[name]
        return slice(o, o + w)

    cst = sb(top, "cst", [128, ncol])
    P.dma("sync", lambda e: e.dma_start(out=cst[:], in_=cst_d[:, :]), [], cst.b)
    cbf = sb(top, "cbf", [128, 128 * 4 + 4 * T1], BF16)
    CB = {"ones": slice(0, 128), "c128": slice(128, 256), "s128": slice(256, 384), "ident": slice(384, 512),
          "f1a": slice(512, 512 + 2 * T1), "f1b": slice(512 + 2 * T1, 512 + 4 * T1)}
    for nm in ("ones", "c128", "s128", "ident", "f1a", "f1b"):
        P.op("vector", lambda e, nm=nm: e.tensor_copy(out=cbf[:, CB[nm]], in_=cst[:, cs(nm)]), [cst.b], [cbf.b])
    mods = sb(top, "mods", [128, 96])
    modc = sb(top, "modc", [128, 32])
    ncl = sb(top, "ncl", [128, 48])
    glg = sb(top, "glg", [128, 8])
    nb2 = sb(top, "nb2", [128, 8])
    wa2f = sb(top, "wa2f", [16, 1024])
    wa2b = sb(top, "wa2b", [16, 1024], BF16)
    G1 = sb(top, "G1", [128, 16])
    G1c = sb(top, "G1c", [128, 16])
    G2 = sb(top, "G2", [128, 16])
    idx = sb(top, "idx", [128, NOB], I32)
    P.dma("sync", lambda e: e.dma_start(out=ncl[:], in_=ncols[:, :]), [], ncl.b)
    P.dma("sync", lambda e: e.dma_start(out=glg[:], in_=glag[:, :]), [], glg.b)
    P.dma("sync", lambda e: e.dma_start(out=nb2[:], in_=ba2[:, :]), [], nb2.b)
    P.dma("sync", lambda e: e.dma_start(out=wa2f[:], in_=wa2[:, :]), [], wa2f.b)
    P.dma("sync", lambda e: e.dma_start(out=idx[:], in_=idx_d[:, :]), [], idx.b)
    P.op("vector", lambda e: e.tensor_scalar(out=nb2[:], in0=nb2[:], scalar1=-1.0, scalar2=None, op0=ALU.mult), [nb2.b], [nb2.b])
    P.op("vector", lambda e: e.tensor_copy(out=wa2b[:], in_=wa2f[:]), [wa2f.b], [wa2b.b])

    def _phase1():
        with (contextlib.nullcontext(top) if cfg.noscope else contextlib.ExitStack()) as st:
            cnd = sb(st, "cnd", [128, 32])
            scd = sb(st, "scd", [128, 32])
            P.dma("sync", lambda e: e.dma_start(out=cnd[:], in_=cond[:, :]), [], cnd.b)
            P.op("scalar", lambda e: e.activation(out=scd[:], in_=cnd[:], func=AF.Silu), [cnd.b], [scd.b])
            wst = [sb(st, "wst%d" % i, [128, 4, 512]) for i in range(4)]
            pr = [psb(st, "pr%d" % i) for i in range(2)]
            rows = [sb(st, "rows%d" % i, [2, 512]) for i in range(2)]
            bad = [sb(st, "bad%d" % i, [2, 512]) for i in range(2)]
            sc_v = scd[:].rearrange("p (c k) -> p c k", k=16)
            wi = 0
            for n in range(24):
                pt = pr[n % 2]
                bt = bad[n % 2]
                P.dma("sync", lambda e, bt=bt, n=n: e.dma_start(out=bt[:], in_=b_ada[0:1, n * 512:(n + 1) * 512].partition_broadcast(2)), [], bt.b)
                for kq in range(4):
                    w = wst[wi % 4]
                    wi += 1
                    P.dma("sync", lambda e, w=w, n=n, kq=kq: e.dma_start(out=w[:], in_=w_ada_v[:, kq * 4:(kq + 1) * 4, n * 512:(n + 1) * 512]), [], w.b)
                    for kk in range(4):
                        k = kq * 4 + kk
                        P.op("tensor", lambda e, pt=pt, w=w, kk=kk, k=k: e.matmul(pt[0:2, :], lhsT=sc_v[:, :, k], rhs=w[:, kk, :], start=(k == 0), stop=(k == 15)),
                             [scd.b, w.b], [pt.b])
                rw = rows[n % 2]
                P.op("vector", lambda e, rw=rw, pt=pt, bt=bt: e.tensor_tensor(out=rw[:], in0=pt[0:2, :], in1=bt[:], op=ALU.add), [pt.b, bt.b], [rw.b])
                P.dma("gpsimd", lambda e, rw=rw, n=n: e.dma_start(out=ADA[0:2, n * 512:(n + 1) * 512], in_=rw[:]), [rw.b], ADA.b)
            P.dma("sync", lambda e: e.dma_start(out=mods[:], in_=ADA[0:1, :].rearrange("o (j p) -> p (o j)", p=128), allow_slow_non_contiguous=True), [ADA.b], mods.b)
            P.dma("sync", lambda e: e.dma_start(out=modc[:], in_=ADA[1:2, 0:4096].rearrange("o (j p) -> p (o j)", p=128), allow_slow_non_contiguous=True), [ADA.b], modc.b)
            P.op("vector", lambda e: e.scalar_tensor_tensor(out=G1[:], in0=mods[:, 16:32], scalar=1.0, in1=ncl[:, 0:16], op0=ALU.add, op1=ALU.mult), [mods.b, ncl.b], [G1.b])
            P.op("vector", lambda e: e.scalar_tensor_tensor(out=G1c[:], in0=modc[:, 16:32], scalar=1.0, in1=ncl[:, 0:16], op0=ALU.add, op1=ALU.mult), [modc.b, ncl.b], [G1c.b])
            P.op("vector", lambda e: e.scalar_tensor_tensor(out=G2[:], in0=mods[:, 64:80], scalar=1.0, in1=ncl[:, 16:32], op0=ALU.add, op1=ALU.mult), [mods.b, ncl.b], [G2.b])
    _phase1()
    dump('mods', mods, [128, 96])
    dump('modc', modc, [128, 32])
    dump('G1', G1, [128, 16])
    P.barrier()

    def rstd_from_sq(pbank, sq, nk, n, dst, inv_dim):
        for k in range(nk):
            P.op("tensor", lambda e, k=k: e.matmul(pbank[:, 0:n], lhsT=cbf[:, CB["ones"]], rhs=sq[:, k, 0:n], start=(k == 0), stop=(k == nk - 1)),
                 [cbf.b, sq.b], [pbank.b])
        P.op("scalar", lambda e: e.activation(out=dst[:, 0:n], in_=pbank[:, 0:n], func=AF.Ln, scale=inv_dim, bias=EPS), [pbank.b], [dst.b])
        P.op("scalar", lambda e: e.activation(out=dst[:, 0:n], in_=dst[:, 0:n], func=AF.Exp, scale=-0.5), [dst.b], [dst.b])

    hc = sb(top, "hc", [128, KC, CTX], BF16)
    def _phase2():
        with (contextlib.nullcontext(top) if cfg.noscope else contextlib.ExitStack()) as st:
            xs = [sb(st, "xs%d" % i, [128, KC, TB]) for i in range(2)] if not cfg.noscope else [sb(st, "xs0", [128, KC, TB])] * 2
            sq = sb(st, "sq", [128, KC, TB], BF16)
            rs = sb(st, "rs", [128, TB])
            tm = sb(st, "tm", [128, KC, TB])
            hts = [sb(st, "hts%d" % i, [128, KC, TB], BF16) for i in range(2)]
            pb = psb(st, "a0p")

            def front(src_ap, n, xsl, Gc, SHc, dst):
                P.dma("sync", lambda e: e.dma_start(out=xsl[:, :, 0:n], in_=src_ap), [], xsl.b)
                P.op("gpsimd", lambda e: e.tensor_tensor(out=sq[:, :, 0:n], in0=xsl[:, :, 0:n], in1=xsl[:, :, 0:n], op=ALU.mult), [xsl.b], [sq.b])
                rstd_from_sq(pb, sq, KC, n, rs, 1.0 / D)
                if n == CTX:
                    dump('xsl', xsl, [128, KC, TB])
                    dump('rs', rs, [128, TB])
                    dump('sq', sq, [128, KC, TB], BF16)
                P.op("vector", lambda e: e.tensor_tensor(out=tm[:, :, 0:n], in0=xsl[:, :, 0:n], in1=rs[:, 0:n].unsqueeze(1).to_broadcast([128, KC, n]), op=ALU.mult),
                     [xsl.b, rs.b], [tm.b])
                P.op("gpsimd", lambda e: e.tensor_tensor(out=tm[:, :, 0:n], in0=tm[:, :, 0:n], in1=Gc[:, 0:16].unsqueeze(2).to_broadcast([128, KC, n]), op=ALU.mult),
                     [tm.b, Gc.b], [tm.b])
                P.op("vector", lambda e: e.tensor_tensor(out=dst[:, :, 0:n], in0=tm[:, :, 0:n], in1=SHc.unsqueeze(2).to_broadcast([128, KC, n]), op=ALU.add),
                     [tm.b, mods.b, modc.b], [dst.b])

            front(ctxT_v[:, :, :], CTX, xs[0], G1c, modc[:, 0:16], hc)
            for blk in range(NB):
                h = hts[blk % 2]
                front(xT_v[:, :, blk * TB:(blk + 1) * TB], TB, xs[(blk + 1) % 2], G1, mods[:, 0:16], h)
                P.dma("gpsimd", lambda e, h=h, blk=blk: e.dma_start(out=HT[blk * 128:(blk + 1) * 128, :], in_=h[:].rearrange("p k t -> p (k t)")), [h.b], HT.b)
    _phase2()
    P.barrier()
    if cfg.stop == "A0":
        P.build(final_waits=[HT.b, ADA.b] + dumps)
        return nc, P

    def _phase3():
        with (contextlib.nullcontext(top) if cfg.noscope else contextlib.ExitStack()) as st:
            wstg = [sb(st, "wstg%d" % i, [128, KC, 256]) for i in range(2)]
            Wq = sb(st, "Wq", [128, KC, 128], BF16)
            Wk = sb(st, "Wk", [128, KC, 128], BF16)
            Wv = sb(st, "Wv", [128, KC, 256], BF16)
            Wg = sb(st, "Wg", [128, KC, 256], BF16)
            Wz = sb(st, "Wz", [128, KC, 32], BF16)
            hb = [sb(st, "hb%d" % i, [128, KC, TB], BF16) for i in range(2)]
            zt = sb(st, "zt", [16, TB], BF16)
            ex = sb(st, "ex", [128, TB])
            sp = sb(st, "sp", [128, TB])
            cum = sb(st, "cum", [128, TB])
            ea = sb(st, "ea", [128, TB])
            eb = sb(st, "eb", [128, TB])
            ec = sb(st, "ec", [128, TB])
            dec = sb(st, "dec", [128, 4])
            qd = sb(st, "qd", [128, TB], BF16)
            ki = sb(st, "ki", [128, TB], BF16)
            kte = sb(st, "kte", [128, TB])
            kteT = sb(st, "kteT", [128, 4, 128], BF16)
            vt = sb(st, "vt", [128, 4, 256], BF16)
            sg = sb(st, "sg", [128, 2, TB], BF16)
            sT = [sb(st, "sT%d" % i, [128, 128], BF16) for i in range(2)]
            state = sb(st, "state", [128, 256])
            sbf = [sb(st, "sbf%d" % i, [128, 256], BF16) for i in range(2)]
            Sf = sb(st, "Sf", [128, 256])
            Sb = sb(st, "Sb", [128, 256])
            osb = sb(st, "osb", [128, 2, TB])
            obl = sb(st, "obl", [128, 2, TB])
            osq = sb(st, "osq", [128, 2, TB], BF16)
            ors = sb(st, "ors", [128, TB])
            yt = sb(st, "yt", [128, 2, TB], BF16)
            B0, B1, B2, B3, B4, B5, B6, B7 = [psb(st, "gp%d" % i) for i in range(8)]
            b4v = Buf("b4v"); b4kv = Buf("b4kv"); b5s = Buf("b5s"); b5t = Buf("b5t")

            def load_w(dst, c0, n):
                w = wstg[load_w.i % 2]
                load_w.i += 1
                P.dma("sync", lambda e: e.dma_start(out=w[:, :, 0:n], in_=w_in_v[:, :, c0:c0 + n]), [], w.b)
                P.op("vector", lambda e: e.tensor_copy(out=dst[:, :, 0:n], in_=w[:, :, 0:n]), [w.b], [dst.b])
            load_w.i = 0

            def gla_block(h, hsrc, hbuf, n, direction, emit_out, blk):
                nch = n // 128
                dcol = 0 if direction == "f" else 1
                for k in range(KC):
                    P.op("tensor", lambda e, k=k: e.matmul(B0[:, 0:n], lhsT=Wq[:, k, :], rhs=hsrc[:, k, 0:n], start=(k == 0), stop=(k == KC - 1)), [Wq.b, hbuf], [B0.b])
                for k in range(KC):
                    P.op("tensor", lambda e, k=k: e.matmul(B1[:, 0:n], lhsT=Wk[:, k, :], rhs=hsrc[:, k, 0:n], start=(k == 0), stop=(k == KC - 1)), [Wk.b, hbuf], [B1.b])
                for k in range(KC):
                    P.op("tensor", lambda e, k=k: e.matmul(B2[0:16, 0:n], lhsT=Wz[:, k, dcol * 16:(dcol + 1) * 16], rhs=hsrc[:, k, 0:n], start=(k == 0), stop=(k == KC - 1)), [Wz.b, hbuf], [B2.b])
                P.op("vector", lambda e: e.tensor_copy(out=zt[:, 0:n], in_=B2[0:16, 0:n]), [B2.b], [zt.b])
                P.op("tensor", lambda e: e.matmul(B3[:, 0:n], lhsT=wa2b[:, dcol * 512 + h * 128:dcol * 512 + (h + 1) * 128], rhs=zt[:, 0:n], start=True, stop=True), [wa2b.b, zt.b], [B3.b])
                P.op("scalar", lambda e: e.activation(out=ex[:, 0:n], in_=B3[:, 0:n], func=AF.Exp, scale=-1.0, bias=nb2[:, dcol * 4 + h:dcol * 4 + h + 1]), [B3.b, nb2.b], [ex.b])
                P.op("scalar", lambda e: e.activation(out=sp[:, 0:n], in_=ex[:, 0:n], func=AF.Ln, bias=1.0), [ex.b], [sp.b])
                P.op("vector", lambda e: e.tensor_tensor_scan(out=cum[:, 0:n], data0=cst[:, lay["reset"][0]:lay["reset"][0] + n], data1=sp[:, 0:n], initial=0.0, op0=ALU.mult, op1=ALU.add),
                     [cst.b, sp.b], [cum.b])
                cum3 = cum[:, 0:n].rearrange("p (c j) -> p c j", j=128)
                tot = cum3[:, :, 127:128]
                P.op("scalar", lambda e: e.activation(out=dec[:, 0:nch], in_=cum3[:, :, 127], func=AF.Exp, scale=-1.0 / 16), [cum.b], [dec.b])
                if direction == "b":
                    P.op("vector", lambda e: e.tensor_tensor(out=ec[:, 0:n], in0=sp[:, 0:n], in1=cum[:, 0:n], op=ALU.subtract), [sp.b, cum.b], [ec.b])
                    ec3 = ec[:, 0:n].rearrange("p (c j) -> p c j", j=128)
                    P.op("vector", lambda e: e.tensor_tensor(out=ec3, in0=ec3, in1=tot.to_broadcast([128, nch, 128]), op=ALU.add), [ec.b, cum.b], [ec.b])
                    cdir = ec
                else:
                    cdir = cum
                P.op("scalar", lambda e: e.activation(out=ea[:, 0:n], in_=cdir[:, 0:n], func=AF.Exp, scale=-1.0 / 16), [cdir.b], [ea.b])
                P.op("scalar", lambda e: e.activation(out=eb[:, 0:n], in_=cdir[:, 0:n], func=AF.Exp, scale=1.0 / 16), [cdir.b], [eb.b])
                P.op("vector", lambda e: e.scalar_tensor_tensor(out=qd[:, 0:n], in0=B0[:, 0:n], scalar=128.0 ** -0.5, in1=ea[:, 0:n], op0=ALU.mult, op1=ALU.mult), [B0.b, ea.b], [qd.b])
                P.op("vector", lambda e: e.tensor_tensor(out=ki[:, 0:n], in0=B1[:, 0:n], in1=eb[:, 0:n], op=ALU.mult), [B1.b, eb.b], [ki.b])
                ea3 = ea[:, 0:n].rearrange("p (c j) -> p c j", j=128)
                cd3 = cdir[:, 0:n].rearrange("p (c j) -> p c j", j=128)
                P.op("vector", lambda e: e.tensor_tensor(out=ea3, in0=cd3, in1=tot.to_broadcast([128, nch, 128]), op=ALU.subtract), [cdir.b, cum.b, qd.b], [ea.b])
                P.op("scalar", lambda e: e.activation(out=ea[:, 0:n], in_=ea[:, 0:n], func=AF.Exp, scale=1.0 / 16), [ea.b], [ea.b])
                P.op("vector", lambda e: e.tensor_tensor(out=kte[:, 0:n], in0=B1[:, 0:n], in1=ea[:, 0:n], op=ALU.mult), [B1.b, ea.b], [kte.b])
                for c in range(nch):
                    for k in range(KC):
                        P.op("tensor", lambda e, k=k, c=c: e.matmul(B4[:, 0:256], lhsT=hsrc[:, k, c * 128:(c + 1) * 128], rhs=Wv[:, k, :], start=(k == 0), stop=(k == KC - 1)), [Wv.b, hbuf], [b4v])
                    P.op("scalar", lambda e, c=c: e.activation(out=vt[:, c, :], in_=B4[:, 0:256], func=AF.Copy), [b4v], [vt.b])
                    P.op("tensor", lambda e, c=c: e.transpose(B5[:, 128:256], kte[:, c * 128:(c + 1) * 128], cst[:, cs("ident")]), [kte.b, cst.b], [b5t])
                    P.op("vector", lambda e, c=c: e.tensor_copy(out=kteT[:, c, :], in_=B5[:, 128:256]), [b5t], [kteT.b])
                if emit_out and direction == "f":
                    for vb in range(2):
                        Bg = (B0, B1)[vb]
                        for k in range(KC):
                            P.op("tensor", lambda e, k=k, vb=vb, Bg=Bg: e.matmul(Bg[:, 0:n], lhsT=Wg[:, k, vb * 128:(vb + 1) * 128], rhs=hsrc[:, k, 0:n], start=(k == 0), stop=(k == KC - 1)), [Wg.b, hbuf], [Bg.b])
                        P.op("scalar", lambda e, vb=vb, Bg=Bg: e.activation(out=sg[:, vb, 0:n], in_=Bg[:, 0:n], func=AF.Silu), [Bg.b], [sg.b])
                order = range(nch) if direction == "f" else range(nch - 1, -1, -1)
                mask = cst[:, cs("maskf")] if direction == "f" else cst[:, cs("maskb")]
                for c in order:
                    csl = slice(c * 128, (c + 1) * 128)
                    if emit_out:
                        s_ = sT[gla_block.si % 2]
                        P.op("tensor", lambda e, csl=csl: e.matmul(B5[:, 0:128], lhsT=ki[:, csl], rhs=qd[:, csl], start=True, stop=True), [ki.b, qd.b], [b5s])
                        P.op("vector", lambda e, s_=s_: e.tensor_tensor(out=s_[:], in0=B5[:, 0:128], in1=mask, op=ALU.mult), [b5s, cst.b], [s_.b])
                        gla_block.si += 1
                        for vb in range(2):
                            Bo = (B6, B7)[vb]
                            P.op("tensor", lambda e, vb=vb, c=c, csl=csl, s_=s_, Bo=Bo: e.matmul(Bo[:, csl], lhsT=vt[:, c, vb * 128:(vb + 1) * 128], rhs=s_[:], start=True, stop=False), [vt.b, s_.b], [Bo.b])
                            P.op("tensor", lambda e, vb=vb, csl=csl, Bo=Bo, cur=gla_block.cur: e.matmul(Bo[:, csl], lhsT=cur[:, vb * 128:(vb + 1) * 128], rhs=qd[:, csl], start=False, stop=True), [gla_block.cur.b, qd.b], [Bo.b])
                    P.op("tensor", lambda e, c=c: e.matmul(B4[:, 256:512], lhsT=kteT[:, c, :], rhs=vt[:, c, :], start=True, stop=True), [kteT.b, vt.b], [b4kv])
                    P.op("vector", lambda e, c=c: e.scalar_tensor_tensor(out=state[:], in0=state[:], scalar=dec[:, c:c + 1], in1=B4[:, 256:512], op0=ALU.mult, op1=ALU.add), [state.b, dec.b, b4kv], [state.b])
                    nxt = sbf[(gla_block.ci + 1) % 2]
                    gla_block.ci += 1
                    P.op("scalar", lambda e, nxt=nxt: e.activation(out=nxt[:], in_=state[:], func=AF.Copy), [state.b], [nxt.b])
                    gla_block.cur = nxt
                if emit_out:
                    if direction == "b":
                        P.op("vector", lambda e: e.tensor_copy(out=osb[:, 0, :], in_=B6[:, :]), [B6.b], [osb.b])
                        P.op("vector", lambda e: e.tensor_copy(out=osb[:, 1, :], in_=B7[:, :]), [B7.b], [osb.b])
                        P.dma("gpsimd", lambda e: e.dma_start(out=OB[blk * 128:(blk + 1) * 128, :], in_=osb[:].rearrange("p a t -> p (a t)")), [osb.b], OB.b)
                    else:
                        P.dma("sync", lambda e: e.dma_start(out=obl[:].rearrange("p a t -> p (a t)"), in_=OB[blk * 128:(blk + 1) * 128, :]), [OB.b], obl.b)
                        P.op("vector", lambda e: e.tensor_tensor(out=osb[:, 0, :], in0=B6[:, :], in1=obl[:, 0, :], op=ALU.add), [B6.b, obl.b], [osb.b])
                        P.op("vector", lambda e: e.tensor_tensor(out=osb[:, 1, :], in0=B7[:, :], in1=obl[:, 1, :], op=ALU.add), [B7.b, obl.b], [osb.b])
                        P.op("gpsimd", lambda e: e.tensor_tensor(out=osq[:], in0=osb[:], in1=osb[:], op=ALU.mult), [osb.b], [osq.b])
                        rstd_from_sq(B2, osq, 2, TB, ors, 1.0 / 256)
                        P.op("vector", lambda e: e.tensor_tensor(out=osb[:], in0=osb[:], in1=ors[:].unsqueeze(1).to_broadcast([128, 2, TB]), op=ALU.mult), [osb.b, ors.b], [osb.b])
                        for vb in range(2):
                            P.op("vector", lambda e, vb=vb: e.scalar_tensor_tensor(out=yt[:, vb, :], in0=osb[:, vb, :], scalar=glg[:, 2 * h + vb:2 * h + vb + 1], in1=sg[:, vb, :], op0=ALU.mult, op1=ALU.mult),
                                 [osb.b, glg.b, sg.b], [yt.b])
                        P.dma("gpsimd", lambda e: e.dma_start(out=YT[blk * 128:(blk + 1) * 128, 2 * h * TB:(2 * h + 2) * TB], in_=yt[:].rearrange("p a t -> p (a t)")), [yt.b], YT.b)
            gla_block.si = 0
            gla_block.ci = 0
            gla_block.cur = sbf[0]

            def set_state(src):
                if src is None:
                    P.op("vector", lambda e: e.memset(state[:], 0.0), [], [state.b])
                else:
                    P.op("vector", lambda e: e.tensor_copy(out=state[:], in_=src[:]), [src.b], [state.b])
                nxt = sbf[(gla_block.ci + 1) % 2]
                gla_block.ci += 1
                P.op("scalar", lambda e: e.activation(out=nxt[:], in_=state[:], func=AF.Copy), [state.b], [nxt.b])
                gla_block.cur = nxt

            for h in range(4):
                load_w(Wq, h * 128, 128)
                load_w(Wk, 512 + h * 128, 128)
                load_w(Wv, 1024 + h * 256, 256)
                load_w(Wg, 2048 + h * 256, 256)
                load_w(Wz, 3072, 32)
                set_state(None)
                gla_block(h, hc, hc.b, CTX, "f", False, 0)
                P.op("vector", lambda e: e.tensor_copy(out=Sf[:], in_=state[:]), [state.b], [Sf.b])
                set_state(None)
                gla_block(h, hc, hc.b, CTX, "b", False, 0)
                P.op("vector", lambda e: e.tensor_copy(out=Sb[:], in_=state[:]), [state.b], [Sb.b])
                set_state(Sb)
                for i, blk in enumerate(range(NB - 1, -1, -1)):
                    hbt = hb[i % 2]
                    P.dma("sync", lambda e, hbt=hbt, blk=blk: e.dma_start(out=hbt[:].rearrange("p k t -> p (k t)"), in_=HT[blk * 128:(blk + 1) * 128, :]), [HT.b], hbt.b)
                    gla_block(h, hbt, hbt.b, TB, "b", True, blk)
                set_state(Sf)
                for i, blk in enumerate(range(NB)):
                    hbt = hb[i % 2]
                    P.dma("sync", lambda e, hbt=hbt, blk=blk: e.dma_start(out=hbt[:].rearrange("p k t -> p (k t)"), in_=HT[blk * 128:(blk + 1) * 128, :]), [HT.b], hbt.b)
                    gla_block(h, hbt, hbt.b, TB, "f", True, blk)
    _phase3()
    P.barrier()

    if cfg.stop == "A1":
        P.build(final_waits=[HT.b, ADA.b, YT.b] + dumps)
        return nc, P

    NK2 = TB // T1
    def _phase4():
        with (contextlib.nullcontext(top) if cfg.noscope else contextlib.ExitStack()) as st:
            wstg = sb(st, "fwst", [128, KC, 128])
            Wu = sb(st, "Wu", [128, KC, 128], BF16)
            hb = [sb(st, "fhb%d" % i, [128, KC, TB], BF16) for i in range(2)]
            ut = sb(st, "ut", [128, TB], BF16)
            at = [sb(st, "at%d" % i, [128, TB], BF16) for i in range(2)]
            bt_ = [sb(st, "btt%d" % i, [128, TB], BF16) for i in range(2)]
            DA = sb(st, "DA", [T1, 128, 128], BF16)
            DB = sb(st, "DB", [T1, 128, 128], BF16)
            Yall = sb(st, "Yall", [T1, 128, 128], BF16)
            xs1 = [sb(st, "xs1%d" % i, [128, 2, 2, T1]) for i in range(2)]
            tA = [sb(st, "tA%d" % i, [128, 2, T1]) for i in range(2)]
            tB = [sb(st, "tB%d" % i, [128, 2, T1]) for i in range(2)]
            br = [sb(st, "br%d" % i, [128, 2, T1], BF16) for i in range(2)]
            bi = [sb(st, "bi%d" % i, [128, 2, T1], BF16) for i in range(2)]
            yo = [sb(st, "yo%d" % i, [128, TB], BF16) for i in range(2)]
            pu = psb(st, "pu")
            pa = psb(st, "pa")
            pbk = psb(st, "pbk")
            p1 = [psb(st, "p1%d" % i) for i in range(2)]
            p2 = [psb(st, "p2%d" % i) for i in range(2)]
            ptr = T(st.enter_context(nc.psum_tensor("ps_ptr", [128, 1024], BF16)), "ptr")
            twc3 = cst[:, cs("twc")].unsqueeze(1).to_broadcast([128, 2, T1])
            tws3 = cst[:, cs("tws")].unsqueeze(1).to_broadcast([128, 2, T1])
            for gi in range(8):
                P.dma("sync", lambda e, gi=gi: e.dma_start(out=wstg[:], in_=w_in_v[:, :, 3104 + gi * 128:3104 + (gi + 1) * 128]), [], wstg.b)
                P.op("vector", lambda e: e.tensor_copy(out=Wu[:], in_=wstg[:]), [wstg.b], [Wu.b])
                for blk in range(NB):
                    hbt = hb[blk % 2]
                    P.dma("sync", lambda e, hbt=hbt, blk=blk: e.dma_start(out=hbt[:].rearrange("p k t -> p (k t)"), in_=HT[blk * 128:(blk + 1) * 128, :]), [HT.b], hbt.b)
                    for k in range(KC):
                        P.op("tensor", lambda e, k=k, hbt=hbt: e.matmul(pu[:, :], lhsT=Wu[:, k, :], rhs=hbt[:, k, :], start=(k == 0), stop=(k == KC - 1)), [Wu.b, hbt.b], [pu.b])
                    P.op("scalar", lambda e: e.activation(out=ut[:], in_=pu[:, :], func=AF.Copy), [pu.b], [ut.b])
                    a_, b_ = at[blk % 2], bt_[blk % 2]
                    P.op("tensor", lambda e: e.matmul(pa[:, :], lhsT=cbf[:, CB["c128"]], rhs=ut[:], start=True, stop=True), [cbf.b, ut.b], [pa.b])
                    P.op("tensor", lambda e: e.matmul(pbk[:, :], lhsT=cbf[:, CB["s128"]], rhs=ut[:], start=True, stop=True), [cbf.b, ut.b], [pbk.b])
                    P.op("vector", lambda e, a_=a_: e.tensor_copy(out=a_[:], in_=pa[:, :]), [pa.b], [a_.b])
                    P.op("scalar", lambda e, b_=b_: e.activation(out=b_[:], in_=pbk[:, :], func=AF.Copy), [pbk.b], [b_.b])
                    P.dma("gpsimd", lambda e, a_=a_, blk=blk: e.dma_start(out=FA[:, blk * TB:(blk + 1) * TB], in_=a_[:]), [a_.b], FA.b)
                    P.dma("gpsimd", lambda e, b_=b_, blk=blk: e.dma_start(out=FB[:, blk * TB:(blk + 1) * TB], in_=b_[:]), [b_.b], FB.b)
                for m0 in range(0, 128, 16):
                    P.dma("sync", lambda e, m0=m0: e.dma_start(out=DA[:, m0:m0 + 16, :], in_=FA[m0:m0 + 16, :].rearrange("m (a b) -> a m b", b=128)), [FA.b], DA.b)
                    P.dma("sync", lambda e, m0=m0: e.dma_start(out=DB[:, m0:m0 + 16, :], in_=FB[m0:m0 + 16, :].rearrange("m (a b) -> a m b", b=128)), [FB.b], DB.b)
                for mp in range(64):
                    q = mp % 2
                    ps1, ps2 = p1[q], p2[q]
                    for j in range(2):
                        m = mp * 2 + j
                        P.op("tensor", lambda e, m=m, j=j, ps1=ps1: e.matmul(ps1[:, j * 2 * T1:(j + 1) * 2 * T1], lhsT=DA[:, m, :], rhs=cbf[0:T1, CB["f1a"]], start=True, stop=False), [DA.b, cbf.b], [ps1.b])
                        P.op("tensor", lambda e, m=m, j=j, ps1=ps1: e.matmul(ps1[:, j * 2 * T1:(j + 1) * 2 * T1], lhsT=DB[:, m, :], rhs=cbf[0:T1, CB["f1b"]], start=False, stop=True), [DB.b, cbf.b], [ps1.b])
                    x1_, ta, tb, br_, bi_ = xs1[q], tA[q], tB[q], br[q], bi[q]
                    P.op("scalar", lambda e, x1_=x1_, ps1=ps1: e.activation(out=x1_[:].rearrange("p a b c -> p (a b c)"), in_=ps1[:, 0:4 * T1], func=AF.Copy), [ps1.b], [x1_.b])
                    re_, im_ = x1_[:, :, 0, :], x1_[:, :, 1, :]
                    P.op("vector", lambda e, ta=ta, re_=re_: e.tensor_tensor(out=ta[:], in0=re_, in1=twc3, op=ALU.mult), [x1_.b, cst.b], [ta.b])
                    P.op("gpsimd", lambda e, tb=tb, im_=im_: e.tensor_tensor(out=tb[:], in0=im_, in1=tws3, op=ALU.mult), [x1_.b, cst.b], [tb.b])
                    P.op("vector", lambda e, ta=ta, tb=tb, br_=br_: e.tensor_tensor(out=br_[:], in0=ta[:], in1=tb[:], op=ALU.add), [ta.b, tb.b], [br_.b])
                    P.op("gpsimd", lambda e, tb=tb, im_=im_: e.tensor_tensor(out=tb[:], in0=im_, in1=twc3, op=ALU.mult), [x1_.b, cst.b, br_.b], [tb.b])
                    P.op("vector", lambda e, ta=ta, re_=re_: e.tensor_tensor(out=ta[:], in0=re_, in1=tws3, op=ALU.mult), [x1_.b, cst.b, br_.b], [ta.b])
                    P.op("gpsimd", lambda e, ta=ta, tb=tb, bi_=bi_: e.tensor_tensor(out=bi_[:], in0=tb[:], in1=ta[:], op=ALU.subtract), [ta.b, tb.b], [bi_.b])
                    for j in range(2):
                        m = mp * 2 + j
                        P.op("tensor", lambda e, j=j, ps2=ps2, br_=br_: e.matmul(ps2[0:T1, j * 128:(j + 1) * 128], lhsT=br_[:, j, :], rhs=cbf[:, CB["c128"]], start=True, stop=False), [br_.b, cbf.b], [ps2.b])
                        P.op("tensor", lambda e, j=j, ps2=ps2, bi_=bi_: e.matmul(ps2[0:T1, j * 128:(j + 1) * 128], lhsT=bi_[:, j, :], rhs=cbf[:, CB["s128"]], start=False, stop=True), [bi_.b, cbf.b], [ps2.b])
                    P.op("scalar", lambda e, mp=mp, ps2=ps2: e.activation(out=Yall[:, :, mp * 2:mp * 2 + 2].rearrange("p k m -> p m k"), in_=ps2[0:T1, 0:256].rearrange("p (m k) -> p m k", k=128), func=AF.Copy),
                         [ps2.b], [Yall.b])
                for blk in range(NB):
                    yb = yo[blk % 2]
                    for jj in range(NK2):
                        k2 = blk * NK2 + jj
                        P.op("tensor", lambda e, k2=k2, jj=jj: e.transpose(ptr[:, jj * T1:(jj + 1) * T1], Yall[:, k2, :], cbf[0:T1, 384:384 + T1]), [Yall.b, cbf.b], [ptr.b])
                    P.op("vector", lambda e, yb=yb: e.tensor_copy(out=yb[:], in_=ptr[:, 0:TB]), [ptr.b], [yb.b])
                    P.dma("gpsimd", lambda e, yb=yb, blk=blk, gi=gi: e.dma_start(out=YT[blk * 128:(blk + 1) * 128, (8 + gi) * TB:(9 + gi) * TB], in_=yb[:]), [yb.b], YT.b)
    _phase4()
    P.barrier()

    if cfg.stop == "A2":
        P.build(final_waits=[HT.b, ADA.b, YT.b] + dumps)
        return nc, P

    NT = L // 128
    affall = sb(top, "affall", [128, NT, E])
    thr = sb(top, "thr", [128, E])
    def _phase5():
        with (contextlib.nullcontext(top) if cfg.noscope else contextlib.ExitStack()) as st:
            wst = sb(st, "bwst", [128, 1, 2048])
            Wo = sb(st, "Wo", [128, KC, D], BF16)
            wrf = sb(st, "wrf", [128, KC, E])
            wrb = sb(st, "wrb", [128, KC, E], BF16)
            yb = [sb(st, "byb0", [128, KC, TB], BF16)] * 2
            xb = [sb(st, "bxb0", [128, KC, TB])] * 2
            sq = sb(st, "bsq", [128, KC, TB], BF16)
            rs = sb(st, "brs", [128, TB])
            hf = [sb(st, "bhf0", [128, KC, TB], BF16)] * 2
            mx = sb(st, "bmx", [128, 4])
            sm = sb(st, "bsm", [128, 4])
            ee = sb(st, "bee", [128, 4, E])
            po = [psb(st, "po%d" % i) for i in range(2)]
            pq = psb(st, "pq")
            pl = psb(st, "pl")
            for kq in range(KC):
                P.dma("sync", lambda e, kq=kq: e.dma_start(out=wst[:], in_=w_out_v[:, kq:kq + 1, :]), [], wst.b)
                P.op("vector", lambda e, kq=kq: e.tensor_copy(out=Wo[:, kq:kq + 1, :], in_=wst[:]), [wst.b], [Wo.b])
            P.dma("sync", lambda e: e.dma_start(out=wrf[:], in_=wr_v), [], wrf.b)
            P.op("vector", lambda e: e.tensor_copy(out=wrb[:], in_=wrf[:]), [wrf.b], [wrb.b])
            for blk in range(NB):
                y_, x_, h_ = yb[0], xb[0], hf[0]
                x1 = x_
                P.dma("sync", lambda e, y_=y_, blk=blk: e.dma_start(out=y_[:].rearrange("p k t -> p (k t)"), in_=YT[blk * 128:(blk + 1) * 128, :]), [YT.b], y_.b)
                P.dma("sync", lambda e, x_=x_, blk=blk: e.dma_start(out=x_[:], in_=xT_v[:, :, blk * TB:(blk + 1) * TB]), [], x_.b)
                for nb in range(KC):
                    pp = po[nb % 2]
                    for k in range(KC):
                        P.op("tensor", lambda e, pp=pp, k=k, nb=nb, y_=y_: e.matmul(pp[:, :], lhsT=Wo[:, k, nb * 128:(nb + 1) * 128], rhs=y_[:, k, :], start=(k == 0), stop=(k == KC - 1)), [Wo.b, y_.b], [pp.b])
                    P.op("vector", lambda e, pp=pp, nb=nb, x_=x_: e.scalar_tensor_tensor(out=x1[:, nb, :], in0=pp[:, :], scalar=mods[:, 32 + nb:33 + nb], in1=x_[:, nb, :], op0=ALU.mult, op1=ALU.add),
                         [pp.b, mods.b, x_.b], [x1.b])
                P.dma("gpsimd", lambda e, blk=blk: e.dma_start(out=X1[blk * 128:(blk + 1) * 128, :], in_=x1[:].rearrange("p k t -> p (k t)")), [x1.b], X1.b)
                P.op("gpsimd", lambda e: e.tensor_tensor(out=sq[:], in0=x1[:], in1=x1[:], op=ALU.mult), [x1.b], [sq.b])
                rstd_from_sq(pq, sq, KC, TB, rs, 1.0 / D)
                P.op("vector", lambda e: e.tensor_tensor(out=x1[:], in0=x1[:], in1=rs[:].unsqueeze(1).to_broadcast([128, KC, TB]), op=ALU.mult), [x1.b, rs.b], [x1.b])
                P.op("gpsimd", lambda e: e.tensor_tensor(out=x1[:], in0=x1[:], in1=G2[:, 0:16].unsqueeze(2).to_broadcast([128, KC, TB]), op=ALU.mult), [x1.b, G2.b], [x1.b])
                P.op("vector", lambda e, h_=h_: e.tensor_tensor(out=h_[:], in0=x1[:], in1=mods[:, 48:64].unsqueeze(2).to_broadcast([128, KC, TB]), op=ALU.add), [x1.b, mods.b], [h_.b])
                P.dma("gpsimd", lambda e, h_=h_, blk=blk: e.dma_start(out=HF[blk * 128:(blk + 1) * 128, :], in_=h_[:].rearrange("p k t -> p (k t)")), [h_.b], HF.b)
                for s in range(4):
                    for k in range(KC):
                        P.op("tensor", lambda e, s=s, k=k, h_=h_: e.matmul(pl[:, s * E:(s + 1) * E], lhsT=h_[:, k, s * 128:(s + 1) * 128], rhs=wrb[:, k, :], start=(k == 0), stop=(k == KC - 1)), [h_.b, wrb.b], [pl.b])
                pl3 = pl[:, 0:4 * E].rearrange("p (s e) -> p s e", e=E)
                P.op("vector", lambda e: e.tensor_reduce(out=mx[:], in_=pl3, axis=AX.X, op=ALU.max), [pl.b], [mx.b])
                P.op("vector", lambda e: e.tensor_tensor(out=ee[:], in0=pl3, in1=mx[:].unsqueeze(2).to_broadcast([128, 4, E]), op=ALU.subtract), [pl.b, mx.b], [ee.b])
                P.op("scalar", lambda e: e.activation(out=ee[:], in_=ee[:], func=AF.Exp), [ee.b], [ee.b])
                P.op("vector", lambda e: e.tensor_reduce(out=sm[:], in_=ee[:], axis=AX.X, op=ALU.add), [ee.b], [sm.b])
                P.op("vector", lambda e: e.reciprocal(out=sm[:], in_=sm[:]), [sm.b], [sm.b])
                P.op("vector", lambda e, blk=blk: e.tensor_tensor(out=affall[:, blk * 4:(blk + 1) * 4, :], in0=ee[:], in1=sm[:].unsqueeze(2).to_broadcast([128, 4, E]), op=ALU.mult), [ee.b, sm.b], [affall.b])
                P.dma("gpsimd", lambda e, blk=blk: e.dma_start(out=AFS[blk * 128:(blk + 1) * 128, :], in_=affall[:, blk * 4:(blk + 1) * 4, :].rearrange("p s e -> p (s e)")), [affall.b], AFS.b)
    _phase5()
    P.barrier()

    if cfg.stop == "B":
        P.build(final_waits=[HT.b, ADA.b, YT.b, X1.b, AFS.b] + dumps)
        return nc, P

    def _phase6():
        with (contextlib.nullcontext(top) if cfg.noscope else contextlib.ExitStack()) as st:
            mid = sb(st, "mid", [128, E])
            cmpt = sb(st, "cmpt", [128, E, NT])
            pc = sb(st, "pc", [128, E])
            ge = sb(st, "ge", [128, E])
            pc_ps = psb(st, "pcps")
            aff_v = affall[:].rearrange("p t e -> p e t")
            P.op("vector", lambda e: e.memset(thr[:], 0.0), [], [thr.b])
            for it in range(30):
                s_i = 2.0 ** -(it + 1)
                P.op("vector", lambda e, s_i=s_i: e.tensor_scalar(out=mid[:], in0=thr[:], scalar1=s_i, scalar2=None, op0=ALU.add), [thr.b], [mid.b])
                P.op("vector", lambda e: e.tensor_tensor(out=cmpt[:], in0=aff_v, in1=mid[:].unsqueeze(2).to_broadcast([128, E, NT]), op=ALU.is_gt), [affall.b, mid.b], [cmpt.b])
                P.op("vector", lambda e: e.tensor_reduce(out=pc[:], in_=cmpt[:], axis=AX.X, op=ALU.add), [cmpt.b], [pc.b])
                P.op("tensor", lambda e: e.matmul(pc_ps[:, 0:E], lhsT=cst[:, cs("ones")], rhs=pc[:], start=True, stop=True), [cst.b, pc.b], [pc_ps.b])
                P.op("vector", lambda e, s_i=s_i: e.tensor_scalar(out=ge[:], in0=pc_ps[:, 0:E], scalar1=float(cfg.CAP) - 0.5, scalar2=s_i, op0=ALU.is_gt, op1=ALU.mult), [pc_ps.b], [ge.b])
                P.op("vector", lambda e: e.tensor_tensor(out=thr[:], in0=thr[:], in1=ge[:], op=ALU.add), [thr.b, ge.b], [thr.b])
    _phase6()
    P.barrier()

    if cfg.stop == "C":
        P.build(final_waits=[HT.b, ADA.b, YT.b, X1.b, AFS.b] + dumps)
        return nc, P

    S = TG // 128
    def _phase7():
        with (contextlib.nullcontext(top) if cfg.noscope else contextlib.ExitStack()) as st:
            hfT = sb(st, "hfT", [128, KC, TG], BF16)
            acc = sb(st, "acc", [128, KC, TG])
            afo = sb(st, "afo", [128, S, E])
            msk = sb(st, "msk", [128, S, E])
            wgt = sb(st, "wgt", [128, S, E])
            wgT = sb(st, "wgT", [E, TG])
            wb = [sb(st, "wb%d" % i, [128, TG], BF16) for i in range(2)]
            gst = [sb(st, "gst%d" % i, [128, KC, 128]) for i in range(2)]
            ust = [sb(st, "ust%d" % i, [128, KC, 128]) for i in range(2)]
            gbf = [sb(st, "gbf%d" % i, [128, KC, 128], BF16) for i in range(2)]
            ubf = [sb(st, "ubf%d" % i, [128, KC, 128], BF16) for i in range(2)]
            dst_ = [sb(st, "dst%d" % i, [128, FC, 128]) for i in range(2)]
            dbf = [sb(st, "dbf%d" % i, [128, FC, 128], BF16) for i in range(2)]
            sgm = [sb(st, "sgm%d" % i, [128, TG], BF16) for i in range(2)]
            tmu = [sb(st, "tmu%d" % i, [128, TG], BF16) for i in range(2)]
            hid = sb(st, "hid", [128, FC, TG], BF16)
            sq = sb(st, "dsq", [128, KC, TG], BF16)
            rs = sb(st, "drs", [128, TG])
            pg = [psb(st, "pg%d" % i) for i in range(2)]
            pu_ = [psb(st, "pu%d" % i) for i in range(2)]
            py = [psb(st, "py%d" % i) for i in range(2)]
            pw = psb(st, "pw")
            pm = psb(st, "pm")
            X1f = X1.t.ap()
            HFf = HF.t.ap()
            AFf = AFS.t.ap()
            cnt = [0, 0]
            for g in range(NG):
                for jb in range(TG // TB):
                    col = g * (TG // TB) + jb
                    P.dma("gpsimd", lambda e, col=col, jb=jb: e.indirect_dma_start(out=hfT[:].rearrange("p k t -> p (k t)"), out_offset=None, in_=HFf,
                                                                                     in_offset=bass.IndirectOffsetOnAxis(ap=idx[:, col:col + 1], axis=0)), [HF.b, idx.b], hfT.b)
                    P.dma("gpsimd", lambda e, col=col, jb=jb: e.indirect_dma_start(out=acc[:].rearrange("p k t -> p (k t)"), out_offset=None, in_=X1f,
                                                                                     in_offset=bass.IndirectOffsetOnAxis(ap=idx[:, col:col + 1], axis=0)), [X1.b, idx.b], acc.b)
                    P.dma("gpsimd", lambda e, col=col, jb=jb: e.indirect_dma_start(out=afo[:, jb * 4:(jb + 1) * 4, :].rearrange("p s e -> p (s e)"), out_offset=None, in_=AFf,
                                                                                     in_offset=bass.IndirectOffsetOnAxis(ap=idx[:, col:col + 1], axis=0)), [AFS.b, idx.b], afo.b)
                P.op("vector", lambda e: e.tensor_tensor(out=msk[:], in0=afo[:], in1=thr[:].unsqueeze(1).to_broadcast([128, S, E]), op=ALU.is_gt), [afo.b, thr.b], [msk.b])
                P.op("vector", lambda e: e.tensor_tensor(out=wgt[:], in0=afo[:], in1=msk[:], op=ALU.mult), [afo.b, msk.b], [wgt.b])
                for s in range(S):
                    P.op("tensor", lambda e, s=s: e.matmul(pm[0:E, s * 128:(s + 1) * 128], lhsT=wgt[:, s, :], rhs=cst[:, cs("ident")], start=True, stop=True), [wgt.b, cst.b], [pm.b])
                P.op("vector", lambda e: e.tensor_copy(out=wgT[:], in_=pm[0:E, 0:TG]), [pm.b], [wgT.b])
                for ex_ in range(E):
                    wbe = wb[ex_ % 2]
                    so = lay["sel"][0] + ex_ * 128
                    P.op("tensor", lambda e, so=so: e.matmul(pw[:, 0:TG], lhsT=cst[0:E, so:so + 128], rhs=wgT[:], start=True, stop=True), [cst.b, wgT.b], [pw.b])
                    P.op("scalar", lambda e, wbe=wbe: e.activation(out=wbe[:], in_=pw[:, 0:TG], func=AF.Copy), [pw.b], [wbe.b])
                    for fb in range(FC):
                        i = cnt[0] % 2
                        cnt[0] += 1
                        gs, us, gb, ub = gst[i], ust[i], gbf[i], ubf[i]
                        P.dma("sync", lambda e, gs=gs, ex_=ex_, fb=fb: e.dma_start(out=gs[:], in_=w_eg[ex_].rearrange("(k p) f -> p k f", p=128)[:, :, fb * 128:(fb + 1) * 128]), [], gs.b)
                        P.dma("sync", lambda e, us=us, ex_=ex_, fb=fb: e.dma_start(out=us[:], in_=w_eu[ex_].rearrange("(k p) f -> p k f", p=128)[:, :, fb * 128:(fb + 1) * 128]), [], us.b)
                        P.op("gpsimd", lambda e, gs=gs, gb=gb: e.tensor_copy(out=gb[:], in_=gs[:]), [gs.b], [gb.b])
                        P.op("vector", lambda e, us=us, ub=ub: e.tensor_copy(out=ub[:], in_=us[:]), [us.b], [ub.b])
                        pgi, pui, sgi, tmi = pg[i], pu_[i], sgm[i], tmu[i]
                        for k in range(KC):
                            P.op("tensor", lambda e, k=k, gb=gb, pgi=pgi: e.matmul(pgi[:, 0:TG], lhsT=gb[:, k, :], rhs=hfT[:, k, :], start=(k == 0), stop=(k == KC - 1)), [gb.b, hfT.b], [pgi.b])
                        for k in range(KC):
                            P.op("tensor", lambda e, k=k, ub=ub, pui=pui: e.matmul(pui[:, 0:TG], lhsT=ub[:, k, :], rhs=hfT[:, k, :], start=(k == 0), stop=(k == KC - 1)), [ub.b, hfT.b], [pui.b])
                        P.op("scalar", lambda e, sgi=sgi, pgi=pgi: e.activation(out=sgi[:], in_=pgi[:, 0:TG], func=AF.Silu), [pgi.b], [sgi.b])
                        P.op("vector", lambda e, sgi=sgi, pui=pui, tmi=tmi: e.tensor_tensor(out=tmi[:], in0=pui[:, 0:TG], in1=sgi[:], op=ALU.mult), [pui.b, sgi.b], [tmi.b])
                        P.op("gpsimd", lambda e, tmi=tmi, fb=fb, wbe=wbe: e.tensor_tensor(out=hid[:, fb, :], in0=tmi[:], in1=wbe[:], op=ALU.mult), [tmi.b, wbe.b], [hid.b])
                    for db in range(KC):
                        i = cnt[1] % 2
                        cnt[1] += 1
                        ds_, dbb, pyi = dst_[i], dbf[i], py[i]
                        P.dma("sync", lambda e, ds_=ds_, ex_=ex_, db=db: e.dma_start(out=ds_[:], in_=w_ed[ex_].rearrange("(c p) d -> p c d", p=128)[:, :, db * 128:(db + 1) * 128]), [], ds_.b)
                        P.op("gpsimd", lambda e, ds_=ds_, dbb=dbb: e.tensor_copy(out=dbb[:], in_=ds_[:]), [ds_.b], [dbb.b])
                        for fc in range(FC):
                            P.op("tensor", lambda e, fc=fc, dbb=dbb, pyi=pyi: e.matmul(pyi[:, 0:TG], lhsT=dbb[:, fc, :], rhs=hid[:, fc, :], start=(fc == 0), stop=(fc == FC - 1)), [dbb.b, hid.b], [pyi.b])
                        P.op("vector", lambda e, db=db, pyi=pyi: e.scalar_tensor_tensor(out=acc[:, db, :], in0=pyi[:, 0:TG], scalar=mods[:, 80 + db:81 + db], in1=acc[:, db, :], op0=ALU.mult, op1=ALU.add),
                             [pyi.b, mods.b, acc.b], [acc.b])
                P.op("gpsimd", lambda e: e.tensor_tensor(out=sq[:], in0=acc[:], in1=acc[:], op=ALU.mult), [acc.b], [sq.b])
                rstd_from_sq(pw, sq, KC, TG, rs, 1.0 / D)
                P.op("vector", lambda e: e.tensor_tensor(out=acc[:], in0=acc[:], in1=rs[:].unsqueeze(1).to_broadcast([128, KC, TG]), op=ALU.mult), [acc.b, rs.b], [acc.b])
                P.op("gpsimd", lambda e: e.tensor_tensor(out=acc[:], in0=acc[:], in1=ncl[:, 32:48].unsqueeze(2).to_broadcast([128, KC, TG]), op=ALU.mult), [acc.b, ncl.b], [acc.b])
                P.dma("sync", lambda e, g=g: e.dma_start(out=outT.t.ap().rearrange("(k p) t -> p k t", p=128)[:, :, g * TG:(g + 1) * TG], in_=acc[:]), [acc.b], outT.b)
    _phase7()

    finals = [outT.b]
    if cfg.dev:
        finals += [YT.b, X1.b, AFS.b, ADA.b, HT.b] + dumps
    P.build(final_waits=finals)
    top.close()
    return nc, P


def host_inputs(cfg, core, x, c, ctx, c_ctx, w_ada, b_ada, norm1_g, w_in, w_a2_f, b_a2_f, w_a2_b, b_a2_b,
                gla_norm_g, w_out, norm2_g, w_router, w_e_gate, w_e_up, w_e_down, final_norm_g, shared):
    b, r = core // 4, core % 4
    f = lambda a: np.ascontiguousarray(np.asarray(a, dtype=np.float32))
    col = lambda v: f(np.asarray(v).reshape(-1, 128).T)
    m = {}
    m["xT"] = shared["xT"][b]
    m["ctxT"] = shared["ctxT"][b]
    m["cond"] = f(np.concatenate([col(c[b]), col(c_ctx)], axis=1))
    m["w_ada"] = shared["w_ada"]
    m["b_ada"] = shared["b_ada"]
    m["ncols"] = shared["ncols"]
    m["glag"] = shared["glag"]
    m["w_in"] = shared["w_in"]
    m["wa2"] = shared["wa2"]
    m["ba2"] = shared["ba2"]
    m["w_out"] = shared["w_out"]
    m["w_router"] = shared["w_router"]
    m["w_eg"] = shared["w_eg"]
    m["w_eu"] = shared["w_eu"]
    m["w_ed"] = shared["w_ed"]
    m["cst"] = shared["cst"]
    ob0 = r * cfg.NOB
    m["idx"] = np.stack([(ob0 + j) * 128 + np.arange(128) for j in range(cfg.NOB)], axis=1).astype(np.int32)
    return m


def host_shared(cfg, x, c, ctx, c_ctx, w_ada, b_ada, norm1_g, w_in, w_a2_f, b_a2_f, w_a2_b, b_a2_b,
                gla_norm_g, w_out, norm2_g, w_router, w_e_gate, w_e_up, w_e_down, final_norm_g, batches=(0, 1)):
    f = lambda a: np.ascontiguousarray(np.asarray(a, dtype=np.float32))
    col = lambda v: f(np.asarray(v).reshape(-1, 128).T)
    sh = {}
    sh["xT"] = {b: f(np.asarray(x[b]).T) for b in batches}
    sh["ctxT"] = {b: f(np.asarray(ctx[b]).T) for b in batches}
    sh["w_ada"] = f(w_ada[0])
    sh["b_ada"] = f(b_ada[0]).reshape(1, -1)
    sh["ncols"] = f(np.concatenate([col(norm1_g[0]), col(norm2_g[0]), col(final_norm_g)], axis=1))
    sh["glag"] = col(gla_norm_g[0])
    sh["w_in"] = f(w_in[0])
    sh["wa2"] = f(np.concatenate([np.asarray(w_a2_f[0]), np.asarray(w_a2_b[0])], axis=1))
    sh["ba2"] = f(np.concatenate([col(b_a2_f[0]), col(b_a2_b[0])], axis=1))
    sh["w_out"] = f(w_out[0])
    sh["w_router"] = f(w_router[0])
    sh["w_eg"] = f(w_e_gate[0])
    sh["w_eu"] = f(w_e_up[0])
    sh["w_ed"] = f(w_e_down[0])
    sh["cst"] = make_consts(cfg)
    return sh


def kernel(**inputs):
    cfg = Cfg()
    nc, _ = build(cfg)
    sh = host_shared(cfg, **inputs)
    in_maps = [host_inputs(cfg, core, shared=sh, **inputs) for core in range(8)]
    res = run_bass_kernel_spmd(nc, in_maps, core_ids=list(range(8)))
    out = np.empty((2, cfg.L, D), np.float32)
    for core in range(8):
        b, r = core // 4, core % 4
        out[b, r * cfg.OWN:(r + 1) * cfg.OWN, :] = np.asarray(res.results[core]["outT"]).T
    return out
```

```python
import contextlib
import numpy as np
import concourse.bass as bass
import concourse.mybir as mybir
from concourse.bass_utils import run_bass_kernel_spmd

F32 = mybir.dt.float32
BF16 = mybir.dt.bfloat16
I32 = mybir.dt.int32
ALU = mybir.AluOpType
AF = mybir.ActivationFunctionType
AX = mybir.AxisListType

D = 2048
KC = 16
TB = 512
EPS = 1e-6
ENGS = ("sync", "scalar", "vector", "gpsimd", "tensor")


class Buf:
    __slots__ = ("name", "last_w", "readers", "dma_sem", "dma_cnt", "dma_writers")

    def __init__(self, name):
        self.name = name
        self.last_w = None
        self.readers = {}
        self.dma_sem = None
        self.dma_cnt = 0
        self.dma_writers = False


class Prog:
    def __init__(self, nc):
        self.nc = nc
        self.ins = []
        self.last_on = {e: None for e in ENGS}
        self.dma_bufs = []
        self.pending = {e: None for e in ENGS}

    def _emit(self, eng, fn, reads, writes, dma=False):
        iid = len(self.ins)
        deps = set()
        for b in reads:
            if b.dma_writers:
                deps.add(("dma", b, b.dma_cnt))
            elif b.last_w is not None:
                deps.add(("ins", b.last_w))
        for b in writes:
            if b.dma_writers:
                deps.add(("dma", b, b.dma_cnt))
            elif b.last_w is not None:
                deps.add(("ins", b.last_w))
            for r in b.readers.values():
                deps.add(("ins", r))
        if self.pending[eng] is not None:
            deps |= self.pending[eng]
            self.pending[eng] = None
        rec = dict(eng=eng, fn=fn, deps=deps, dma=dma, dst=None, signal=False, cnt=None)
        if dma:
            d = writes[0]
            rec["dst"] = d
            d.dma_cnt += 1
            rec["cnt"] = d.dma_cnt
            d.dma_writers = True
            d.last_w = None
            d.readers = {}
            if d not in self.dma_bufs:
                self.dma_bufs.append(d)
        else:
            for b in writes:
                b.last_w = iid
                b.dma_writers = False
                b.readers = {}
            self.last_on[eng] = iid
        rkey = ("dma", id(writes[0])) if dma else eng
        for b in reads:
            if b not in writes:
                b.readers[rkey] = iid
        self.ins.append(rec)
        return iid

    def op(self, eng, fn, reads=(), writes=()):
        return self._emit(eng, fn, list(reads), list(writes))

    def dma(self, eng, fn, reads, write):
        return self._emit(eng, fn, list(reads), [write], dma=True)

    def barrier(self):
        deps = set()
        for e in ENGS:
            if self.last_on[e] is not None:
                deps.add(("ins", self.last_on[e]))
        for b in self.dma_bufs:
            deps.add(("dma", b, b.dma_cnt))
        for e in ENGS:
            self.pending[e] = set(deps) | (self.pending[e] or set())

    def build(self, final_waits=()):
        nc = self.nc
        ins = self.ins
        for rec in ins:
            for d in rec["deps"]:
                if d[0] == "ins":
                    p = ins[d[1]]
                    if p["dma"]:
                        continue
                    if p["eng"] == "tensor" and rec["eng"] == "tensor":
                        continue
                    p["signal"] = True
        cnt = {e: 0 for e in ENGS}
        for rec in ins:
            if not rec["dma"] and rec["signal"]:
                cnt[rec["eng"]] += 1
                rec["cnt"] = cnt[rec["eng"]]
        self.cnt = cnt
        with contextlib.ExitStack() as st:
            esem = {e: st.enter_context(nc.semaphore("s_" + e)) for e in ENGS}
            for i, b in enumerate(self.dma_bufs):
                b.dma_sem = st.enter_context(nc.semaphore("d%d" % i))
            block = st.enter_context(nc.Block())
            per = {e: [r for r in ins if r["eng"] == e] for e in ENGS}

            def run(engname, eng):
                known = {}
                for rec in per[engname]:
                    waits = {}
                    for d in rec["deps"]:
                        if d[0] == "dma":
                            s, v = d[1].dma_sem, 16 * d[2]
                        else:
                            p = ins[d[1]]
                            if p["dma"]:
                                s, v = p["dst"].dma_sem, 16 * p["cnt"]
                            else:
                                if p["eng"] == "tensor" and engname == "tensor":
                                    continue
                                s, v = esem[p["eng"]], p["cnt"]
                        if known.get(id(s), 0) >= v:
                            continue
                        if waits.get(id(s), (None, 0))[1] < v:
                            waits[id(s)] = (s, v)
                    for s, v in waits.values():
                        eng.wait_ge(s, v)
                        known[id(s)] = v
                    r = rec["fn"](eng)
                    if rec["dma"]:
                        r.then_inc(rec["dst"].dma_sem, 16)
                    elif rec["signal"]:
                        r.then_inc(esem[engname], 1)
                if engname == "sync":
                    for b in final_waits:
                        eng.wait_ge(b.dma_sem, 16 * b.dma_cnt)

            @block.sync
            def _(e):
                run("sync", e)

            @block.scalar
            def _(e):
                run("scalar", e)

            @block.vector
            def _(e):
                run("vector", e)

            @block.gpsimd
            def _(e):
                run("gpsimd", e)

            @block.tensor
            def _(e):
                run("tensor", e)


class T:
    def __init__(self, t, name):
        self.t = t
        self.b = Buf(name)

    def __getitem__(self, k):
        return self.t[k]


class Cfg:
    def __init__(self, L=16384, CTX=256, E=16, DE=1024, dev=False):
        self.L, self.CTX, self.E, self.DE, self.dev = L, CTX, E, DE, dev
        self.stop = None
        self.noscope = False
        self.T1 = L // 128
        self.NB = L // TB
        self.OWN = L // 4
        self.NOB = self.OWN // TB
        self.TG = TB
        self.NG = self.OWN // self.TG
        self.FC = DE // 128
        self.CAP = 2 * L // E
        self.PW = 4128


def const_layout(cfg):
    lay = {}
    off = 0
    for name, w in (("ident", 128), ("ones", 128), ("maskf", 128), ("maskb", 128), ("c128", 128),
                    ("s128", 128), ("reset", TB), ("twc", cfg.T1), ("tws", cfg.T1),
                    ("f1a", 2 * cfg.T1), ("f1b", 2 * cfg.T1), ("sel", cfg.E * 128)):
        lay[name] = (off, w)
        off += w
    return lay, off


def make_consts(cfg):
    lay, ncol = const_layout(cfg)
    c = np.zeros((128, ncol), np.float64)

    def put(name, arr):
        o, w = lay[name]
        c[:arr.shape[0], o:o + w] = arr

    a = np.arange(128)
    put("ident", np.eye(128))
    put("ones", np.ones((128, 128)))
    put("maskf", (a[:, None] <= a[None, :]).astype(np.float64))
    put("maskb", (a[:, None] >= a[None, :]).astype(np.float64))
    ang = 2 * np.pi * np.outer(a, a) / 128.0
    put("c128", np.cos(ang))
    put("s128", np.sin(ang))
    r = np.ones((128, TB))
    r[:, ::128] = 0.0
    put("reset", r)
    T1, L = cfg.T1, cfg.L
    k1 = np.arange(T1)
    nrm = 1.0 / np.sqrt(L * 128.0)
    angt = 2 * np.pi * np.outer(a, k1) / L
    put("twc", np.cos(angt) * nrm)
    put("tws", np.sin(angt) * nrm)
    ang1 = 2 * np.pi * np.outer(k1, k1) / T1
    put("f1a", np.concatenate([np.cos(ang1), -np.sin(ang1)], axis=1))
    put("f1b", np.concatenate([-np.sin(ang1), -np.cos(ang1)], axis=1))
    sel = np.zeros((cfg.E, cfg.E * 128))
    for e in range(cfg.E):
        sel[e, e * 128:(e + 1) * 128] = 1.0
    put("sel", sel)
    return c.astype(np.float32)


def build(cfg):
    L, CTX, E, DE, T1, NB, OWN, NOB = cfg.L, cfg.CTX, cfg.E, cfg.DE, cfg.T1, cfg.NB, cfg.OWN, cfg.NOB
    TG, NG, FC, PW = cfg.TG, cfg.NG, cfg.FC, cfg.PW
    lay, ncol = const_layout(cfg)
    nc = bass.Bass("TRN2", target_bir_lowering=False)
    P = Prog(nc)

    def din(name, shape, dt=F32):
        return nc.dram_tensor(name, list(shape), dt, kind="ExternalInput")

    dbg = "ExternalOutput" if cfg.dev else None

    def dscr(name, shape, dt, out=False):
        if out and dbg:
            return T(nc.dram_tensor(name, list(shape), dt, kind=dbg), name)
        return T(nc.dram_tensor(name, list(shape), dt), name)

    dumps = []

    def dump(name, tl, shape, dt=F32):
        if not cfg.dev:
            return
        dd = T(nc.dram_tensor("dbg_" + name, list(shape), dt, kind="ExternalOutput"), "dbg_" + name)
        P.dma("gpsimd", lambda e: e.dma_start(out=dd.t.ap(), in_=tl[:]), [tl.b], dd.b)
        dumps.append(dd.b)

    xT = din("xT", [D, L])
    ctxT = din("ctxT", [D, CTX])
    cond = din("cond", [128, 32])
    w_ada = din("w_ada", [D, 6 * D])
    b_ada = din("b_ada", [1, 6 * D])
    ncols = din("ncols", [128, 48])
    glag = din("glag", [128, 8])
    w_in = din("w_in", [D, PW])
    wa2 = din("wa2", [16, 1024])
    ba2 = din("ba2", [128, 8])
    w_out = din("w_out", [D, D])
    w_router = din("w_router", [D, E])
    w_eg = din("w_eg", [E * FC * 128, KC * 128])
    w_eu = din("w_eu", [E * FC * 128, KC * 128])
    w_ed = din("w_ed", [E * KC * 128, FC * 128])
    cst_d = din("cst", [128, ncol])
    idx_d = din("idx", [128, NOB], I32)
    outT = T(nc.dram_tensor("outT", [D, OWN], F32, kind="ExternalOutput"), "outT")

    ADA = dscr("ADA", [2, 6 * D], F32, out=True)
    HT = dscr("HT", [NB * 128, KC * TB], BF16, out=True)
    OB = dscr("OB", [NB * 128, 2 * TB], F32)
    YT = dscr("YT", [NB * 128, KC * TB], BF16, out=True)
    FA = dscr("FA", [128, L], BF16)
    FB = dscr("FB", [128, L], BF16)
    X1 = dscr("X1", [NB * 128, KC * TB], F32, out=True)
    HF = dscr("HF", [NB * 128, KC * TB], BF16, out=True)
    AFS = dscr("AFS", [NB * 128, 4 * E], F32, out=True)

    xT_v = xT.ap().rearrange("(k p) t -> p k t", p=128)
    ctxT_v = ctxT.ap().rearrange("(k p) t -> p k t", p=128)
    w_in_v = w_in.ap().rearrange("(k p) n -> p k n", p=128)
    w_out_v = w_out.ap().rearrange("(k p) n -> p k n", p=128)
    w_ada_v = w_ada.ap().rearrange("(k p) n -> p k n", p=128)
    wr_v = w_router.ap().rearrange("(k p) n -> p k n", p=128)

    top = contextlib.ExitStack()

    def sb(st, name, shape, dt=F32):
        return T(st.enter_context(nc.sbuf_tensor("sb_" + name, list(shape), dt)), name)

    def psb(st, name, dt=F32, n=512):
        return T(st.enter_context(nc.psum_tensor("ps_" + name, [128, n], dt)), name)

    def cs(name):
        o, w = lay[name]
        return slice(o, o + w)

    cst = sb(top, "cst", [128, ncol])
    P.dma("sync", lambda e: e.dma_start(out=cst[:], in_=cst_d[:, :]), [], cst.b)
    cbf = sb(top, "cbf", [128, 128 * 4 + 4 * T1], BF16)
    CB = {"ones": slice(0, 128), "c128": slice(128, 256), "s128": slice(256, 384), "ident": slice(384, 512),
          "f1a": slice(512, 512 + 2 * T1), "f1b": slice(512 + 2 * T1, 512 + 4 * T1)}
    for nm in ("ones", "c128", "s128", "ident", "f1a", "f1b"):
        P.op("vector", lambda e, nm=nm: e.tensor_copy(out=cbf[:, CB[nm]], in_=cst[:, cs(nm)]), [cst.b], [cbf.b])
    mods = sb(top, "mods", [128, 96])
    modc = sb(top, "modc", [128, 32])
    ncl = sb(top, "ncl", [128, 48])
    glg = sb(top, "glg", [128, 8])
    nb2 = sb(top, "nb2", [128, 8])
    wa2f = sb(top, "wa2f", [16, 1024])
    wa2b = sb(top, "wa2b", [16, 1024], BF16)
    G1 = sb(top, "G1", [128, 16])
    G1c = sb(top, "G1c", [128, 16])
    G2 = sb(top, "G2", [128, 16])
    idx = sb(top, "idx", [128, NOB], I32)
    P.dma("sync", lambda e: e.dma_start(out=ncl[:], in_=ncols[:, :]), [], ncl.b)
    P.dma("sync", lambda e: e.dma_start(out=glg[:], in_=glag[:, :]), [], glg.b)
    P.dma("sync", lambda e: e.dma_start(out=nb2[:], in_=ba2[:, :]), [], nb2.b)
    P.dma("sync", lambda e: e.dma_start(out=wa2f[:], in_=wa2[:, :]), [], wa2f.b)
    P.dma("sync", lambda e: e.dma_start(out=idx[:], in_=idx_d[:, :]), [], idx.b)
    P.op("vector", lambda e: e.tensor_scalar(out=nb2[:], in0=nb2[:], scalar1=-1.0, scalar2=None, op0=ALU.mult), [nb2.b], [nb2.b])
    P.op("vector", lambda e: e.tensor_copy(out=wa2b[:], in_=wa2f[:]), [wa2f.b], [wa2b.b])

    def _phase1():
        with (contextlib.nullcontext(top) if cfg.noscope else contextlib.ExitStack()) as st:
            cnd = sb(st, "cnd", [128, 32])
            scd = sb(st, "scd", [128, 32])
            P.dma("sync", lambda e: e.dma_start(out=cnd[:], in_=cond[:, :]), [], cnd.b)
            P.op("scalar", lambda e: e.activation(out=scd[:], in_=cnd[:], func=AF.Silu), [cnd.b], [scd.b])
            wst = [sb(st, "wst%d" % i, [128, 4, 512]) for i in range(4)]
            pr = [psb(st, "pr%d" % i) for i in range(2)]
            rows = [sb(st, "rows%d" % i, [2, 512]) for i in range(2)]
            bad = [sb(st, "bad%d" % i, [2, 512]) for i in range(2)]
            sc_v = scd[:].rearrange("p (c k) -> p c k", k=16)
            wi = 0
            for n in range(24):
                pt = pr[n % 2]
                bt = bad[n % 2]
                P.dma("sync", lambda e, bt=bt, n=n: e.dma_start(out=bt[:], in_=b_ada[0:1, n * 512:(n + 1) * 512].partition_broadcast(2)), [], bt.b)
                for kq in range(4):
                    w = wst[wi % 4]
                    wi += 1
                    P.dma("sync", lambda e, w=w, n=n, kq=kq: e.dma_start(out=w[:], in_=w_ada_v[:, kq * 4:(kq + 1) * 4, n * 512:(n + 1) * 512]), [], w.b)
                    for kk in range(4):
                        k = kq * 4 + kk
                        P.op("tensor", lambda e, pt=pt, w=w, kk=kk, k=k: e.matmul(pt[0:2, :], lhsT=sc_v[:, :, k], rhs=w[:, kk, :], start=(k == 0), stop=(k == 15)),
                             [scd.b, w.b], [pt.b])
                rw = rows[n % 2]
                P.op("vector", lambda e, rw=rw, pt=pt, bt=bt: e.tensor_tensor(out=rw[:], in0=pt[0:2, :], in1=bt[:], op=ALU.add), [pt.b, bt.b], [rw.b])
                P.dma("gpsimd", lambda e, rw=rw, n=n: e.dma_start(out=ADA[0:2, n * 512:(n + 1) * 512], in_=rw[:]), [rw.b], ADA.b)
            P.dma("sync", lambda e: e.dma_start(out=mods[:], in_=ADA[0:1, :].rearrange("o (j p) -> p (o j)", p=128), allow_slow_non_contiguous=True), [ADA.b], mods.b)
            P.dma("sync", lambda e: e.dma_start(out=modc[:], in_=ADA[1:2, 0:4096].rearrange("o (j p) -> p (o j)", p=128), allow_slow_non_contiguous=True), [ADA.b], modc.b)
            P.op("vector", lambda e: e.scalar_tensor_tensor(out=G1[:], in0=mods[:, 16:32], scalar=1.0, in1=ncl[:, 0:16], op0=ALU.add, op1=ALU.mult), [mods.b, ncl.b], [G1.b])
            P.op("vector", lambda e: e.scalar_tensor_tensor(out=G1c[:], in0=modc[:, 16:32], scalar=1.0, in1=ncl[:, 0:16], op0=ALU.add, op1=ALU.mult), [modc.b, ncl.b], [G1c.b])
            P.op("vector", lambda e: e.scalar_tensor_tensor(out=G2[:], in0=mods[:, 64:80], scalar=1.0, in1=ncl[:, 16:32], op0=ALU.add, op1=ALU.mult), [mods.b, ncl.b], [G2.b])
    _phase1()
    dump('mods', mods, [128, 96])
    dump('modc', modc, [128, 32])
    dump('G1', G1, [128, 16])
    P.barrier()

    def rstd_from_sq(pbank, sq, nk, n, dst, inv_dim):
        for k in range(nk):
            P.op("tensor", lambda e, k=k: e.matmul(pbank[:, 0:n], lhsT=cbf[:, CB["ones"]], rhs=sq[:, k, 0:n], start=(k == 0), stop=(k == nk - 1)),
                 [cbf.b, sq.b], [pbank.b])
        P.op("scalar", lambda e: e.activation(out=dst[:, 0:n], in_=pbank[:, 0:n], func=AF.Ln, scale=inv_dim, bias=EPS), [pbank.b], [dst.b])
        P.op("scalar", lambda e: e.activation(out=dst[:, 0:n], in_=dst[:, 0:n], func=AF.Exp, scale=-0.5), [dst.b], [dst.b])

    hc = sb(top, "hc", [128, KC, CTX], BF16)
    def _phase2():
        with (contextlib.nullcontext(top) if cfg.noscope else contextlib.ExitStack()) as st:
            xs = [sb(st, "xs%d" % i, [128, KC, TB]) for i in range(2)] if not cfg.noscope else [sb(st, "xs0", [128, KC, TB])] * 2
            sq = sb(st, "sq", [128, KC, TB], BF16)
            rs = sb(st, "rs", [128, TB])
            tm = sb(st, "tm", [128, KC, TB])
            hts = [sb(st, "hts%d" % i, [128, KC, TB], BF16) for i in range(2)]
            pb = psb(st, "a0p")

            def front(src_ap, n, xsl, Gc, SHc, dst):
                P.dma("sync", lambda e: e.dma_start(out=xsl[:, :, 0:n], in_=src_ap), [], xsl.b)
                P.op("gpsimd", lambda e: e.tensor_tensor(out=sq[:, :, 0:n], in0=xsl[:, :, 0:n], in1=xsl[:, :, 0:n], op=ALU.mult), [xsl.b], [sq.b])
                rstd_from_sq(pb, sq, KC, n, rs, 1.0 / D)
                if n == CTX:
                    dump('xsl', xsl, [128, KC, TB])
                    dump('rs', rs, [128, TB])
                    dump('sq', sq, [128, KC, TB], BF16)
                P.op("vector", lambda e: e.tensor_tensor(out=tm[:, :, 0:n], in0=xsl[:, :, 0:n], in1=rs[:, 0:n].unsqueeze(1).to_broadcast([128, KC, n]), op=ALU.mult),
                     [xsl.b, rs.b], [tm.b])
                P.op("gpsimd", lambda e: e.tensor_tensor(out=tm[:, :, 0:n], in0=tm[:, :, 0:n], in1=Gc[:, 0:16].unsqueeze(2).to_broadcast([128, KC, n]), op=ALU.mult),
                     [tm.b, Gc.b], [tm.b])
                P.op("vector", lambda e: e.tensor_tensor(out=dst[:, :, 0:n], in0=tm[:, :, 0:n], in1=SHc.unsqueeze(2).to_broadcast([128, KC, n]), op=ALU.add),
                     [tm.b, mods.b, modc.b], [dst.b])

            front(ctxT_v[:, :, :], CTX, xs[0], G1c, modc[:, 0:16], hc)
            for blk in range(NB):
                h = hts[blk % 2]
                front(xT_v[:, :, blk * TB:(blk + 1) * TB], TB, xs[(blk + 1) % 2], G1, mods[:, 0:16], h)
                P.dma("gpsimd", lambda e, h=h, blk=blk: e.dma_start(out=HT[blk * 128:(blk + 1) * 128, :], in_=h[:].rearrange("p k t -> p (k t)")), [h.b], HT.b)
    _phase2()
    P.barrier()
    if cfg.stop == "A0":
        P.build(final_waits=[HT.b, ADA.b] + dumps)
        return nc, P

    def _phase3():
        with (contextlib.nullcontext(top) if cfg.noscope else contextlib.ExitStack()) as st:
            wstg = [sb(st, "wstg%d" % i, [128, KC, 256]) for i in range(2)]
            Wq = sb(st, "Wq", [128, KC, 128], BF16)
            Wk = sb(st, "Wk", [128, KC, 128], BF16)
            Wv = sb(st, "Wv", [128, KC, 256], BF16)
            Wg = sb(st, "Wg", [128, KC, 256], BF16)
            Wz = sb(st, "Wz", [128, KC, 32], BF16)
            hb = [sb(st, "hb%d" % i, [128, KC, TB], BF16) for i in range(2)]
            zt = sb(st, "zt", [16, TB], BF16)
            ex = sb(st, "ex", [128, TB])
            sp = sb(st, "sp", [128, TB])
            cum = sb(st, "cum", [128, TB])
            ea = sb(st, "ea", [128, TB])
            eb = sb(st, "eb", [128, TB])
            ec = sb(st, "ec", [128, TB])
            dec = sb(st, "dec", [128, 4])
            qd = sb(st, "qd", [128, TB], BF16)
            ki = sb(st, "ki", [128, TB], BF16)
            kte = sb(st, "kte", [128, TB])
            kteT = sb(st, "kteT", [128, 4, 128], BF16)
            vt = sb(st, "vt", [128, 4, 256], BF16)
            sg = sb(st, "sg", [128, 2, TB], BF16)
            sT = [sb(st, "sT%d" % i, [128, 128], BF16) for i in range(2)]
            state = sb(st, "state", [128, 256])
            sbf = [sb(st, "sbf%d" % i, [128, 256], BF16) for i in range(2)]
            Sf = sb(st, "Sf", [128, 256])
            Sb = sb(st, "Sb", [128, 256])
            osb = sb(st, "osb", [128, 2, TB])
            obl = sb(st, "obl", [128, 2, TB])
            osq = sb(st, "osq", [128, 2, TB], BF16)
            ors = sb(st, "ors", [128, TB])
            yt = sb(st, "yt", [128, 2, TB], BF16)
            B0, B1, B2, B3, B4, B5, B6, B7 = [psb(st, "gp%d" % i) for i in range(8)]
            b4v = Buf("b4v"); b4kv = Buf("b4kv"); b5s = Buf("b5s"); b5t = Buf("b5t")

            def load_w(dst, c0, n):
                w = wstg[load_w.i % 2]
                load_w.i += 1
                P.dma("sync", lambda e: e.dma_start(out=w[:, :, 0:n], in_=w_in_v[:, :, c0:c0 + n]), [], w.b)
                P.op("vector", lambda e: e.tensor_copy(out=dst[:, :, 0:n], in_=w[:, :, 0:n]), [w.b], [dst.b])
            load_w.i = 0

            def gla_block(h, hsrc, hbuf, n, direction, emit_out, blk):
                nch = n // 128
                dcol = 0 if direction == "f" else 1
                for k in range(KC):
                    P.op("tensor", lambda e, k=k: e.matmul(B0[:, 0:n], lhsT=Wq[:, k, :], rhs=hsrc[:, k, 0:n], start=(k == 0), stop=(k == KC - 1)), [Wq.b, hbuf], [B0.b])
                for k in range(KC):
                    P.op("tensor", lambda e, k=k: e.matmul(B1[:, 0:n], lhsT=Wk[:, k, :], rhs=hsrc[:, k, 0:n], start=(k == 0), stop=(k == KC - 1)), [Wk.b, hbuf], [B1.b])
                for k in range(KC):
                    P.op("tensor", lambda e, k=k: e.matmul(B2[0:16, 0:n], lhsT=Wz[:, k, dcol * 16:(dcol + 1) * 16], rhs=hsrc[:, k, 0:n], start=(k == 0), stop=(k == KC - 1)), [Wz.b, hbuf], [B2.b])
                P.op("vector", lambda e: e.tensor_copy(out=zt[:, 0:n], in_=B2[0:16, 0:n]), [B2.b], [zt.b])
                P.op("tensor", lambda e: e.matmul(B3[:, 0:n], lhsT=wa2b[:, dcol * 512 + h * 128:dcol * 512 + (h + 1) * 128], rhs=zt[:, 0:n], start=True, stop=True), [wa2b.b, zt.b], [B3.b])
                P.op("scalar", lambda e: e.activation(out=ex[:, 0:n], in_=B3[:, 0:n], func=AF.Exp, scale=-1.0, bias=nb2[:, dcol * 4 + h:dcol * 4 + h + 1]), [B3.b, nb2.b], [ex.b])
                P.op("scalar", lambda e: e.activation(out=sp[:, 0:n], in_=ex[:, 0:n], func=AF.Ln, bias=1.0), [ex.b], [sp.b])
                P.op("vector", lambda e: e.tensor_tensor_scan(out=cum[:, 0:n], data0=cst[:, lay["reset"][0]:lay["reset"][0] + n], data1=sp[:, 0:n], initial=0.0, op0=ALU.mult, op1=ALU.add),
                     [cst.b, sp.b], [cum.b])
                cum3 = cum[:, 0:n].rearrange("p (c j) -> p c j", j=128)
                tot = cum3[:, :, 127:128]
                P.op("scalar", lambda e: e.activation(out=dec[:, 0:nch], in_=cum3[:, :, 127], func=AF.Exp, scale=-1.0 / 16), [cum.b], [dec.b])
                if direction == "b":
                    P.op("vector", lambda e: e.tensor_tensor(out=ec[:, 0:n], in0=sp[:, 0:n], in1=cum[:, 0:n], op=ALU.subtract), [sp.b, cum.b], [ec.b])
                    ec3 = ec[:, 0:n].rearrange("p (c j) -> p c j", j=128)
                    P.op("vector", lambda e: e.tensor_tensor(out=ec3, in0=ec3, in1=tot.to_broadcast([128, nch, 128]), op=ALU.add), [ec.b, cum.b], [ec.b])
                    cdir = ec
                else:
                    cdir = cum
                P.op("scalar", lambda e: e.activation(out=ea[:, 0:n], in_=cdir[:, 0:n], func=AF.Exp, scale=-1.0 / 16), [cdir.b], [ea.b])
                P.op("scalar", lambda e: e.activation(out=eb[:, 0:n], in_=cdir[:, 0:n], func=AF.Exp, scale=1.0 / 16), [cdir.b], [eb.b])
                P.op("vector", lambda e: e.scalar_tensor_tensor(out=qd[:, 0:n], in0=B0[:, 0:n], scalar=128.0 ** -0.5, in1=ea[:, 0:n], op0=ALU.mult, op1=ALU.mult), [B0.b, ea.b], [qd.b])
                P.op("vector", lambda e: e.tensor_tensor(out=ki[:, 0:n], in0=B1[:, 0:n], in1=eb[:, 0:n], op=ALU.mult), [B1.b, eb.b], [ki.b])
                ea3 = ea[:, 0:n].rearrange("p (c j) -> p c j", j=128)
                cd3 = cdir[:, 0:n].rearrange("p (c j) -> p c j", j=128)
                P.op("vector", lambda e: e.tensor_tensor(out=ea3, in0=cd3, in1=tot.to_broadcast([128, nch, 128]), op=ALU.subtract), [cdir.b, cum.b, qd.b], [ea.b])
                P.op("scalar", lambda e: e.activation(out=ea[:, 0:n], in_=ea[:, 0:n], func=AF.Exp, scale=1.0 / 16), [ea.b], [ea.b])
                P.op("vector", lambda e: e.tensor_tensor(out=kte[:, 0:n], in0=B1[:, 0:n], in1=ea[:, 0:n], op=ALU.mult), [B1.b, ea.b], [kte.b])
                for c in range(nch):
                    for k in range(KC):
                        P.op("tensor", lambda e, k=k, c=c: e.matmul(B4[:, 0:256], lhsT=hsrc[:, k, c * 128:(c + 1) * 128], rhs=Wv[:, k, :], start=(k == 0), stop=(k == KC - 1)), [Wv.b, hbuf], [b4v])
                    P.op("scalar", lambda e, c=c: e.activation(out=vt[:, c, :], in_=B4[:, 0:256], func=AF.Copy), [b4v], [vt.b])
                    P.op("tensor", lambda e, c=c: e.transpose(B5[:, 128:256], kte[:, c * 128:(c + 1) * 128], cst[:, cs("ident")]), [kte.b, cst.b], [b5t])
                    P.op("vector", lambda e, c=c: e.tensor_copy(out=kteT[:, c, :], in_=B5[:, 128:256]), [b5t], [kteT.b])
                if emit_out and direction == "f":
                    for vb in range(2):
                        Bg = (B0, B1)[vb]
                        for k in range(KC):
                            P.op("tensor", lambda e, k=k, vb=vb, Bg=Bg: e.matmul(Bg[:, 0:n], lhsT=Wg[:, k, vb * 128:(vb + 1) * 128], rhs=hsrc[:, k, 0:n], start=(k == 0), stop=(k == KC - 1)), [Wg.b, hbuf], [Bg.b])
                        P.op("scalar", lambda e, vb=vb, Bg=Bg: e.activation(out=sg[:, vb, 0:n], in_=Bg[:, 0:n], func=AF.Silu), [Bg.b], [sg.b])
                order = range(nch) if direction == "f" else range(nch - 1, -1, -1)
                mask = cst[:, cs("maskf")] if direction == "f" else cst[:, cs("maskb")]
                for c in order:
                    csl = slice(c * 128, (c + 1) * 128)
                    if emit_out:
                        s_ = sT[gla_block.si % 2]
                        P.op("tensor", lambda e, csl=csl: e.matmul(B5[:, 0:128], lhsT=ki[:, csl], rhs=qd[:, csl], start=True, stop=True), [ki.b, qd.b], [b5s])
                        P.op("vector", lambda e, s_=s_: e.tensor_tensor(out=s_[:], in0=B5[:, 0:128], in1=mask, op=ALU.mult), [b5s, cst.b], [s_.b])
                        gla_block.si += 1
                        for vb in range(2):
                            Bo = (B6, B7)[vb]
                            P.op("tensor", lambda e, vb=vb, c=c, csl=csl, s_=s_, Bo=Bo: e.matmul(Bo[:, csl], lhsT=vt[:, c, vb * 128:(vb + 1) * 128], rhs=s_[:], start=True, stop=False), [vt.b, s_.b], [Bo.b])
                            P.op("tensor", lambda e, vb=vb, csl=csl, Bo=Bo, cur=gla_block.cur: e.matmul(Bo[:, csl], lhsT=cur[:, vb * 128:(vb + 1) * 128], rhs=qd[:, csl], start=False, stop=True), [gla_block.cur.b, qd.b], [Bo.b])
                    P.op("tensor", lambda e, c=c: e.matmul(B4[:, 256:512], lhsT=kteT[:, c, :], rhs=vt[:, c, :], start=True, stop=True), [kteT.b, vt.b], [b4kv])
                    P.op("vector", lambda e, c=c: e.scalar_tensor_tensor(out=state[:], in0=state[:], scalar=dec[:, c:c + 1], in1=B4[:, 256:512], op0=ALU.mult, op1=ALU.add), [state.b, dec.b, b4kv], [state.b])
                    nxt = sbf[(gla_block.ci + 1) % 2]
                    gla_block.ci += 1
                    P.op("scalar", lambda e, nxt=nxt: e.activation(out=nxt[:], in_=state[:], func=AF.Copy), [state.b], [nxt.b])
                    gla_block.cur = nxt
                if emit_out:
                    if direction == "b":
                        P.op("vector", lambda e: e.tensor_copy(out=osb[:, 0, :], in_=B6[:, :]), [B6.b], [osb.b])
                        P.op("vector", lambda e: e.tensor_copy(out=osb[:, 1, :], in_=B7[:, :]), [B7.b], [osb.b])
                        P.dma("gpsimd", lambda e: e.dma_start(out=OB[blk * 128:(blk + 1) * 128, :], in_=osb[:].rearrange("p a t -> p (a t)")), [osb.b], OB.b)
                    else:
                        P.dma("sync", lambda e: e.dma_start(out=obl[:].rearrange("p a t -> p (a t)"), in_=OB[blk * 128:(blk + 1) * 128, :]), [OB.b], obl.b)
                        P.op("vector", lambda e: e.tensor_tensor(out=osb[:, 0, :], in0=B6[:, :], in1=obl[:, 0, :], op=ALU.add), [B6.b, obl.b], [osb.b])
                        P.op("vector", lambda e: e.tensor_tensor(out=osb[:, 1, :], in0=B7[:, :], in1=obl[:, 1, :], op=ALU.add), [B7.b, obl.b], [osb.b])
                        P.op("gpsimd", lambda e: e.tensor_tensor(out=osq[:], in0=osb[:], in1=osb[:], op=ALU.mult), [osb.b], [osq.b])
                        rstd_from_sq(B2, osq, 2, TB, ors, 1.0 / 256)
                        P.op("vector", lambda e: e.tensor_tensor(out=osb[:], in0=osb[:], in1=ors[:].unsqueeze(1).to_broadcast([128, 2, TB]), op=ALU.mult), [osb.b, ors.b], [osb.b])
                        for vb in range(2):
                            P.op("vector", lambda e, vb=vb: e.scalar_tensor_tensor(out=yt[:, vb, :], in0=osb[:, vb, :], scalar=glg[:, 2 * h + vb:2 * h + vb + 1], in1=sg[:, vb, :], op0=ALU.mult, op1=ALU.mult),
                                 [osb.b, glg.b, sg.b], [yt.b])
                        P.dma("gpsimd", lambda e: e.dma_start(out=YT[blk * 128:(blk + 1) * 128, 2 * h * TB:(2 * h + 2) * TB], in_=yt[:].rearrange("p a t -> p (a t)")), [yt.b], YT.b)
            gla_block.si = 0
            gla_block.ci = 0
            gla_block.cur = sbf[0]

            def set_state(src):
                if src is None:
                    P.op("vector", lambda e: e.memset(state[:], 0.0), [], [state.b])
                else:
                    P.op("vector", lambda e: e.tensor_copy(out=state[:], in_=src[:]), [src.b], [state.b])
                nxt = sbf[(gla_block.ci + 1) % 2]
                gla_block.ci += 1
                P.op("scalar", lambda e: e.activation(out=nxt[:], in_=state[:], func=AF.Copy), [state.b], [nxt.b])
                gla_block.cur = nxt

            for h in range(4):
                load_w(Wq, h * 128, 128)
                load_w(Wk, 512 + h * 128, 128)
                load_w(Wv, 1024 + h * 256, 256)
                load_w(Wg, 2048 + h * 256, 256)
                load_w(Wz, 3072, 32)
                set_state(None)
                gla_block(h, hc, hc.b, CTX, "f", False, 0)
                P.op("vector", lambda e: e.tensor_copy(out=Sf[:], in_=state[:]), [state.b], [Sf.b])
                set_state(None)
                gla_block(h, hc, hc.b, CTX, "b", False, 0)
                P.op("vector", lambda e: e.tensor_copy(out=Sb[:], in_=state[:]), [state.b], [Sb.b])
                set_state(Sb)
                for i, blk in enumerate(range(NB - 1, -1, -1)):
                    hbt = hb[i % 2]
                    P.dma("sync", lambda e, hbt=hbt, blk=blk: e.dma_start(out=hbt[:].rearrange("p k t -> p (k t)"), in_=HT[blk * 128:(blk + 1) * 128, :]), [HT.b], hbt.b)
                    gla_block(h, hbt, hbt.b, TB, "b", True, blk)
                set_state(Sf)
                for i, blk in enumerate(range(NB)):
                    hbt = hb[i % 2]
                    P.dma("sync", lambda e, hbt=hbt, blk=blk: e.dma_start(out=hbt[:].rearrange("p k t -> p (k t)"), in_=HT[blk * 128:(blk + 1) * 128, :]), [HT.b], hbt.b)
                    gla_block(h, hbt, hbt.b, TB, "f", True, blk)
    _phase3()
    P.barrier()

    if cfg.stop == "A1":
        P.build(final_waits=[HT.b, ADA.b, YT.b] + dumps)
        return nc, P

    NK2 = TB // T1
    def _phase4():
        with (contextlib.nullcontext(top) if cfg.noscope else contextlib.ExitStack()) as st:
            wstg = sb(st, "fwst", [128, KC, 128])
            Wu = sb(st, "Wu", [128, KC, 128], BF16)
            hb = [sb(st, "fhb%d" % i, [128, KC, TB], BF16) for i in range(2)]
            ut = sb(st, "ut", [128, TB], BF16)
            at = [sb(st, "at%d" % i, [128, TB], BF16) for i in range(2)]
            bt_ = [sb(st, "btt%d" % i, [128, TB], BF16) for i in range(2)]
            DA = sb(st, "DA", [T1, 128, 128], BF16)
            DB = sb(st, "DB", [T1, 128, 128], BF16)
            Yall = sb(st, "Yall", [T1, 128, 128], BF16)
            xs1 = [sb(st, "xs1%d" % i, [128, 2, 2, T1]) for i in range(2)]
            tA = [sb(st, "tA%d" % i, [128, 2, T1]) for i in range(2)]
            tB = [sb(st, "tB%d" % i, [128, 2, T1]) for i in range(2)]
            br = [sb(st, "br%d" % i, [128, 2, T1], BF16) for i in range(2)]
            bi = [sb(st, "bi%d" % i, [128, 2, T1], BF16) for i in range(2)]
            yo = [sb(st, "yo%d" % i, [128, TB], BF16) for i in range(2)]
            pu = psb(st, "pu")
            pa = psb(st, "pa")
            pbk = psb(st, "pbk")
            p1 = [psb(st, "p1%d" % i) for i in range(2)]
            p2 = [psb(st, "p2%d" % i) for i in range(2)]
            ptr = T(st.enter_context(nc.psum_tensor("ps_ptr", [128, 1024], BF16)), "ptr")
            twc3 = cst[:, cs("twc")].unsqueeze(1).to_broadcast([128, 2, T1])
            tws3 = cst[:, cs("tws")].unsqueeze(1).to_broadcast([128, 2, T1])
            for gi in range(8):
                P.dma("sync", lambda e, gi=gi: e.dma_start(out=wstg[:], in_=w_in_v[:, :, 3104 + gi * 128:3104 + (gi + 1) * 128]), [], wstg.b)
                P.op("vector", lambda e: e.tensor_copy(out=Wu[:], in_=wstg[:]), [wstg.b], [Wu.b])
                for blk in range(NB):
                    hbt = hb[blk % 2]
                    P.dma("sync", lambda e, hbt=hbt, blk=blk: e.dma_start(out=hbt[:].rearrange("p k t -> p (k t)"), in_=HT[blk * 128:(blk + 1) * 128, :]), [HT.b], hbt.b)
                    for k in range(KC):
                        P.op("tensor", lambda e, k=k, hbt=hbt: e.matmul(pu[:, :], lhsT=Wu[:, k, :], rhs=hbt[:, k, :], start=(k == 0), stop=(k == KC - 1)), [Wu.b, hbt.b], [pu.b])
                    P.op("scalar", lambda e: e.activation(out=ut[:], in_=pu[:, :], func=AF.Copy), [pu.b], [ut.b])
                    a_, b_ = at[blk % 2], bt_[blk % 2]
                    P.op("tensor", lambda e: e.matmul(pa[:, :], lhsT=cbf[:, CB["c128"]], rhs=ut[:], start=True, stop=True), [cbf.b, ut.b], [pa.b])
                    P.op("tensor", lambda e: e.matmul(pbk[:, :], lhsT=cbf[:, CB["s128"]], rhs=ut[:], start=True, stop=True), [cbf.b, ut.b], [pbk.b])
                    P.op("vector", lambda e, a_=a_: e.tensor_copy(out=a_[:], in_=pa[:, :]), [pa.b], [a_.b])
                    P.op("scalar", lambda e, b_=b_: e.activation(out=b_[:], in_=pbk[:, :], func=AF.Copy), [pbk.b], [b_.b])
                    P.dma("gpsimd", lambda e, a_=a_, blk=blk: e.dma_start(out=FA[:, blk * TB:(blk + 1) * TB], in_=a_[:]), [a_.b], FA.b)
                    P.dma("gpsimd", lambda e, b_=b_, blk=blk: e.dma_start(out=FB[:, blk * TB:(blk + 1) * TB], in_=b_[:]), [b_.b], FB.b)
                for m0 in range(0, 128, 16):
                    P.dma("sync", lambda e, m0=m0: e.dma_start(out=DA[:, m0:m0 + 16, :], in_=FA[m0:m0 + 16, :].rearrange("m (a b) -> a m b", b=128)), [FA.b], DA.b)
                    P.dma("sync", lambda e, m0=m0: e.dma_start(out=DB[:, m0:m0 + 16, :], in_=FB[m0:m0 + 16, :].rearrange("m (a b) -> a m b", b=128)), [FB.b], DB.b)
                for mp in range(64):
                    q = mp % 2
                    ps1, ps2 = p1[q], p2[q]
                    for j in range(2):
                        m = mp * 2 + j
                        P.op("tensor", lambda e, m=m, j=j, ps1=ps1: e.matmul(ps1[:, j * 2 * T1:(j + 1) * 2 * T1], lhsT=DA[:, m, :], rhs=cbf[0:T1, CB["f1a"]], start=True, stop=False), [DA.b, cbf.b], [ps1.b])
                        P.op("tensor", lambda e, m=m, j=j, ps1=ps1: e.matmul(ps1[:, j * 2 * T1:(j + 1) * 2 * T1], lhsT=DB[:, m, :], rhs=cbf[0:T1, CB["f1b"]], start=False, stop=True), [DB.b, cbf.b], [ps1.b])
                    x1_, ta, tb, br_, bi_ = xs1[q], tA[q], tB[q], br[q], bi[q]
                    P.op("scalar", lambda e, x1_=x1_, ps1=ps1: e.activation(out=x1_[:].rearrange("p a b c -> p (a b c)"), in_=ps1[:, 0:4 * T1], func=AF.Copy), [ps1.b], [x1_.b])
                    re_, im_ = x1_[:, :, 0, :], x1_[:, :, 1, :]
                    P.op("vector", lambda e, ta=ta, re_=re_: e.tensor_tensor(out=ta[:], in0=re_, in1=twc3, op=ALU.mult), [x1_.b, cst.b], [ta.b])
                    P.op("gpsimd", lambda e, tb=tb, im_=im_: e.tensor_tensor(out=tb[:], in0=im_, in1=tws3, op=ALU.mult), [x1_.b, cst.b], [tb.b])
                    P.op("vector", lambda e, ta=ta, tb=tb, br_=br_: e.tensor_tensor(out=br_[:], in0=ta[:], in1=tb[:], op=ALU.add), [ta.b, tb.b], [br_.b])
                    P.op("gpsimd", lambda e, tb=tb, im_=im_: e.tensor_tensor(out=tb[:], in0=im_, in1=twc3, op=ALU.mult), [x1_.b, cst.b, br_.b], [tb.b])
                    P.op("vector", lambda e, ta=ta, re_=re_: e.tensor_tensor(out=ta[:], in0=re_, in1=tws3, op=ALU.mult), [x1_.b, cst.b, br_.b], [ta.b])
                    P.op("gpsimd", lambda e, ta=ta, tb=tb, bi_=bi_: e.tensor_tensor(out=bi_[:], in0=tb[:], in1=ta[:], op=ALU.subtract), [ta.b, tb.b], [bi_.b])
                    for j in range(2):
                        m = mp * 2 + j
                        P.op("tensor", lambda e, j=j, ps2=ps2, br_=br_: e.matmul(ps2[0:T1, j * 128:(j + 1) * 128], lhsT=br_[:, j, :], rhs=cbf[:, CB["c128"]], start=True, stop=False), [br_.b, cbf.b], [ps2.b])
                        P.op("tensor", lambda e, j=j, ps2=ps2, bi_=bi_: e.matmul(ps2[0:T1, j * 128:(j + 1) * 128], lhsT=bi_[:, j, :], rhs=cbf[:, CB["s128"]], start=False, stop=True), [bi_.b, cbf.b], [ps2.b])
                    P.op("scalar", lambda e, mp=mp, ps2=ps2: e.activation(out=Yall[:, :, mp * 2:mp * 2 + 2].rearrange("p k m -> p m k"), in_=ps2[0:T1, 0:256].rearrange("p (m k) -> p m k", k=128), func=AF.Copy),
                         [ps2.b], [Yall.b])
                for blk in range(NB):
                    yb = yo[blk % 2]
                    for jj in range(NK2):
                        k2 = blk * NK2 + jj
                        P.op("tensor", lambda e, k2=k2, jj=jj: e.transpose(ptr[:, jj * T1:(jj + 1) * T1], Yall[:, k2, :], cbf[0:T1, 384:384 + T1]), [Yall.b, cbf.b], [ptr.b])
                    P.op("vector", lambda e, yb=yb: e.tensor_copy(out=yb[:], in_=ptr[:, 0:TB]), [ptr.b], [yb.b])
                    P.dma("gpsimd", lambda e, yb=yb, blk=blk, gi=gi: e.dma_start(out=YT[blk * 128:(blk + 1) * 128, (8 + gi) * TB:(9 + gi) * TB], in_=yb[:]), [yb.b], YT.b)
    _phase4()
    P.barrier()

    if cfg.stop == "A2":
        P.build(final_waits=[HT.b, ADA.b, YT.b] + dumps)
        return nc, P

    NT = L // 128
    affall = sb(top, "affall", [128, NT, E])
    thr = sb(top, "thr", [128, E])
    def _phase5():
        with (contextlib.nullcontext(top) if cfg.noscope else contextlib.ExitStack()) as st:
            wst = sb(st, "bwst", [128, 1, 2048])
            Wo = sb(st, "Wo", [128, KC, D], BF16)
            wrf = sb(st, "wrf", [128, KC, E])
            wrb = sb(st, "wrb", [128, KC, E], BF16)
            yb = [sb(st, "byb0", [128, KC, TB], BF16)] * 2
            xb = [sb(st, "bxb0", [128, KC, TB])] * 2
            sq = sb(st, "bsq", [128, KC, TB], BF16)
            rs = sb(st, "brs", [128, TB])
            hf = [sb(st, "bhf0", [128, KC, TB], BF16)] * 2
            mx = sb(st, "bmx", [128, 4])
            sm = sb(st, "bsm", [128, 4])
            ee = sb(st, "bee", [128, 4, E])
            po = [psb(st, "po%d" % i) for i in range(2)]
            pq = psb(st, "pq")
            pl = psb(st, "pl")
            for kq in range(KC):
                P.dma("sync", lambda e, kq=kq: e.dma_start(out=wst[:], in_=w_out_v[:, kq:kq + 1, :]), [], wst.b)
                P.op("vector", lambda e, kq=kq: e.tensor_copy(out=Wo[:, kq:kq + 1, :], in_=wst[:]), [wst.b], [Wo.b])
            P.dma("sync", lambda e: e.dma_start(out=wrf[:], in_=wr_v), [], wrf.b)
            P.op("vector", lambda e: e.tensor_copy(out=wrb[:], in_=wrf[:]), [wrf.b], [wrb.b])
            for blk in range(NB):
                y_, x_, h_ = yb[0], xb[0], hf[0]
                x1 = x_
                P.dma("sync", lambda e, y_=y_, blk=blk: e.dma_start(out=y_[:].rearrange("p k t -> p (k t)"), in_=YT[blk * 128:(blk + 1) * 128, :]), [YT.b], y_.b)
                P.dma("sync", lambda e, x_=x_, blk=blk: e.dma_start(out=x_[:], in_=xT_v[:, :, blk * TB:(blk + 1) * TB]), [], x_.b)
                for nb in range(KC):
                    pp = po[nb % 2]
                    for k in range(KC):
                        P.op("tensor", lambda e, pp=pp, k=k, nb=nb, y_=y_: e.matmul(pp[:, :], lhsT=Wo[:, k, nb * 128:(nb + 1) * 128], rhs=y_[:, k, :], start=(k == 0), stop=(k == KC - 1)), [Wo.b, y_.b], [pp.b])
                    P.op("vector", lambda e, pp=pp, nb=nb, x_=x_: e.scalar_tensor_tensor(out=x1[:, nb, :], in0=pp[:, :], scalar=mods[:, 32 + nb:33 + nb], in1=x_[:, nb, :], op0=ALU.mult, op1=ALU.add),
                         [pp.b, mods.b, x_.b], [x1.b])
                P.dma("gpsimd", lambda e, blk=blk: e.dma_start(out=X1[blk * 128:(blk + 1) * 128, :], in_=x1[:].rearrange("p k t -> p (k t)")), [x1.b], X1.b)
                P.op("gpsimd", lambda e: e.tensor_tensor(out=sq[:], in0=x1[:], in1=x1[:], op=ALU.mult), [x1.b], [sq.b])
                rstd_from_sq(pq, sq, KC, TB, rs, 1.0 / D)
                P.op("vector", lambda e: e.tensor_tensor(out=x1[:], in0=x1[:], in1=rs[:].unsqueeze(1).to_broadcast([128, KC, TB]), op=ALU.mult), [x1.b, rs.b], [x1.b])
                P.op("gpsimd", lambda e: e.tensor_tensor(out=x1[:], in0=x1[:], in1=G2[:, 0:16].unsqueeze(2).to_broadcast([128, KC, TB]), op=ALU.mult), [x1.b, G2.b], [x1.b])
                P.op("vector", lambda e, h_=h_: e.tensor_tensor(out=h_[:], in0=x1[:], in1=mods[:, 48:64].unsqueeze(2).to_broadcast([128, KC, TB]), op=ALU.add), [x1.b, mods.b], [h_.b])
                P.dma("gpsimd", lambda e, h_=h_, blk=blk: e.dma_start(out=HF[blk * 128:(blk + 1) * 128, :], in_=h_[:].rearrange("p k t -> p (k t)")), [h_.b], HF.b)
                for s in range(4):
                    for k in range(KC):
                        P.op("tensor", lambda e, s=s, k=k, h_=h_: e.matmul(pl[:, s * E:(s + 1) * E], lhsT=h_[:, k, s * 128:(s + 1) * 128], rhs=wrb[:, k, :], start=(k == 0), stop=(k == KC - 1)), [h_.b, wrb.b], [pl.b])
                pl3 = pl[:, 0:4 * E].rearrange("p (s e) -> p s e", e=E)
                P.op("vector", lambda e: e.tensor_reduce(out=mx[:], in_=pl3, axis=AX.X, op=ALU.max), [pl.b], [mx.b])
                P.op("vector", lambda e: e.tensor_tensor(out=ee[:], in0=pl3, in1=mx[:].unsqueeze(2).to_broadcast([128, 4, E]), op=ALU.subtract), [pl.b, mx.b], [ee.b])
                P.op("scalar", lambda e: e.activation(out=ee[:], in_=ee[:], func=AF.Exp), [ee.b], [ee.b])
                P.op("vector", lambda e: e.tensor_reduce(out=sm[:], in_=ee[:], axis=AX.X, op=ALU.add), [ee.b], [sm.b])
                P.op("vector", lambda e: e.reciprocal(out=sm[:], in_=sm[:]), [sm.b], [sm.b])
                P.op("vector", lambda e, blk=blk: e.tensor_tensor(out=affall[:, blk * 4:(blk + 1) * 4, :], in0=ee[:], in1=sm[:].unsqueeze(2).to_broadcast([128, 4, E]), op=ALU.mult), [ee.b, sm.b], [affall.b])
                P.dma("gpsimd", lambda e, blk=blk: e.dma_start(out=AFS[blk * 128:(blk + 1) * 128, :], in_=affall[:, blk * 4:(blk + 1) * 4, :].rearrange("p s e -> p (s e)")), [affall.b], AFS.b)
    _phase5()
    P.barrier()

    if cfg.stop == "B":
        P.build(final_waits=[HT.b, ADA.b, YT.b, X1.b, AFS.b] + dumps)
        return nc, P

    def _phase6():
        with (contextlib.nullcontext(top) if cfg.noscope else contextlib.ExitStack()) as st:
            mid = sb(st, "mid", [128, E])
            cmpt = sb(st, "cmpt", [128, E, NT])
            pc = sb(st, "pc", [128, E])
            ge = sb(st, "ge", [128, E])
            pc_ps = psb(st, "pcps")
            aff_v = affall[:].rearrange("p t e -> p e t")
            P.op("vector", lambda e: e.memset(thr[:], 0.0), [], [thr.b])
            for it in range(30):
                s_i = 2.0 ** -(it + 1)
                P.op("vector", lambda e, s_i=s_i: e.tensor_scalar(out=mid[:], in0=thr[:], scalar1=s_i, scalar2=None, op0=ALU.add), [thr.b], [mid.b])
                P.op("vector", lambda e: e.tensor_tensor(out=cmpt[:], in0=aff_v, in1=mid[:].unsqueeze(2).to_broadcast([128, E, NT]), op=ALU.is_gt), [affall.b, mid.b], [cmpt.b])
                P.op("vector", lambda e: e.tensor_reduce(out=pc[:], in_=cmpt[:], axis=AX.X, op=ALU.add), [cmpt.b], [pc.b])
                P.op("tensor", lambda e: e.matmul(pc_ps[:, 0:E], lhsT=cst[:, cs("ones")], rhs=pc[:], start=True, stop=True), [cst.b, pc.b], [pc_ps.b])
                P.op("vector", lambda e, s_i=s_i: e.tensor_scalar(out=ge[:], in0=pc_ps[:, 0:E], scalar1=float(cfg.CAP) - 0.5, scalar2=s_i, op0=ALU.is_gt, op1=ALU.mult), [pc_ps.b], [ge.b])
                P.op("vector", lambda e: e.tensor_tensor(out=thr[:], in0=thr[:], in1=ge[:], op=ALU.add), [thr.b, ge.b], [thr.b])
    _phase6()
    P.barrier()

    if cfg.stop == "C":
        P.build(final_waits=[HT.b, ADA.b, YT.b, X1.b, AFS.b] + dumps)
        return nc, P

    S = TG // 128
    def _phase7():
        with (contextlib.nullcontext(top) if cfg.noscope else contextlib.ExitStack()) as st:
            hfT = sb(st, "hfT", [128, KC, TG], BF16)
            acc = sb(st, "acc", [128, KC, TG])
            afo = sb(st, "afo", [128, S, E])
            msk = sb(st, "msk", [128, S, E])
            wgt = sb(st, "wgt", [128, S, E])
            wgT = sb(st, "wgT", [E, TG])
            wb = [sb(st, "wb%d" % i, [128, TG], BF16) for i in range(2)]
            gst = [sb(st, "gst%d" % i, [128, KC, 128]) for i in range(2)]
            ust = [sb(st, "ust%d" % i, [128, KC, 128]) for i in range(2)]
            gbf = [sb(st, "gbf%d" % i, [128, KC, 128], BF16) for i in range(2)]
            ubf = [sb(st, "ubf%d" % i, [128, KC, 128], BF16) for i in range(2)]
            dst_ = [sb(st, "dst%d" % i, [128, FC, 128]) for i in range(2)]
            dbf = [sb(st, "dbf%d" % i, [128, FC, 128], BF16) for i in range(2)]
            sgm = [sb(st, "sgm%d" % i, [128, TG], BF16) for i in range(2)]
            tmu = [sb(st, "tmu%d" % i, [128, TG], BF16) for i in range(2)]
            hid = sb(st, "hid", [128, FC, TG], BF16)
            sq = sb(st, "dsq", [128, KC, TG], BF16)
            rs = sb(st, "drs", [128, TG])
            pg = [psb(st, "pg%d" % i) for i in range(2)]
            pu_ = [psb(st, "pu%d" % i) for i in range(2)]
            py = [psb(st, "py%d" % i) for i in range(2)]
            pw = psb(st, "pw")
            pm = psb(st, "pm")
            X1f = X1.t.ap()
            HFf = HF.t.ap()
            AFf = AFS.t.ap()
            cnt = [0, 0]
            for g in range(NG):
                for jb in range(TG // TB):
                    col = g * (TG // TB) + jb
                    P.dma("gpsimd", lambda e, col=col, jb=jb: e.indirect_dma_start(out=hfT[:].rearrange("p k t -> p (k t)"), out_offset=None, in_=HFf,
                                                                                     in_offset=bass.IndirectOffsetOnAxis(ap=idx[:, col:col + 1], axis=0)), [HF.b, idx.b], hfT.b)
                    P.dma("gpsimd", lambda e, col=col, jb=jb: e.indirect_dma_start(out=acc[:].rearrange("p k t -> p (k t)"), out_offset=None, in_=X1f,
                                                                                     in_offset=bass.IndirectOffsetOnAxis(ap=idx[:, col:col + 1], axis=0)), [X1.b, idx.b], acc.b)
                    P.dma("gpsimd", lambda e, col=col, jb=jb: e.indirect_dma_start(out=afo[:, jb * 4:(jb + 1) * 4, :].rearrange("p s e -> p (s e)"), out_offset=None, in_=AFf,
                                                                                     in_offset=bass.IndirectOffsetOnAxis(ap=idx[:, col:col + 1], axis=0)), [AFS.b, idx.b], afo.b)
                P.op("vector", lambda e: e.tensor_tensor(out=msk[:], in0=afo[:], in1=thr[:].unsqueeze(1).to_broadcast([128, S, E]), op=ALU.is_gt), [afo.b, thr.b], [msk.b])
                P.op("vector", lambda e: e.tensor_tensor(out=wgt[:], in0=afo[:], in1=msk[:], op=ALU.mult), [afo.b, msk.b], [wgt.b])
                for s in range(S):
                    P.op("tensor", lambda e, s=s: e.matmul(pm[0:E, s * 128:(s + 1) * 128], lhsT=wgt[:, s, :], rhs=cst[:, cs("ident")], start=True, stop=True), [wgt.b, cst.b], [pm.b])
                P.op("vector", lambda e: e.tensor_copy(out=wgT[:], in_=pm[0:E, 0:TG]), [pm.b], [wgT.b])
                for ex_ in range(E):
                    wbe = wb[ex_ % 2]
                    so = lay["sel"][0] + ex_ * 128
                    P.op("tensor", lambda e, so=so: e.matmul(pw[:, 0:TG], lhsT=cst[0:E, so:so + 128], rhs=wgT[:], start=True, stop=True), [cst.b, wgT.b], [pw.b])
                    P.op("scalar", lambda e, wbe=wbe: e.activation(out=wbe[:], in_=pw[:, 0:TG], func=AF.Copy), [pw.b], [wbe.b])
                    for fb in range(FC):
                        i = cnt[0] % 2
                        cnt[0] += 1
                        gs, us, gb, ub = gst[i], ust[i], gbf[i], ubf[i]
                        P.dma("sync", lambda e, gs=gs, ex_=ex_, fb=fb: e.dma_start(out=gs[:].rearrange("p k f -> p (k f)"), in_=w_eg[(ex_ * FC + fb) * 128:(ex_ * FC + fb + 1) * 128, :]), [], gs.b)
                        P.dma("sync", lambda e, us=us, ex_=ex_, fb=fb: e.dma_start(out=us[:].rearrange("p k f -> p (k f)"), in_=w_eu[(ex_ * FC + fb) * 128:(ex_ * FC + fb + 1) * 128, :]), [], us.b)
                        P.op("gpsimd", lambda e, gs=gs, gb=gb: e.tensor_copy(out=gb[:], in_=gs[:]), [gs.b], [gb.b])
                        P.op("vector", lambda e, us=us, ub=ub: e.tensor_copy(out=ub[:], in_=us[:]), [us.b], [ub.b])
                        pgi, pui, sgi, tmi = pg[i], pu_[i], sgm[i], tmu[i]
                        for k in range(KC):
                            P.op("tensor", lambda e, k=k, gb=gb, pgi=pgi: e.matmul(pgi[:, 0:TG], lhsT=gb[:, k, :], rhs=hfT[:, k, :], start=(k == 0), stop=(k == KC - 1)), [gb.b, hfT.b], [pgi.b])
                        for k in range(KC):
                            P.op("tensor", lambda e, k=k, ub=ub, pui=pui: e.matmul(pui[:, 0:TG], lhsT=ub[:, k, :], rhs=hfT[:, k, :], start=(k == 0), stop=(k == KC - 1)), [ub.b, hfT.b], [pui.b])
                        P.op("scalar", lambda e, sgi=sgi, pgi=pgi: e.activation(out=sgi[:], in_=pgi[:, 0:TG], func=AF.Silu), [pgi.b], [sgi.b])
                        P.op("vector", lambda e, sgi=sgi, pui=pui, tmi=tmi: e.tensor_tensor(out=tmi[:], in0=pui[:, 0:TG], in1=sgi[:], op=ALU.mult), [pui.b, sgi.b], [tmi.b])
                        P.op("gpsimd", lambda e, tmi=tmi, fb=fb, wbe=wbe: e.tensor_tensor(out=hid[:, fb, :], in0=tmi[:], in1=wbe[:], op=ALU.mult), [tmi.b, wbe.b], [hid.b])
                    for db in range(KC):
                        i = cnt[1] % 2
                        cnt[1] += 1
                        ds_, dbb, pyi = dst_[i], dbf[i], py[i]
                        P.dma("sync", lambda e, ds_=ds_, ex_=ex_, db=db: e.dma_start(out=ds_[:].rearrange("p c d -> p (c d)"), in_=w_ed[(ex_ * KC + db) * 128:(ex_ * KC + db + 1) * 128, :]), [], ds_.b)
                        P.op("gpsimd", lambda e, ds_=ds_, dbb=dbb: e.tensor_copy(out=dbb[:], in_=ds_[:]), [ds_.b], [dbb.b])
                        for fc in range(FC):
                            P.op("tensor", lambda e, fc=fc, dbb=dbb, pyi=pyi: e.matmul(pyi[:, 0:TG], lhsT=dbb[:, fc, :], rhs=hid[:, fc, :], start=(fc == 0), stop=(fc == FC - 1)), [dbb.b, hid.b], [pyi.b])
                        P.op("vector", lambda e, db=db, pyi=pyi: e.scalar_tensor_tensor(out=acc[:, db, :], in0=pyi[:, 0:TG], scalar=mods[:, 80 + db:81 + db], in1=acc[:, db, :], op0=ALU.mult, op1=ALU.add),
                             [pyi.b, mods.b, acc.b], [acc.b])
                P.op("gpsimd", lambda e: e.tensor_tensor(out=sq[:], in0=acc[:], in1=acc[:], op=ALU.mult), [acc.b], [sq.b])
                rstd_from_sq(pw, sq, KC, TG, rs, 1.0 / D)
                P.op("vector", lambda e: e.tensor_tensor(out=acc[:], in0=acc[:], in1=rs[:].unsqueeze(1).to_broadcast([128, KC, TG]), op=ALU.mult), [acc.b, rs.b], [acc.b])
                P.op("gpsimd", lambda e: e.tensor_tensor(out=acc[:], in0=acc[:], in1=ncl[:, 32:48].unsqueeze(2).to_broadcast([128, KC, TG]), op=ALU.mult), [acc.b, ncl.b], [acc.b])
                P.dma("sync", lambda e, g=g: e.dma_start(out=outT.t.ap().rearrange("(k p) t -> p k t", p=128)[:, :, g * TG:(g + 1) * TG], in_=acc[:]), [acc.b], outT.b)
    _phase7()

    finals = [outT.b]
    if cfg.dev:
        finals += [YT.b, X1.b, AFS.b, ADA.b, HT.b] + dumps
    P.build(final_waits=finals)
    top.close()
    return nc, P


def host_inputs(cfg, core, x, c, ctx, c_ctx, w_ada, b_ada, norm1_g, w_in, w_a2_f, b_a2_f, w_a2_b, b_a2_b,
                gla_norm_g, w_out, norm2_g, w_router, w_e_gate, w_e_up, w_e_down, final_norm_g, shared):
    b, r = core // 4, core % 4
    f = lambda a: np.ascontiguousarray(np.asarray(a, dtype=np.float32))
    col = lambda v: f(np.asarray(v).reshape(-1, 128).T)
    m = {}
    m["xT"] = shared["xT"][b]
    m["ctxT"] = shared["ctxT"][b]
    m["cond"] = f(np.concatenate([col(c[b]), col(c_ctx)], axis=1))
    m["w_ada"] = shared["w_ada"]
    m["b_ada"] = shared["b_ada"]
    m["ncols"] = shared["ncols"]
    m["glag"] = shared["glag"]
    m["w_in"] = shared["w_in"]
    m["wa2"] = shared["wa2"]
    m["ba2"] = shared["ba2"]
    m["w_out"] = shared["w_out"]
    m["w_router"] = shared["w_router"]
    m["w_eg"] = shared["w_eg"]
    m["w_eu"] = shared["w_eu"]
    m["w_ed"] = shared["w_ed"]
    m["cst"] = shared["cst"]
    ob0 = r * cfg.NOB
    m["idx"] = np.stack([(ob0 + j) * 128 + np.arange(128) for j in range(cfg.NOB)], axis=1).astype(np.int32)
    return m


def host_shared(cfg, x, c, ctx, c_ctx, w_ada, b_ada, norm1_g, w_in, w_a2_f, b_a2_f, w_a2_b, b_a2_b,
                gla_norm_g, w_out, norm2_g, w_router, w_e_gate, w_e_up, w_e_down, final_norm_g, batches=(0, 1)):
    f = lambda a: np.ascontiguousarray(np.asarray(a, dtype=np.float32))
    col = lambda v: f(np.asarray(v).reshape(-1, 128).T)
    sh = {}
    sh["xT"] = {b: f(np.asarray(x[b]).T) for b in batches}
    sh["ctxT"] = {b: f(np.asarray(ctx[b]).T) for b in batches}
    sh["w_ada"] = f(w_ada[0])
    sh["b_ada"] = f(b_ada[0]).reshape(1, -1)
    sh["ncols"] = f(np.concatenate([col(norm1_g[0]), col(norm2_g[0]), col(final_norm_g)], axis=1))
    sh["glag"] = col(gla_norm_g[0])
    sh["w_in"] = f(w_in[0])
    sh["wa2"] = f(np.concatenate([np.asarray(w_a2_f[0]), np.asarray(w_a2_b[0])], axis=1))
    sh["ba2"] = f(np.concatenate([col(b_a2_f[0]), col(b_a2_b[0])], axis=1))
    sh["w_out"] = f(w_out[0])
    sh["w_router"] = f(w_router[0])
    E_, FC_ = cfg.E, cfg.FC
    slab = lambda w: np.ascontiguousarray(np.asarray(w, dtype=np.float32).reshape(E_, KC, 128, FC_, 128).transpose(0, 3, 2, 1, 4)).reshape(E_ * FC_ * 128, KC * 128)
    sh["w_eg"] = slab(w_e_gate[0])
    sh["w_eu"] = slab(w_e_up[0])
    sh["w_ed"] = np.ascontiguousarray(np.asarray(w_e_down[0], dtype=np.float32).reshape(E_, FC_, 128, KC, 128).transpose(0, 3, 2, 1, 4)).reshape(E_ * KC * 128, FC_ * 128)
    sh["cst"] = make_consts(cfg)
    return sh


def kernel(**inputs):
    cfg = Cfg()
    nc, _ = build(cfg)
    sh = host_shared(cfg, **inputs)
    in_maps = [host_inputs(cfg, core, shared=sh, **inputs) for core in range(8)]
    res = run_bass_kernel_spmd(nc, in_maps, core_ids=list(range(8)))
    out = np.empty((2, cfg.L, D), np.float32)
    for core in range(8):
        b, r = core // 4, core % 4
        out[b, r * cfg.OWN:(r + 1) * cfg.OWN, :] = np.asarray(res.results[core]["outT"]).T
    return out
```

```python
import contextlib
import numpy as np
import concourse.bass as bass
import concourse.mybir as mybir
from concourse.bass_utils import run_bass_kernel_spmd

F32 = mybir.dt.float32
BF16 = mybir.dt.bfloat16
I32 = mybir.dt.int32
ALU = mybir.AluOpType
AF = mybir.ActivationFunctionType
AX = mybir.AxisListType

D = 2048
KC = 16
TB = 512
EPS = 1e-6
ENGS = ("sync", "scalar", "vector", "gpsimd", "tensor")


class Buf:
    __slots__ = ("name", "last_w", "readers", "dma_sem", "dma_cnt", "dma_writers")

    def __init__(self, name):
        self.name = name
        self.last_w = None
        self.readers = {}
        self.dma_sem = None
        self.dma_cnt = 0
        self.dma_writers = False


class Prog:
    def __init__(self, nc):
        self.nc = nc
        self.ins = []
        self.last_on = {e: None for e in ENGS}
        self.dma_bufs = []
        self.pending = {e: None for e in ENGS}

    def _emit(self, eng, fn, reads, writes, dma=False):
        iid = len(self.ins)
        deps = set()
        for b in reads:
            if b.dma_writers:
                deps.add(("dma", b, b.dma_cnt))
            elif b.last_w is not None:
                deps.add(("ins", b.last_w))
        for b in writes:
            if b.dma_writers:
                deps.add(("dma", b, b.dma_cnt))
            elif b.last_w is not None:
                deps.add(("ins", b.last_w))
            for r in b.readers.values():
                deps.add(("ins", r))
        if self.pending[eng] is not None:
            deps |= self.pending[eng]
            self.pending[eng] = None
        rec = dict(eng=eng, fn=fn, deps=deps, dma=dma, dst=None, signal=False, cnt=None)
        if dma:
            d = writes[0]
            rec["dst"] = d
            d.dma_cnt += 1
            rec["cnt"] = d.dma_cnt
            d.dma_writers = True
            d.last_w = None
            d.readers = {}
            if d not in self.dma_bufs:
                self.dma_bufs.append(d)
        else:
            for b in writes:
                b.last_w = iid
                b.dma_writers = False
                b.readers = {}
            self.last_on[eng] = iid
        rkey = ("dma", id(writes[0])) if dma else eng
        for b in reads:
            if b not in writes:
                b.readers[rkey] = iid
        self.ins.append(rec)
        return iid

    def op(self, eng, fn, reads=(), writes=()):
        return self._emit(eng, fn, list(reads), list(writes))

    def dma(self, eng, fn, reads, write):
        return self._emit(eng, fn, list(reads), [write], dma=True)

    def barrier(self):
        deps = set()
        for e in ENGS:
            if self.last_on[e] is not None:
                deps.add(("ins", self.last_on[e]))
        for b in self.dma_bufs:
            deps.add(("dma", b, b.dma_cnt))
        for e in ENGS:
            self.pending[e] = set(deps) | (self.pending[e] or set())

    def build(self, final_waits=()):
        nc = self.nc
        ins = self.ins
        for rec in ins:
            for d in rec["deps"]:
                if d[0] == "ins":
                    p = ins[d[1]]
                    if p["dma"]:
                        continue
                    if p["eng"] == "tensor" and rec["eng"] == "tensor":
                        continue
                    p["signal"] = True
        cnt = {e: 0 for e in ENGS}
        for rec in ins:
            if not rec["dma"] and rec["signal"]:
                cnt[rec["eng"]] += 1
                rec["cnt"] = cnt[rec["eng"]]
        self.cnt = cnt
        with contextlib.ExitStack() as st:
            esem = {e: st.enter_context(nc.semaphore("s_" + e)) for e in ENGS}
            for i, b in enumerate(self.dma_bufs):
                b.dma_sem = st.enter_context(nc.semaphore("d%d" % i))
            block = st.enter_context(nc.Block())
            per = {e: [r for r in ins if r["eng"] == e] for e in ENGS}

            def run(engname, eng):
                known = {}
                for rec in per[engname]:
                    waits = {}
                    for d in rec["deps"]:
                        if d[0] == "dma":
                            s, v = d[1].dma_sem, 16 * d[2]
                        else:
                            p = ins[d[1]]
                            if p["dma"]:
                                s, v = p["dst"].dma_sem, 16 * p["cnt"]
                            else:
                                if p["eng"] == "tensor" and engname == "tensor":
                                    continue
                                s, v = esem[p["eng"]], p["cnt"]
                        if known.get(id(s), 0) >= v:
                            continue
                        if waits.get(id(s), (None, 0))[1] < v:
                            waits[id(s)] = (s, v)
                    for s, v in waits.values():
                        eng.wait_ge(s, v)
                        known[id(s)] = v
                    r = rec["fn"](eng)
                    if rec["dma"]:
                        r.then_inc(rec["dst"].dma_sem, 16)
                    elif rec["signal"]:
                        r.then_inc(esem[engname], 1)
                if engname == "sync":
                    for b in final_waits:
                        eng.wait_ge(b.dma_sem, 16 * b.dma_cnt)

            @block.sync
            def _(e):
                run("sync", e)

            @block.scalar
            def _(e):
                run("scalar", e)

            @block.vector
            def _(e):
                run("vector", e)

            @block.gpsimd
            def _(e):
                run("gpsimd", e)

            @block.tensor
            def _(e):
                run("tensor", e)


class T:
    def __init__(self, t, name):
        self.t = t
        self.b = Buf(name)

    def __getitem__(self, k):
        return self.t[k]


class Cfg:
    def __init__(self, L=16384, CTX=256, E=16, DE=1024, dev=False):
        self.L, self.CTX, self.E, self.DE, self.dev = L, CTX, E, DE, dev
        self.stop = None
        self.noscope = False
        self.T1 = L // 128
        self.NB = L // TB
        self.OWN = L // 4
        self.NOB = self.OWN // TB
        self.TG = TB
        self.NG = self.OWN // self.TG
        self.FC = DE // 128
        self.CAP = 2 * L // E
        self.PW = 4128


def const_layout(cfg):
    lay = {}
    off = 0
    for name, w in (("ident", 128), ("ones", 128), ("maskf", 128), ("maskb", 128), ("c128", 128),
                    ("s128", 128), ("reset", TB), ("twc", cfg.T1), ("tws", cfg.T1),
                    ("f1a", 2 * cfg.T1), ("f1b", 2 * cfg.T1), ("sel", cfg.E * 128)):
        lay[name] = (off, w)
        off += w
    return lay, off


def make_consts(cfg):
    lay, ncol = const_layout(cfg)
    c = np.zeros((128, ncol), np.float64)

    def put(name, arr):
        o, w = lay[name]
        c[:arr.shape[0], o:o + w] = arr

    a = np.arange(128)
    put("ident", np.eye(128))
    put("ones", np.ones((128, 128)))
    put("maskf", (a[:, None] <= a[None, :]).astype(np.float64))
    put("maskb", (a[:, None] >= a[None, :]).astype(np.float64))
    ang = 2 * np.pi * np.outer(a, a) / 128.0
    put("c128", np.cos(ang))
    put("s128", np.sin(ang))
    r = np.ones((128, TB))
    r[:, ::128] = 0.0
    put("reset", r)
    T1, L = cfg.T1, cfg.L
    k1 = np.arange(T1)
    nrm = 1.0 / np.sqrt(L * 128.0)
    angt = 2 * np.pi * np.outer(a, k1) / L
    put("twc", np.cos(angt) * nrm)
    put("tws", np.sin(angt) * nrm)
    ang1 = 2 * np.pi * np.outer(k1, k1) / T1
    put("f1a", np.concatenate([np.cos(ang1), -np.sin(ang1)], axis=1))
    put("f1b", np.concatenate([-np.sin(ang1), -np.cos(ang1)], axis=1))
    sel = np.zeros((cfg.E, cfg.E * 128))
    for e in range(cfg.E):
        sel[e, e * 128:(e + 1) * 128] = 1.0
    put("sel", sel)
    return c.astype(np.float32)


def build(cfg):
    L, CTX, E, DE, T1, NB, OWN, NOB = cfg.L, cfg.CTX, cfg.E, cfg.DE, cfg.T1, cfg.NB, cfg.OWN, cfg.NOB
    TG, NG, FC, PW = cfg.TG, cfg.NG, cfg.FC, cfg.PW
    lay, ncol = const_layout(cfg)
    nc = bass.Bass("TRN2", target_bir_lowering=False)
    P = Prog(nc)

    def din(name, shape, dt=F32):
        return nc.dram_tensor(name, list(shape), dt, kind="ExternalInput")

    dbg = "ExternalOutput" if cfg.dev else None

    def dscr(name, shape, dt, out=False):
        if out and dbg:
            return T(nc.dram_tensor(name, list(shape), dt, kind=dbg), name)
        return T(nc.dram_tensor(name, list(shape), dt), name)

    dumps = []

    def dump(name, tl, shape, dt=F32):
        if not cfg.dev:
            return
        dd = T(nc.dram_tensor("dbg_" + name, list(shape), dt, kind="ExternalOutput"), "dbg_" + name)
        P.dma("gpsimd", lambda e: e.dma_start(out=dd.t.ap(), in_=tl[:]), [tl.b], dd.b)
        dumps.append(dd.b)

    xT = din("xT", [D, L])
    ctxT = din("ctxT", [D, CTX])
    cond = din("cond", [128, 32])
    w_ada = din("w_ada", [D, 6 * D])
    b_ada = din("b_ada", [1, 6 * D])
    ncols = din("ncols", [128, 48])
    glag = din("glag", [128, 8])
    w_in = din("w_in", [D, PW])
    wa2 = din("wa2", [16, 1024])
    ba2 = din("ba2", [128, 8])
    w_out = din("w_out", [D, D])
    w_router = din("w_router", [D, E])
    w_eg = din("w_eg", [E * FC * 128, KC * 128])
    w_eu = din("w_eu", [E * FC * 128, KC * 128])
    w_ed = din("w_ed", [E * KC * 128, FC * 128])
    cst_d = din("cst", [128, ncol])
    idx_d = din("idx", [128, NOB], I32)
    outT = T(nc.dram_tensor("outT", [D, OWN], F32, kind="ExternalOutput"), "outT")

    ADA = dscr("ADA", [2, 6 * D], F32, out=True)
    HT = dscr("HT", [NB * 128, KC * TB], BF16, out=True)
    OB = dscr("OB", [NB * 128, 2 * TB], F32)
    QS = dscr("QS", [NB * 128, TB], BF16)
    KS = dscr("KS", [NB * 128, TB], BF16)
    VS = dscr("VS", [NB * 128, 4 * 256], BF16)
    YT = dscr("YT", [NB * 128, KC * TB], BF16, out=True)
    FA = dscr("FA", [128, L], BF16)
    FB = dscr("FB", [128, L], BF16)
    X1 = dscr("X1", [NB * 128, KC * TB], F32, out=True)
    HF = dscr("HF", [NB * 128, KC * TB], BF16, out=True)
    AFS = dscr("AFS", [NB * 128, 4 * E], F32, out=True)

    xT_v = xT.ap().rearrange("(k p) t -> p k t", p=128)
    ctxT_v = ctxT.ap().rearrange("(k p) t -> p k t", p=128)
    w_in_v = w_in.ap().rearrange("(k p) n -> p k n", p=128)
    w_out_v = w_out.ap().rearrange("(k p) n -> p k n", p=128)
    w_ada_v = w_ada.ap().rearrange("(k p) n -> p k n", p=128)
    wr_v = w_router.ap().rearrange("(k p) n -> p k n", p=128)

    top = contextlib.ExitStack()

    def sb(st, name, shape, dt=F32):
        return T(st.enter_context(nc.sbuf_tensor("sb_" + name, list(shape), dt)), name)

    def psb(st, name, dt=F32, n=512):
        return T(st.enter_context(nc.psum_tensor("ps_" + name, [128, n], dt)), name)

    def cs(name):
        o, w = lay[name]
        return slice(o, o + w)

    cst = sb(top, "cst", [128, ncol])
    P.dma("sync", lambda e: e.dma_start(out=cst[:], in_=cst_d[:, :]), [], cst.b)
    cbf = sb(top, "cbf", [128, 128 * 4 + 4 * T1], BF16)
    CB = {"ones": slice(0, 128), "c128": slice(128, 256), "s128": slice(256, 384), "ident": slice(384, 512),
          "f1a": slice(512, 512 + 2 * T1), "f1b": slice(512 + 2 * T1, 512 + 4 * T1)}
    for nm in ("ones", "c128", "s128", "ident", "f1a", "f1b"):
        P.op("vector", lambda e, nm=nm: e.tensor_copy(out=cbf[:, CB[nm]], in_=cst[:, cs(nm)]), [cst.b], [cbf.b])
    mods = sb(top, "mods", [128, 96])
    modc = sb(top, "modc", [128, 32])
    ncl = sb(top, "ncl", [128, 48])
    glg = sb(top, "glg", [128, 8])
    nb2 = sb(top, "nb2", [128, 8])
    wa2f = sb(top, "wa2f", [16, 1024])
    wa2b = sb(top, "wa2b", [16, 1024], BF16)
    G1 = sb(top, "G1", [128, 16])
    G1c = sb(top, "G1c", [128, 16])
    G2 = sb(top, "G2", [128, 16])
    idx = sb(top, "idx", [128, NOB], I32)
    P.dma("sync", lambda e: e.dma_start(out=ncl[:], in_=ncols[:, :]), [], ncl.b)
    P.dma("sync", lambda e: e.dma_start(out=glg[:], in_=glag[:, :]), [], glg.b)
    P.dma("sync", lambda e: e.dma_start(out=nb2[:], in_=ba2[:, :]), [], nb2.b)
    P.dma("sync", lambda e: e.dma_start(out=wa2f[:], in_=wa2[:, :]), [], wa2f.b)
    P.dma("sync", lambda e: e.dma_start(out=idx[:], in_=idx_d[:, :]), [], idx.b)
    P.op("vector", lambda e: e.tensor_scalar(out=nb2[:], in0=nb2[:], scalar1=-1.0, scalar2=None, op0=ALU.mult), [nb2.b], [nb2.b])
    P.op("vector", lambda e: e.tensor_copy(out=wa2b[:], in_=wa2f[:]), [wa2f.b], [wa2b.b])

    def _phase1():
        with (contextlib.nullcontext(top) if cfg.noscope else contextlib.ExitStack()) as st:
            cnd = sb(st, "cnd", [128, 32])
            scd = sb(st, "scd", [128, 32])
            P.dma("sync", lambda e: e.dma_start(out=cnd[:], in_=cond[:, :]), [], cnd.b)
            P.op("scalar", lambda e: e.activation(out=scd[:], in_=cnd[:], func=AF.Silu), [cnd.b], [scd.b])
            wst = [sb(st, "wst%d" % i, [128, 4, 512]) for i in range(4)]
            pr = [psb(st, "pr%d" % i) for i in range(2)]
            rows = [sb(st, "rows%d" % i, [2, 512]) for i in range(2)]
            bad = [sb(st, "bad%d" % i, [2, 512]) for i in range(2)]
            sc_v = scd[:].rearrange("p (c k) -> p c k", k=16)
            wi = 0
            for n in range(24):
                pt = pr[n % 2]
                bt = bad[n % 2]
                P.dma("sync", lambda e, bt=bt, n=n: e.dma_start(out=bt[:], in_=b_ada[0:1, n * 512:(n + 1) * 512].partition_broadcast(2)), [], bt.b)
                for kq in range(4):
                    w = wst[wi % 4]
                    wi += 1
                    P.dma("sync", lambda e, w=w, n=n, kq=kq: e.dma_start(out=w[:], in_=w_ada_v[:, kq * 4:(kq + 1) * 4, n * 512:(n + 1) * 512]), [], w.b)
                    for kk in range(4):
                        k = kq * 4 + kk
                        P.op("tensor", lambda e, pt=pt, w=w, kk=kk, k=k: e.matmul(pt[0:2, :], lhsT=sc_v[:, :, k], rhs=w[:, kk, :], start=(k == 0), stop=(k == 15)),
                             [scd.b, w.b], [pt.b])
                rw = rows[n % 2]
                P.op("vector", lambda e, rw=rw, pt=pt, bt=bt: e.tensor_tensor(out=rw[:], in0=pt[0:2, :], in1=bt[:], op=ALU.add), [pt.b, bt.b], [rw.b])
                P.dma("gpsimd", lambda e, rw=rw, n=n: e.dma_start(out=ADA[0:2, n * 512:(n + 1) * 512], in_=rw[:]), [rw.b], ADA.b)
            P.dma("sync", lambda e: e.dma_start(out=mods[:], in_=ADA[0:1, :].rearrange("o (j p) -> p (o j)", p=128), allow_slow_non_contiguous=True), [ADA.b], mods.b)
            P.dma("sync", lambda e: e.dma_start(out=modc[:], in_=ADA[1:2, 0:4096].rearrange("o (j p) -> p (o j)", p=128), allow_slow_non_contiguous=True), [ADA.b], modc.b)
            P.op("vector", lambda e: e.scalar_tensor_tensor(out=G1[:], in0=mods[:, 16:32], scalar=1.0, in1=ncl[:, 0:16], op0=ALU.add, op1=ALU.mult), [mods.b, ncl.b], [G1.b])
            P.op("vector", lambda e: e.scalar_tensor_tensor(out=G1c[:], in0=modc[:, 16:32], scalar=1.0, in1=ncl[:, 0:16], op0=ALU.add, op1=ALU.mult), [modc.b, ncl.b], [G1c.b])
            P.op("vector", lambda e: e.scalar_tensor_tensor(out=G2[:], in0=mods[:, 64:80], scalar=1.0, in1=ncl[:, 16:32], op0=ALU.add, op1=ALU.mult), [mods.b, ncl.b], [G2.b])
    _phase1()
    dump('mods', mods, [128, 96])
    dump('modc', modc, [128, 32])
    dump('G1', G1, [128, 16])
    P.barrier()

    def rstd_from_sq(pbank, sq, nk, n, dst, inv_dim):
        for k in range(nk):
            P.op("tensor", lambda e, k=k: e.matmul(pbank[:, 0:n], lhsT=cbf[:, CB["ones"]], rhs=sq[:, k, 0:n], start=(k == 0), stop=(k == nk - 1)),
                 [cbf.b, sq.b], [pbank.b])
        P.op("scalar", lambda e: e.activation(out=dst[:, 0:n], in_=pbank[:, 0:n], func=AF.Ln, scale=inv_dim, bias=EPS), [pbank.b], [dst.b])
        P.op("scalar", lambda e: e.activation(out=dst[:, 0:n], in_=dst[:, 0:n], func=AF.Exp, scale=-0.5), [dst.b], [dst.b])

    hc = sb(top, "hc", [128, KC, CTX], BF16)
    def _phase2():
        with (contextlib.nullcontext(top) if cfg.noscope else contextlib.ExitStack()) as st:
            xs = [sb(st, "xs%d" % i, [128, KC, TB]) for i in range(2)] if not cfg.noscope else [sb(st, "xs0", [128, KC, TB])] * 2
            sq = sb(st, "sq", [128, KC, TB], BF16)
            rs = sb(st, "rs", [128, TB])
            tm = sb(st, "tm", [128, KC, TB])
            hts = [sb(st, "hts%d" % i, [128, KC, TB], BF16) for i in range(2)]
            pb = psb(st, "a0p")

            def front(src_ap, n, xsl, Gc, SHc, dst):
                P.dma("sync", lambda e: e.dma_start(out=xsl[:, :, 0:n], in_=src_ap), [], xsl.b)
                P.op("gpsimd", lambda e: e.tensor_tensor(out=sq[:, :, 0:n], in0=xsl[:, :, 0:n], in1=xsl[:, :, 0:n], op=ALU.mult), [xsl.b], [sq.b])
                rstd_from_sq(pb, sq, KC, n, rs, 1.0 / D)
                if n == CTX:
                    dump('xsl', xsl, [128, KC, TB])
                    dump('rs', rs, [128, TB])
                    dump('sq', sq, [128, KC, TB], BF16)
                P.op("vector", lambda e: e.tensor_tensor(out=tm[:, :, 0:n], in0=xsl[:, :, 0:n], in1=rs[:, 0:n].unsqueeze(1).to_broadcast([128, KC, n]), op=ALU.mult),
                     [xsl.b, rs.b], [tm.b])
                P.op("gpsimd", lambda e: e.tensor_tensor(out=tm[:, :, 0:n], in0=tm[:, :, 0:n], in1=Gc[:, 0:16].unsqueeze(2).to_broadcast([128, KC, n]), op=ALU.mult),
                     [tm.b, Gc.b], [tm.b])
                P.op("vector", lambda e: e.tensor_tensor(out=dst[:, :, 0:n], in0=tm[:, :, 0:n], in1=SHc.unsqueeze(2).to_broadcast([128, KC, n]), op=ALU.add),
                     [tm.b, mods.b, modc.b], [dst.b])

            front(ctxT_v[:, :, :], CTX, xs[0], G1c, modc[:, 0:16], hc)
            for blk in range(NB):
                h = hts[blk % 2]
                front(xT_v[:, :, blk * TB:(blk + 1) * TB], TB, xs[(blk + 1) % 2], G1, mods[:, 0:16], h)
                P.dma("gpsimd", lambda e, h=h, blk=blk: e.dma_start(out=HT[blk * 128:(blk + 1) * 128, :], in_=h[:].rearrange("p k t -> p (k t)")), [h.b], HT.b)
    _phase2()
    P.barrier()
    if cfg.stop == "A0":
        P.build(final_waits=[HT.b, ADA.b] + dumps)
        return nc, P

    def _phase3():
        with (contextlib.nullcontext(top) if cfg.noscope else contextlib.ExitStack()) as st:
            wstg = [sb(st, "wstg%d" % i, [128, KC, 256]) for i in range(2)]
            Wq = sb(st, "Wq", [128, KC, 128], BF16)
            Wk = sb(st, "Wk", [128, KC, 128], BF16)
            Wv = sb(st, "Wv", [128, KC, 256], BF16)
            Wg = sb(st, "Wg", [128, KC, 256], BF16)
            Wz = sb(st, "Wz", [128, KC, 32], BF16)
            hb = [sb(st, "hb%d" % i, [128, KC, TB], BF16) for i in range(2)]
            zt = sb(st, "zt", [16, TB], BF16)
            ex = sb(st, "ex", [128, TB])
            sp = sb(st, "sp", [128, TB])
            cum = sb(st, "cum", [128, TB])
            ea = sb(st, "ea", [128, TB])
            eb = sb(st, "eb", [128, TB])
            ec = sb(st, "ec", [128, TB])
            dec = sb(st, "dec", [128, 4])
            qs = sb(st, "qs", [128, TB], BF16)
            ks = sb(st, "ks", [128, TB], BF16)
            qd = sb(st, "qd", [128, TB], BF16)
            ki = sb(st, "ki", [128, TB], BF16)
            kte = sb(st, "kte", [128, TB])
            kteT = sb(st, "kteT", [128, 4, 128], BF16)
            vt = sb(st, "vt", [128, 4, 256], BF16)
            sg = sb(st, "sg", [128, 2, TB], BF16)
            sT = [sb(st, "sT%d" % i, [128, 128], BF16) for i in range(2)]
            state = sb(st, "state", [128, 256])
            sbf = [sb(st, "sbf%d" % i, [128, 256], BF16) for i in range(2)]
            Sf = sb(st, "Sf", [128, 256])
            Sb = sb(st, "Sb", [128, 256])
            osb = sb(st, "osb", [128, 2, TB])
            obl = sb(st, "obl", [128, 2, TB])
            osq = sb(st, "osq", [128, 2, TB], BF16)
            ors = sb(st, "ors", [128, TB])
            yt = sb(st, "yt", [128, 2, TB], BF16)
            B0, B1, B2, B3, B4, B5, B6, B7 = [psb(st, "gp%d" % i) for i in range(8)]
            b4v = Buf("b4v"); b4kv = Buf("b4kv"); b5s = Buf("b5s"); b5t = Buf("b5t")

            def load_w(dst, c0, n):
                w = wstg[load_w.i % 2]
                load_w.i += 1
                P.dma("sync", lambda e: e.dma_start(out=w[:, :, 0:n], in_=w_in_v[:, :, c0:c0 + n]), [], w.b)
                P.op("vector", lambda e: e.tensor_copy(out=dst[:, :, 0:n], in_=w[:, :, 0:n]), [w.b], [dst.b])
            load_w.i = 0

            def gla_block(h, hsrc, hbuf, n, direction, emit_out, blk):
                nch = n // 128
                dcol = 0 if direction == "f" else 1
                use_cache = emit_out and direction == "f"
                if not use_cache:
                    for k in range(KC):
                        P.op("tensor", lambda e, k=k: e.matmul(B0[:, 0:n], lhsT=Wq[:, k, :], rhs=hsrc[:, k, 0:n], start=(k == 0), stop=(k == KC - 1)), [Wq.b, hbuf], [B0.b])
                    for k in range(KC):
                        P.op("tensor", lambda e, k=k: e.matmul(B1[:, 0:n], lhsT=Wk[:, k, :], rhs=hsrc[:, k, 0:n], start=(k == 0), stop=(k == KC - 1)), [Wk.b, hbuf], [B1.b])
                    qsrc, qsb, ksrc, ksb = B0[:, 0:n], B0.b, B1[:, 0:n], B1.b
                    if emit_out:
                        P.op("scalar", lambda e: e.activation(out=qs[:], in_=B0[:, :], func=AF.Copy), [B0.b], [qs.b])
                        P.op("scalar", lambda e: e.activation(out=ks[:], in_=B1[:, :], func=AF.Copy), [B1.b], [ks.b])
                        P.dma("gpsimd", lambda e: e.dma_start(out=QS[blk * 128:(blk + 1) * 128, :], in_=qs[:]), [qs.b], QS.b)
                        P.dma("gpsimd", lambda e: e.dma_start(out=KS[blk * 128:(blk + 1) * 128, :], in_=ks[:]), [ks.b], KS.b)
                else:
                    P.dma("sync", lambda e: e.dma_start(out=qs[:], in_=QS[blk * 128:(blk + 1) * 128, :]), [QS.b], qs.b)
                    P.dma("sync", lambda e: e.dma_start(out=ks[:], in_=KS[blk * 128:(blk + 1) * 128, :]), [KS.b], ks.b)
                    P.dma("sync", lambda e: e.dma_start(out=vt[:].rearrange("p c v -> p (c v)"), in_=VS[blk * 128:(blk + 1) * 128, :]), [VS.b], vt.b)
                    qsrc, qsb, ksrc, ksb = qs[:, 0:n], qs.b, ks[:, 0:n], ks.b
                for k in range(KC):
                    P.op("tensor", lambda e, k=k: e.matmul(B2[0:16, 0:n], lhsT=Wz[:, k, dcol * 16:(dcol + 1) * 16], rhs=hsrc[:, k, 0:n], start=(k == 0), stop=(k == KC - 1)), [Wz.b, hbuf], [B2.b])
                P.op("vector", lambda e: e.tensor_copy(out=zt[:, 0:n], in_=B2[0:16, 0:n]), [B2.b], [zt.b])
                P.op("tensor", lambda e: e.matmul(B3[:, 0:n], lhsT=wa2b[:, dcol * 512 + h * 128:dcol * 512 + (h + 1) * 128], rhs=zt[:, 0:n], start=True, stop=True), [wa2b.b, zt.b], [B3.b])
                P.op("scalar", lambda e: e.activation(out=ex[:, 0:n], in_=B3[:, 0:n], func=AF.Exp, scale=-1.0, bias=nb2[:, dcol * 4 + h:dcol * 4 + h + 1]), [B3.b, nb2.b], [ex.b])
                P.op("scalar", lambda e: e.activation(out=sp[:, 0:n], in_=ex[:, 0:n], func=AF.Ln, bias=1.0), [ex.b], [sp.b])
                P.op("vector", lambda e: e.tensor_tensor_scan(out=cum[:, 0:n], data0=cst[:, lay["reset"][0]:lay["reset"][0] + n], data1=sp[:, 0:n], initial=0.0, op0=ALU.mult, op1=ALU.add),
                     [cst.b, sp.b], [cum.b])
                cum3 = cum[:, 0:n].rearrange("p (c j) -> p c j", j=128)
                tot = cum3[:, :, 127:128]
                P.op("scalar", lambda e: e.activation(out=dec[:, 0:nch], in_=cum3[:, :, 127], func=AF.Exp, scale=-1.0 / 16), [cum.b], [dec.b])
                if direction == "b":
                    P.op("vector", lambda e: e.tensor_tensor(out=ec[:, 0:n], in0=sp[:, 0:n], in1=cum[:, 0:n], op=ALU.subtract), [sp.b, cum.b], [ec.b])
                    ec3 = ec[:, 0:n].rearrange("p (c j) -> p c j", j=128)
                    P.op("vector", lambda e: e.tensor_tensor(out=ec3, in0=ec3, in1=tot.to_broadcast([128, nch, 128]), op=ALU.add), [ec.b, cum.b], [ec.b])
                    cdir = ec
                else:
                    cdir = cum
                P.op("scalar", lambda e: e.activation(out=ea[:, 0:n], in_=cdir[:, 0:n], func=AF.Exp, scale=-1.0 / 16), [cdir.b], [ea.b])
                P.op("scalar", lambda e: e.activation(out=eb[:, 0:n], in_=cdir[:, 0:n], func=AF.Exp, scale=1.0 / 16), [cdir.b], [eb.b])
                P.op("vector", lambda e: e.scalar_tensor_tensor(out=qd[:, 0:n], in0=qsrc, scalar=128.0 ** -0.5, in1=ea[:, 0:n], op0=ALU.mult, op1=ALU.mult), [qsb, ea.b], [qd.b])
                P.op("vector", lambda e: e.tensor_tensor(out=ki[:, 0:n], in0=ksrc, in1=eb[:, 0:n], op=ALU.mult), [ksb, eb.b], [ki.b])
                ea3 = ea[:, 0:n].rearrange("p (c j) -> p c j", j=128)
                cd3 = cdir[:, 0:n].rearrange("p (c j) -> p c j", j=128)
                P.op("vector", lambda e: e.tensor_tensor(out=ea3, in0=cd3, in1=tot.to_broadcast([128, nch, 128]), op=ALU.subtract), [cdir.b, cum.b, qd.b], [ea.b])
                P.op("scalar", lambda e: e.activation(out=ea[:, 0:n], in_=ea[:, 0:n], func=AF.Exp, scale=1.0 / 16), [ea.b], [ea.b])
                P.op("vector", lambda e: e.tensor_tensor(out=kte[:, 0:n], in0=ksrc, in1=ea[:, 0:n], op=ALU.mult), [ksb, ea.b], [kte.b])
                for c in range(nch):
                    if not use_cache:
                        for k in range(KC):
                            P.op("tensor", lambda e, k=k, c=c: e.matmul(B4[:, 0:256], lhsT=hsrc[:, k, c * 128:(c + 1) * 128], rhs=Wv[:, k, :], start=(k == 0), stop=(k == KC - 1)), [Wv.b, hbuf], [b4v])
                        P.op("scalar", lambda e, c=c: e.activation(out=vt[:, c, :], in_=B4[:, 0:256], func=AF.Copy), [b4v], [vt.b])
                    P.op("tensor", lambda e, c=c: e.transpose(B5[:, 128:256], kte[:, c * 128:(c + 1) * 128], cst[:, cs("ident")]), [kte.b, cst.b], [b5t])
                    P.op("vector", lambda e, c=c: e.tensor_copy(out=kteT[:, c, :], in_=B5[:, 128:256]), [b5t], [kteT.b])
                if emit_out and direction == "b":
                    P.dma("gpsimd", lambda e: e.dma_start(out=VS[blk * 128:(blk + 1) * 128, :], in_=vt[:].rearrange("p c v -> p (c v)")), [vt.b], VS.b)
                if emit_out and direction == "f":
                    for vb in range(2):
                        Bg = (B0, B1)[vb]
                        for k in range(KC):
                            P.op("tensor", lambda e, k=k, vb=vb, Bg=Bg: e.matmul(Bg[:, 0:n], lhsT=Wg[:, k, vb * 128:(vb + 1) * 128], rhs=hsrc[:, k, 0:n], start=(k == 0), stop=(k == KC - 1)), [Wg.b, hbuf], [Bg.b])
                        P.op("scalar", lambda e, vb=vb, Bg=Bg: e.activation(out=sg[:, vb, 0:n], in_=Bg[:, 0:n], func=AF.Silu), [Bg.b], [sg.b])
                order = range(nch) if direction == "f" else range(nch - 1, -1, -1)
                mask = cst[:, cs("maskf")] if direction == "f" else cst[:, cs("maskb")]
                for c in order:
                    csl = slice(c * 128, (c + 1) * 128)
                    if emit_out:
                        s_ = sT[gla_block.si % 2]
                        P.op("tensor", lambda e, csl=csl: e.matmul(B5[:, 0:128], lhsT=ki[:, csl], rhs=qd[:, csl], start=True, stop=True), [ki.b, qd.b], [b5s])
                        P.op("vector", lambda e, s_=s_: e.tensor_tensor(out=s_[:], in0=B5[:, 0:128], in1=mask, op=ALU.mult), [b5s, cst.b], [s_.b])
                        gla_block.si += 1
                        for vb in range(2):
                            Bo = (B6, B7)[vb]
                            P.op("tensor", lambda e, vb=vb, c=c, csl=csl, s_=s_, Bo=Bo: e.matmul(Bo[:, csl], lhsT=vt[:, c, vb * 128:(vb + 1) * 128], rhs=s_[:], start=True, stop=False), [vt.b, s_.b], [Bo.b])
                            P.op("tensor", lambda e, vb=vb, csl=csl, Bo=Bo, cur=gla_block.cur: e.matmul(Bo[:, csl], lhsT=cur[:, vb * 128:(vb + 1) * 128], rhs=qd[:, csl], start=False, stop=True), [gla_block.cur.b, qd.b], [Bo.b])
                    P.op("tensor", lambda e, c=c: e.matmul(B4[:, 256:512], lhsT=kteT[:, c, :], rhs=vt[:, c, :], start=True, stop=True), [kteT.b, vt.b], [b4kv])
                    P.op("vector", lambda e, c=c: e.scalar_tensor_tensor(out=state[:], in0=state[:], scalar=dec[:, c:c + 1], in1=B4[:, 256:512], op0=ALU.mult, op1=ALU.add), [state.b, dec.b, b4kv], [state.b])
                    nxt = sbf[(gla_block.ci + 1) % 2]
                    gla_block.ci += 1
                    P.op("scalar", lambda e, nxt=nxt: e.activation(out=nxt[:], in_=state[:], func=AF.Copy), [state.b], [nxt.b])
                    gla_block.cur = nxt
                if emit_out:
                    if direction == "b":
                        P.op("vector", lambda e: e.tensor_copy(out=osb[:, 0, :], in_=B6[:, :]), [B6.b], [osb.b])
                        P.op("vector", lambda e: e.tensor_copy(out=osb[:, 1, :], in_=B7[:, :]), [B7.b], [osb.b])
                        P.dma("gpsimd", lambda e: e.dma_start(out=OB[blk * 128:(blk + 1) * 128, :], in_=osb[:].rearrange("p a t -> p (a t)")), [osb.b], OB.b)
                    else:
                        P.dma("sync", lambda e: e.dma_start(out=obl[:].rearrange("p a t -> p (a t)"), in_=OB[blk * 128:(blk + 1) * 128, :]), [OB.b], obl.b)
                        P.op("vector", lambda e: e.tensor_tensor(out=osb[:, 0, :], in0=B6[:, :], in1=obl[:, 0, :], op=ALU.add), [B6.b, obl.b], [osb.b])
                        P.op("vector", lambda e: e.tensor_tensor(out=osb[:, 1, :], in0=B7[:, :], in1=obl[:, 1, :], op=ALU.add), [B7.b, obl.b], [osb.b])
                        P.op("gpsimd", lambda e: e.tensor_tensor(out=osq[:], in0=osb[:], in1=osb[:], op=ALU.mult), [osb.b], [osq.b])
                        rstd_from_sq(B2, osq, 2, TB, ors, 1.0 / 256)
                        P.op("vector", lambda e: e.tensor_tensor(out=osb[:], in0=osb[:], in1=ors[:].unsqueeze(1).to_broadcast([128, 2, TB]), op=ALU.mult), [osb.b, ors.b], [osb.b])
                        for vb in range(2):
                            P.op("vector", lambda e, vb=vb: e.scalar_tensor_tensor(out=yt[:, vb, :], in0=osb[:, vb, :], scalar=glg[:, 2 * h + vb:2 * h + vb + 1], in1=sg[:, vb, :], op0=ALU.mult, op1=ALU.mult),
                                 [osb.b, glg.b, sg.b], [yt.b])
                        P.dma("gpsimd", lambda e: e.dma_start(out=YT[blk * 128:(blk + 1) * 128, 2 * h * TB:(2 * h + 2) * TB], in_=yt[:].rearrange("p a t -> p (a t)")), [yt.b], YT.b)
            gla_block.si = 0
            gla_block.ci = 0
            gla_block.cur = sbf[0]

            def set_state(src):
                if src is None:
                    P.op("vector", lambda e: e.memset(state[:], 0.0), [], [state.b])
                else:
                    P.op("vector", lambda e: e.tensor_copy(out=state[:], in_=src[:]), [src.b], [state.b])
                nxt = sbf[(gla_block.ci + 1) % 2]
                gla_block.ci += 1
                P.op("scalar", lambda e: e.activation(out=nxt[:], in_=state[:], func=AF.Copy), [state.b], [nxt.b])
                gla_block.cur = nxt

            for h in range(4):
                load_w(Wq, h * 128, 128)
                load_w(Wk, 512 + h * 128, 128)
                load_w(Wv, 1024 + h * 256, 256)
                load_w(Wg, 2048 + h * 256, 256)
                load_w(Wz, 3072, 32)
                set_state(None)
                gla_block(h, hc, hc.b, CTX, "f", False, 0)
                P.op("vector", lambda e: e.tensor_copy(out=Sf[:], in_=state[:]), [state.b], [Sf.b])
                set_state(None)
                gla_block(h, hc, hc.b, CTX, "b", False, 0)
                P.op("vector", lambda e: e.tensor_copy(out=Sb[:], in_=state[:]), [state.b], [Sb.b])
                set_state(Sb)
                for i, blk in enumerate(range(NB - 1, -1, -1)):
                    hbt = hb[i % 2]
                    P.dma("sync", lambda e, hbt=hbt, blk=blk: e.dma_start(out=hbt[:].rearrange("p k t -> p (k t)"), in_=HT[blk * 128:(blk + 1) * 128, :]), [HT.b], hbt.b)
                    gla_block(h, hbt, hbt.b, TB, "b", True, blk)
                set_state(Sf)
                for i, blk in enumerate(range(NB)):
                    hbt = hb[i % 2]
                    P.dma("sync", lambda e, hbt=hbt, blk=blk: e.dma_start(out=hbt[:].rearrange("p k t -> p (k t)"), in_=HT[blk * 128:(blk + 1) * 128, :]), [HT.b], hbt.b)
                    gla_block(h, hbt, hbt.b, TB, "f", True, blk)
    _phase3()
    P.barrier()

    if cfg.stop == "A1":
        P.build(final_waits=[HT.b, ADA.b, YT.b] + dumps)
        return nc, P

    NK2 = TB // T1
    def _phase4():
        with (contextlib.nullcontext(top) if cfg.noscope else contextlib.ExitStack()) as st:
            wstg = sb(st, "fwst", [128, KC, 128])
            Wu = sb(st, "Wu", [128, KC, 128], BF16)
            hb = [sb(st, "fhb%d" % i, [128, KC, TB], BF16) for i in range(2)]
            ut = sb(st, "ut", [128, TB], BF16)
            at = [sb(st, "at%d" % i, [128, TB], BF16) for i in range(2)]
            bt_ = [sb(st, "btt%d" % i, [128, TB], BF16) for i in range(2)]
            DA = sb(st, "DA", [T1, 128, 128], BF16)
            DB = sb(st, "DB", [T1, 128, 128], BF16)
            Yall = sb(st, "Yall", [T1, 128, 128], BF16)
            xs1 = [sb(st, "xs1%d" % i, [128, 2, 2, T1]) for i in range(2)]
            tA = [sb(st, "tA%d" % i, [128, 2, T1]) for i in range(2)]
            tB = [sb(st, "tB%d" % i, [128, 2, T1]) for i in range(2)]
            br = [sb(st, "br%d" % i, [128, 2, T1], BF16) for i in range(2)]
            bi = [sb(st, "bi%d" % i, [128, 2, T1], BF16) for i in range(2)]
            yo = [sb(st, "yo%d" % i, [128, TB], BF16) for i in range(2)]
            pu = psb(st, "pu")
            pa = psb(st, "pa")
            pbk = psb(st, "pbk")
            p1 = [psb(st, "p1%d" % i) for i in range(2)]
            p2 = [psb(st, "p2%d" % i) for i in range(2)]
            ptr = T(st.enter_context(nc.psum_tensor("ps_ptr", [128, 1024], BF16)), "ptr")
            twc3 = cst[:, cs("twc")].unsqueeze(1).to_broadcast([128, 2, T1])
            tws3 = cst[:, cs("tws")].unsqueeze(1).to_broadcast([128, 2, T1])
            for gi in range(8):
                P.dma("sync", lambda e, gi=gi: e.dma_start(out=wstg[:], in_=w_in_v[:, :, 3104 + gi * 128:3104 + (gi + 1) * 128]), [], wstg.b)
                P.op("vector", lambda e: e.tensor_copy(out=Wu[:], in_=wstg[:]), [wstg.b], [Wu.b])
                for blk in range(NB):
                    hbt = hb[blk % 2]
                    P.dma("sync", lambda e, hbt=hbt, blk=blk: e.dma_start(out=hbt[:].rearrange("p k t -> p (k t)"), in_=HT[blk * 128:(blk + 1) * 128, :]), [HT.b], hbt.b)
                    for k in range(KC):
                        P.op("tensor", lambda e, k=k, hbt=hbt: e.matmul(pu[:, :], lhsT=Wu[:, k, :], rhs=hbt[:, k, :], start=(k == 0), stop=(k == KC - 1)), [Wu.b, hbt.b], [pu.b])
                    P.op("scalar", lambda e: e.activation(out=ut[:], in_=pu[:, :], func=AF.Copy), [pu.b], [ut.b])
                    a_, b_ = at[blk % 2], bt_[blk % 2]
                    P.op("tensor", lambda e: e.matmul(pa[:, :], lhsT=cbf[:, CB["c128"]], rhs=ut[:], start=True, stop=True), [cbf.b, ut.b], [pa.b])
                    P.op("tensor", lambda e: e.matmul(pbk[:, :], lhsT=cbf[:, CB["s128"]], rhs=ut[:], start=True, stop=True), [cbf.b, ut.b], [pbk.b])
                    P.op("vector", lambda e, a_=a_: e.tensor_copy(out=a_[:], in_=pa[:, :]), [pa.b], [a_.b])
                    P.op("scalar", lambda e, b_=b_: e.activation(out=b_[:], in_=pbk[:, :], func=AF.Copy), [pbk.b], [b_.b])
                    P.dma("gpsimd", lambda e, a_=a_, blk=blk: e.dma_start(out=FA[:, blk * TB:(blk + 1) * TB], in_=a_[:]), [a_.b], FA.b)
                    P.dma("gpsimd", lambda e, b_=b_, blk=blk: e.dma_start(out=FB[:, blk * TB:(blk + 1) * TB], in_=b_[:]), [b_.b], FB.b)
                for m0 in range(0, 128, 16):
                    P.dma("sync", lambda e, m0=m0: e.dma_start(out=DA[:, m0:m0 + 16, :], in_=FA[m0:m0 + 16, :].rearrange("m (a b) -> a m b", b=128)), [FA.b], DA.b)
                    P.dma("sync", lambda e, m0=m0: e.dma_start(out=DB[:, m0:m0 + 16, :], in_=FB[m0:m0 + 16, :].rearrange("m (a b) -> a m b", b=128)), [FB.b], DB.b)
                for mp in range(64):
                    q = mp % 2
                    ps1, ps2 = p1[q], p2[q]
                    for j in range(2):
                        m = mp * 2 + j
                        P.op("tensor", lambda e, m=m, j=j, ps1=ps1: e.matmul(ps1[:, j * 2 * T1:(j + 1) * 2 * T1], lhsT=DA[:, m, :], rhs=cbf[0:T1, CB["f1a"]], start=True, stop=False), [DA.b, cbf.b], [ps1.b])
                        P.op("tensor", lambda e, m=m, j=j, ps1=ps1: e.matmul(ps1[:, j * 2 * T1:(j + 1) * 2 * T1], lhsT=DB[:, m, :], rhs=cbf[0:T1, CB["f1b"]], start=False, stop=True), [DB.b, cbf.b], [ps1.b])
                    x1_, ta, tb, br_, bi_ = xs1[q], tA[q], tB[q], br[q], bi[q]
                    P.op("scalar", lambda e, x1_=x1_, ps1=ps1: e.activation(out=x1_[:].rearrange("p a b c -> p (a b c)"), in_=ps1[:, 0:4 * T1], func=AF.Copy), [ps1.b], [x1_.b])
                    re_, im_ = x1_[:, :, 0, :], x1_[:, :, 1, :]
                    P.op("vector", lambda e, ta=ta, re_=re_: e.tensor_tensor(out=ta[:], in0=re_, in1=twc3, op=ALU.mult), [x1_.b, cst.b], [ta.b])
                    P.op("gpsimd", lambda e, tb=tb, im_=im_: e.tensor_tensor(out=tb[:], in0=im_, in1=tws3, op=ALU.mult), [x1_.b, cst.b], [tb.b])
                    P.op("vector", lambda e, ta=ta, tb=tb, br_=br_: e.tensor_tensor(out=br_[:], in0=ta[:], in1=tb[:], op=ALU.add), [ta.b, tb.b], [br_.b])
                    P.op("gpsimd", lambda e, tb=tb, im_=im_: e.tensor_tensor(out=tb[:], in0=im_, in1=twc3, op=ALU.mult), [x1_.b, cst.b, br_.b], [tb.b])
                    P.op("vector", lambda e, ta=ta, re_=re_: e.tensor_tensor(out=ta[:], in0=re_, in1=tws3, op=ALU.mult), [x1_.b, cst.b, br_.b], [ta.b])
                    P.op("gpsimd", lambda e, ta=ta, tb=tb, bi_=bi_: e.tensor_tensor(out=bi_[:], in0=tb[:], in1=ta[:], op=ALU.subtract), [ta.b, tb.b], [bi_.b])
                    for j in range(2):
                        m = mp * 2 + j
                        P.op("tensor", lambda e, j=j, ps2=ps2, br_=br_: e.matmul(ps2[0:T1, j * 128:(j + 1) * 128], lhsT=br_[:, j, :], rhs=cbf[:, CB["c128"]], start=True, stop=False), [br_.b, cbf.b], [ps2.b])
                        P.op("tensor", lambda e, j=j, ps2=ps2, bi_=bi_: e.matmul(ps2[0:T1, j * 128:(j + 1) * 128], lhsT=bi_[:, j, :], rhs=cbf[:, CB["s128"]], start=False, stop=True), [bi_.b, cbf.b], [ps2.b])
                    P.op("scalar", lambda e, mp=mp, ps2=ps2: e.activation(out=Yall[:, :, mp * 2:mp * 2 + 2].rearrange("p k m -> p m k"), in_=ps2[0:T1, 0:256].rearrange("p (m k) -> p m k", k=128), func=AF.Copy),
                         [ps2.b], [Yall.b])
                for blk in range(NB):
                    yb = yo[blk % 2]
                    for jj in range(NK2):
                        k2 = blk * NK2 + jj
                        P.op("tensor", lambda e, k2=k2, jj=jj: e.transpose(ptr[:, jj * T1:(jj + 1) * T1], Yall[:, k2, :], cbf[0:T1, 384:384 + T1]), [Yall.b, cbf.b], [ptr.b])
                    P.op("vector", lambda e, yb=yb: e.tensor_copy(out=yb[:], in_=ptr[:, 0:TB]), [ptr.b], [yb.b])
                    P.dma("gpsimd", lambda e, yb=yb, blk=blk, gi=gi: e.dma_start(out=YT[blk * 128:(blk + 1) * 128, (8 + gi) * TB:(9 + gi) * TB], in_=yb[:]), [yb.b], YT.b)
    _phase4()
    P.barrier()

    if cfg.stop == "A2":
        P.build(final_waits=[HT.b, ADA.b, YT.b] + dumps)
        return nc, P

    NT = L // 128
    affall = sb(top, "affall", [128, NT, E])
    thr = sb(top, "thr", [128, E])
    def _phase5():
        with (contextlib.nullcontext(top) if cfg.noscope else contextlib.ExitStack()) as st:
            wst = sb(st, "bwst", [128, 1, 2048])
            Wo = sb(st, "Wo", [128, KC, D], BF16)
            wrf = sb(st, "wrf", [128, KC, E])
            wrb = sb(st, "wrb", [128, KC, E], BF16)
            yb = [sb(st, "byb0", [128, KC, TB], BF16)] * 2
            xb = [sb(st, "bxb0", [128, KC, TB])] * 2
            sq = sb(st, "bsq", [128, KC, TB], BF16)
            rs = sb(st, "brs", [128, TB])
            hf = [sb(st, "bhf0", [128, KC, TB], BF16)] * 2
            mx = sb(st, "bmx", [128, 4])
            sm = sb(st, "bsm", [128, 4])
            ee = sb(st, "bee", [128, 4, E])
            po = [psb(st, "po%d" % i) for i in range(2)]
            pq = psb(st, "pq")
            pl = psb(st, "pl")
            for kq in range(KC):
                P.dma("sync", lambda e, kq=kq: e.dma_start(out=wst[:], in_=w_out_v[:, kq:kq + 1, :]), [], wst.b)
                P.op("vector", lambda e, kq=kq: e.tensor_copy(out=Wo[:, kq:kq + 1, :], in_=wst[:]), [wst.b], [Wo.b])
            P.dma("sync", lambda e: e.dma_start(out=wrf[:], in_=wr_v), [], wrf.b)
            P.op("vector", lambda e: e.tensor_copy(out=wrb[:], in_=wrf[:]), [wrf.b], [wrb.b])
            for blk in range(NB):
                y_, x_, h_ = yb[0], xb[0], hf[0]
                x1 = x_
                P.dma("sync", lambda e, y_=y_, blk=blk: e.dma_start(out=y_[:].rearrange("p k t -> p (k t)"), in_=YT[blk * 128:(blk + 1) * 128, :]), [YT.b], y_.b)
                P.dma("sync", lambda e, x_=x_, blk=blk: e.dma_start(out=x_[:], in_=xT_v[:, :, blk * TB:(blk + 1) * TB]), [], x_.b)
                for nb in range(KC):
                    pp = po[nb % 2]
                    for k in range(KC):
                        P.op("tensor", lambda e, pp=pp, k=k, nb=nb, y_=y_: e.matmul(pp[:, :], lhsT=Wo[:, k, nb * 128:(nb + 1) * 128], rhs=y_[:, k, :], start=(k == 0), stop=(k == KC - 1)), [Wo.b, y_.b], [pp.b])
                    P.op("vector", lambda e, pp=pp, nb=nb, x_=x_: e.scalar_tensor_tensor(out=x1[:, nb, :], in0=pp[:, :], scalar=mods[:, 32 + nb:33 + nb], in1=x_[:, nb, :], op0=ALU.mult, op1=ALU.add),
                         [pp.b, mods.b, x_.b], [x1.b])
                P.dma("gpsimd", lambda e, blk=blk: e.dma_start(out=X1[blk * 128:(blk + 1) * 128, :], in_=x1[:].rearrange("p k t -> p (k t)")), [x1.b], X1.b)
                P.op("gpsimd", lambda e: e.tensor_tensor(out=sq[:], in0=x1[:], in1=x1[:], op=ALU.mult), [x1.b], [sq.b])
                rstd_from_sq(pq, sq, KC, TB, rs, 1.0 / D)
                P.op("vector", lambda e: e.tensor_tensor(out=x1[:], in0=x1[:], in1=rs[:].unsqueeze(1).to_broadcast([128, KC, TB]), op=ALU.mult), [x1.b, rs.b], [x1.b])
                P.op("gpsimd", lambda e: e.tensor_tensor(out=x1[:], in0=x1[:], in1=G2[:, 0:16].unsqueeze(2).to_broadcast([128, KC, TB]), op=ALU.mult), [x1.b, G2.b], [x1.b])
                P.op("vector", lambda e, h_=h_: e.tensor_tensor(out=h_[:], in0=x1[:], in1=mods[:, 48:64].unsqueeze(2).to_broadcast([128, KC, TB]), op=ALU.add), [x1.b, mods.b], [h_.b])
                P.dma("gpsimd", lambda e, h_=h_, blk=blk: e.dma_start(out=HF[blk * 128:(blk + 1) * 128, :], in_=h_[:].rearrange("p k t -> p (k t)")), [h_.b], HF.b)
                for s in range(4):
                    for k in range(KC):
                        P.op("tensor", lambda e, s=s, k=k, h_=h_: e.matmul(pl[:, s * E:(s + 1) * E], lhsT=h_[:, k, s * 128:(s + 1) * 128], rhs=wrb[:, k, :], start=(k == 0), stop=(k == KC - 1)), [h_.b, wrb.b], [pl.b])
                pl3 = pl[:, 0:4 * E].rearrange("p (s e) -> p s e", e=E)
                P.op("vector", lambda e: e.tensor_reduce(out=mx[:], in_=pl3, axis=AX.X, op=ALU.max), [pl.b], [mx.b])
                P.op("vector", lambda e: e.tensor_tensor(out=ee[:], in0=pl3, in1=mx[:].unsqueeze(2).to_broadcast([128, 4, E]), op=ALU.subtract), [pl.b, mx.b], [ee.b])
                P.op("scalar", lambda e: e.activation(out=ee[:], in_=ee[:], func=AF.Exp), [ee.b], [ee.b])
                P.op("vector", lambda e: e.tensor_reduce(out=sm[:], in_=ee[:], axis=AX.X, op=ALU.add), [ee.b], [sm.b])
                P.op("vector", lambda e: e.reciprocal(out=sm[:], in_=sm[:]), [sm.b], [sm.b])
                P.op("vector", lambda e, blk=blk: e.tensor_tensor(out=affall[:, blk * 4:(blk + 1) * 4, :], in0=ee[:], in1=sm[:].unsqueeze(2).to_broadcast([128, 4, E]), op=ALU.mult), [ee.b, sm.b], [affall.b])
                P.dma("gpsimd", lambda e, blk=blk: e.dma_start(out=AFS[blk * 128:(blk + 1) * 128, :], in_=affall[:, blk * 4:(blk + 1) * 4, :].rearrange("p s e -> p (s e)")), [affall.b], AFS.b)
    _phase5()
    P.barrier()

    if cfg.stop == "B":
        P.build(final_waits=[HT.b, ADA.b, YT.b, X1.b, AFS.b] + dumps)
        return nc, P

    def _phase6():
        with (contextlib.nullcontext(top) if cfg.noscope else contextlib.ExitStack()) as st:
            mid = sb(st, "mid", [128, E])
            cmpt = sb(st, "cmpt", [128, E, NT])
            pc = sb(st, "pc", [128, E])
            ge = sb(st, "ge", [128, E])
            pc_ps = psb(st, "pcps")
            aff_v = affall[:].rearrange("p t e -> p e t")
            P.op("vector", lambda e: e.memset(thr[:], 0.0), [], [thr.b])
            for it in range(30):
                s_i = 2.0 ** -(it + 1)
                P.op("vector", lambda e, s_i=s_i: e.tensor_scalar(out=mid[:], in0=thr[:], scalar1=s_i, scalar2=None, op0=ALU.add), [thr.b], [mid.b])
                P.op("vector", lambda e: e.tensor_tensor(out=cmpt[:], in0=aff_v, in1=mid[:].unsqueeze(2).to_broadcast([128, E, NT]), op=ALU.is_gt), [affall.b, mid.b], [cmpt.b])
                P.op("vector", lambda e: e.tensor_reduce(out=pc[:], in_=cmpt[:], axis=AX.X, op=ALU.add), [cmpt.b], [pc.b])
                P.op("tensor", lambda e: e.matmul(pc_ps[:, 0:E], lhsT=cst[:, cs("ones")], rhs=pc[:], start=True, stop=True), [cst.b, pc.b], [pc_ps.b])
                P.op("vector", lambda e, s_i=s_i: e.tensor_scalar(out=ge[:], in0=pc_ps[:, 0:E], scalar1=float(cfg.CAP) - 0.5, scalar2=s_i, op0=ALU.is_gt, op1=ALU.mult), [pc_ps.b], [ge.b])
                P.op("vector", lambda e: e.tensor_tensor(out=thr[:], in0=thr[:], in1=ge[:], op=ALU.add), [thr.b, ge.b], [thr.b])
    _phase6()
    P.barrier()

    if cfg.stop == "C":
        P.build(final_waits=[HT.b, ADA.b, YT.b, X1.b, AFS.b] + dumps)
        return nc, P

    S = TG // 128
    def _phase7():
        with (contextlib.nullcontext(top) if cfg.noscope else contextlib.ExitStack()) as st:
            hfT = sb(st, "hfT", [128, KC, TG], BF16)
            acc = sb(st, "acc", [128, KC, TG])
            afo = sb(st, "afo", [128, S, E])
            msk = sb(st, "msk", [128, S, E])
            wgt = sb(st, "wgt", [128, S, E])
            wgT = sb(st, "wgT", [E, TG])
            wb = [sb(st, "wb%d" % i, [128, TG], BF16) for i in range(2)]
            gst = [sb(st, "gst%d" % i, [128, KC, 128]) for i in range(2)]
            ust = [sb(st, "ust%d" % i, [128, KC, 128]) for i in range(2)]
            gbf = [sb(st, "gbf%d" % i, [128, KC, 128], BF16) for i in range(2)]
            ubf = [sb(st, "ubf%d" % i, [128, KC, 128], BF16) for i in range(2)]
            dst_ = [sb(st, "dst%d" % i, [128, FC, 128]) for i in range(2)]
            dbf = [sb(st, "dbf%d" % i, [128, FC, 128], BF16) for i in range(2)]
            sgm = [sb(st, "sgm%d" % i, [128, TG], BF16) for i in range(2)]
            tmu = [sb(st, "tmu%d" % i, [128, TG], BF16) for i in range(2)]
            hid = sb(st, "hid", [128, FC, TG], BF16)
            sq = sb(st, "dsq", [128, KC, TG], BF16)
            rs = sb(st, "drs", [128, TG])
            pg = [psb(st, "pg%d" % i) for i in range(2)]
            pu_ = [psb(st, "pu%d" % i) for i in range(2)]
            py = [psb(st, "py%d" % i) for i in range(2)]
            pw = psb(st, "pw")
            pm = psb(st, "pm")
            X1f = X1.t.ap()
            HFf = HF.t.ap()
            AFf = AFS.t.ap()
            cnt = [0, 0]
            for g in range(NG):
                for jb in range(TG // TB):
                    col = g * (TG // TB) + jb
                    P.dma("gpsimd", lambda e, col=col, jb=jb: e.indirect_dma_start(out=hfT[:].rearrange("p k t -> p (k t)"), out_offset=None, in_=HFf,
                                                                                     in_offset=bass.IndirectOffsetOnAxis(ap=idx[:, col:col + 1], axis=0)), [HF.b, idx.b], hfT.b)
                    P.dma("gpsimd", lambda e, col=col, jb=jb: e.indirect_dma_start(out=acc[:].rearrange("p k t -> p (k t)"), out_offset=None, in_=X1f,
                                                                                     in_offset=bass.IndirectOffsetOnAxis(ap=idx[:, col:col + 1], axis=0)), [X1.b, idx.b], acc.b)
                    P.dma("gpsimd", lambda e, col=col, jb=jb: e.indirect_dma_start(out=afo[:, jb * 4:(jb + 1) * 4, :].rearrange("p s e -> p (s e)"), out_offset=None, in_=AFf,
                                                                                     in_offset=bass.IndirectOffsetOnAxis(ap=idx[:, col:col + 1], axis=0)), [AFS.b, idx.b], afo.b)
                P.op("vector", lambda e: e.tensor_tensor(out=msk[:], in0=afo[:], in1=thr[:].unsqueeze(1).to_broadcast([128, S, E]), op=ALU.is_gt), [afo.b, thr.b], [msk.b])
                P.op("vector", lambda e: e.tensor_tensor(out=wgt[:], in0=afo[:], in1=msk[:], op=ALU.mult), [afo.b, msk.b], [wgt.b])
                for s in range(S):
                    P.op("tensor", lambda e, s=s: e.matmul(pm[0:E, s * 128:(s + 1) * 128], lhsT=wgt[:, s, :], rhs=cst[:, cs("ident")], start=True, stop=True), [wgt.b, cst.b], [pm.b])
                P.op("vector", lambda e: e.tensor_copy(out=wgT[:], in_=pm[0:E, 0:TG]), [pm.b], [wgT.b])
                for ex_ in range(E):
                    wbe = wb[ex_ % 2]
                    so = lay["sel"][0] + ex_ * 128
                    P.op("tensor", lambda e, so=so: e.matmul(pw[:, 0:TG], lhsT=cst[0:E, so:so + 128], rhs=wgT[:], start=True, stop=True), [cst.b, wgT.b], [pw.b])
                    P.op("scalar", lambda e, wbe=wbe: e.activation(out=wbe[:], in_=pw[:, 0:TG], func=AF.Copy), [pw.b], [wbe.b])
                    for fb in range(FC):
                        i = cnt[0] % 2
                        cnt[0] += 1
                        gs, us, gb, ub = gst[i], ust[i], gbf[i], ubf[i]
                        P.dma("sync", lambda e, gs=gs, ex_=ex_, fb=fb: e.dma_start(out=gs[:].rearrange("p k f -> p (k f)"), in_=w_eg[(ex_ * FC + fb) * 128:(ex_ * FC + fb + 1) * 128, :]), [], gs.b)
                        P.dma("sync", lambda e, us=us, ex_=ex_, fb=fb: e.dma_start(out=us[:].rearrange("p k f -> p (k f)"), in_=w_eu[(ex_ * FC + fb) * 128:(ex_ * FC + fb + 1) * 128, :]), [], us.b)
                        P.op("gpsimd", lambda e, gs=gs, gb=gb: e.tensor_copy(out=gb[:], in_=gs[:]), [gs.b], [gb.b])
                        P.op("vector", lambda e, us=us, ub=ub: e.tensor_copy(out=ub[:], in_=us[:]), [us.b], [ub.b])
                        pgi, pui, sgi, tmi = pg[i], pu_[i], sgm[i], tmu[i]
                        for k in range(KC):
                            P.op("tensor", lambda e, k=k, gb=gb, pgi=pgi: e.matmul(pgi[:, 0:TG], lhsT=gb[:, k, :], rhs=hfT[:, k, :], start=(k == 0), stop=(k == KC - 1)), [gb.b, hfT.b], [pgi.b])
                        for k in range(KC):
                            P.op("tensor", lambda e, k=k, ub=ub, pui=pui: e.matmul(pui[:, 0:TG], lhsT=ub[:, k, :], rhs=hfT[:, k, :], start=(k == 0), stop=(k == KC - 1)), [ub.b, hfT.b], [pui.b])
                        P.op("scalar", lambda e, sgi=sgi, pgi=pgi: e.activation(out=sgi[:], in_=pgi[:, 0:TG], func=AF.Silu), [pgi.b], [sgi.b])
                        P.op("vector", lambda e, sgi=sgi, pui=pui, tmi=tmi: e.tensor_tensor(out=tmi[:], in0=pui[:, 0:TG], in1=sgi[:], op=ALU.mult), [pui.b, sgi.b], [tmi.b])
                        P.op("gpsimd", lambda e, tmi=tmi, fb=fb, wbe=wbe: e.tensor_tensor(out=hid[:, fb, :], in0=tmi[:], in1=wbe[:], op=ALU.mult), [tmi.b, wbe.b], [hid.b])
                    for db in range(KC):
                        i = cnt[1] % 2
                        cnt[1] += 1
                        ds_, dbb, pyi = dst_[i], dbf[i], py[i]
                        P.dma("sync", lambda e, ds_=ds_, ex_=ex_, db=db: e.dma_start(out=ds_[:].rearrange("p c d -> p (c d)"), in_=w_ed[(ex_ * KC + db) * 128:(ex_ * KC + db + 1) * 128, :]), [], ds_.b)
                        P.op("gpsimd", lambda e, ds_=ds_, dbb=dbb: e.tensor_copy(out=dbb[:], in_=ds_[:]), [ds_.b], [dbb.b])
                        for fc in range(FC):
                            P.op("tensor", lambda e, fc=fc, dbb=dbb, pyi=pyi: e.matmul(pyi[:, 0:TG], lhsT=dbb[:, fc, :], rhs=hid[:, fc, :], start=(fc == 0), stop=(fc == FC - 1)), [dbb.b, hid.b], [pyi.b])
                        P.op("vector", lambda e, db=db, pyi=pyi: e.scalar_tensor_tensor(out=acc[:, db, :], in0=pyi[:, 0:TG], scalar=mods[:, 80 + db:81 + db], in1=acc[:, db, :], op0=ALU.mult, op1=ALU.add),
                             [pyi.b, mods.b, acc.b], [acc.b])
                P.op("gpsimd", lambda e: e.tensor_tensor(out=sq[:], in0=acc[:], in1=acc[:], op=ALU.mult), [acc.b], [sq.b])
                rstd_from_sq(pw, sq, KC, TG, rs, 1.0 / D)
                P.op("vector", lambda e: e.tensor_tensor(out=acc[:], in0=acc[:], in1=rs[:].unsqueeze(1).to_broadcast([128, KC, TG]), op=ALU.mult), [acc.b, rs.b], [acc.b])
                P.op("gpsimd", lambda e: e.tensor_tensor(out=acc[:], in0=acc[:], in1=ncl[:, 32:48].unsqueeze(2).to_broadcast([128, KC, TG]), op=ALU.mult), [acc.b, ncl.b], [acc.b])
                P.dma("sync", lambda e, g=g: e.dma_start(out=outT.t.ap().rearrange("(k p) t -> p k t", p=128)[:, :, g * TG:(g + 1) * TG], in_=acc[:]), [acc.b], outT.b)
    _phase7()

    finals = [outT.b]
    if cfg.dev:
        finals += [YT.b, X1.b, AFS.b, ADA.b, HT.b] + dumps
    P.build(final_waits=finals)
    top.close()
    return nc, P


def host_inputs(cfg, core, x, c, ctx, c_ctx, w_ada, b_ada, norm1_g, w_in, w_a2_f, b_a2_f, w_a2_b, b_a2_b,
                gla_norm_g, w_out, norm2_g, w_router, w_e_gate, w_e_up, w_e_down, final_norm_g, shared):
    b, r = core // 4, core % 4
    f = lambda a: np.ascontiguousarray(np.asarray(a, dtype=np.float32))
    col = lambda v: f(np.asarray(v).reshape(-1, 128).T)
    m = {}
    m["xT"] = shared["xT"][b]
    m["ctxT"] = shared["ctxT"][b]
    m["cond"] = f(np.concatenate([col(c[b]), col(c_ctx)], axis=1))
    m["w_ada"] = shared["w_ada"]
    m["b_ada"] = shared["b_ada"]
    m["ncols"] = shared["ncols"]
    m["glag"] = shared["glag"]
    m["w_in"] = shared["w_in"]
    m["wa2"] = shared["wa2"]
    m["ba2"] = shared["ba2"]
    m["w_out"] = shared["w_out"]
    m["w_router"] = shared["w_router"]
    m["w_eg"] = shared["w_eg"]
    m["w_eu"] = shared["w_eu"]
    m["w_ed"] = shared["w_ed"]
    m["cst"] = shared["cst"]
    ob0 = r * cfg.NOB
    m["idx"] = np.stack([(ob0 + j) * 128 + np.arange(128) for j in range(cfg.NOB)], axis=1).astype(np.int32)
    return m


def host_shared(cfg, x, c, ctx, c_ctx, w_ada, b_ada, norm1_g, w_in, w_a2_f, b_a2_f, w_a2_b, b_a2_b,
                gla_norm_g, w_out, norm2_g, w_router, w_e_gate, w_e_up, w_e_down, final_norm_g, batches=(0, 1)):
    f = lambda a: np.ascontiguousarray(np.asarray(a, dtype=np.float32))
    col = lambda v: f(np.asarray(v).reshape(-1, 128).T)
    sh = {}
    sh["xT"] = {b: f(np.asarray(x[b]).T) for b in batches}
    sh["ctxT"] = {b: f(np.asarray(ctx[b]).T) for b in batches}
    sh["w_ada"] = f(w_ada[0])
    sh["b_ada"] = f(b_ada[0]).reshape(1, -1)
    sh["ncols"] = f(np.concatenate([col(norm1_g[0]), col(norm2_g[0]), col(final_norm_g)], axis=1))
    sh["glag"] = col(gla_norm_g[0])
    sh["w_in"] = f(w_in[0])
    sh["wa2"] = f(np.concatenate([np.asarray(w_a2_f[0]), np.asarray(w_a2_b[0])], axis=1))
    sh["ba2"] = f(np.concatenate([col(b_a2_f[0]), col(b_a2_b[0])], axis=1))
    sh["w_out"] = f(w_out[0])
    sh["w_router"] = f(w_router[0])
    E_, FC_ = cfg.E, cfg.FC
    slab = lambda w: np.ascontiguousarray(np.asarray(w, dtype=np.float32).reshape(E_, KC, 128, FC_, 128).transpose(0, 3, 2, 1, 4)).reshape(E_ * FC_ * 128, KC * 128)
    sh["w_eg"] = slab(w_e_gate[0])
    sh["w_eu"] = slab(w_e_up[0])
    sh["w_ed"] = np.ascontiguousarray(np.asarray(w_e_down[0], dtype=np.float32).reshape(E_, FC_, 128, KC, 128).transpose(0, 3, 2, 1, 4)).reshape(E_ * KC * 128, FC_ * 128)
    sh["cst"] = make_consts(cfg)
    return sh


def kernel(**inputs):
    cfg = Cfg()
    nc, _ = build(cfg)
    sh = host_shared(cfg, **inputs)
    in_maps = [host_inputs(cfg, core, shared=sh, **inputs) for core in range(8)]
    res = run_bass_kernel_spmd(nc, in_maps, core_ids=list(range(8)))
    out = np.empty((2, cfg.L, D), np.float32)
    for core in range(8):
        b, r = core // 4, core % 4
        out[b, r * cfg.OWN:(r + 1) * cfg.OWN, :] = np.asarray(res.results[core]["outT"]).T
    return out
```

```python
import contextlib
import numpy as np
import concourse.bass as bass
import concourse.mybir as mybir
from concourse.bass_utils import run_bass_kernel_spmd

F32 = mybir.dt.float32
BF16 = mybir.dt.bfloat16
I32 = mybir.dt.int32
ALU = mybir.AluOpType
AF = mybir.ActivationFunctionType
AX = mybir.AxisListType

D = 2048
KC = 16
TB = 512
EPS = 1e-6
ENGS = ("sync", "scalar", "vector", "gpsimd", "tensor")


class Buf:
    __slots__ = ("name", "last_w", "readers", "dma_sem", "dma_cnt", "dma_writers")

    def __init__(self, name):
        self.name = name
        self.last_w = None
        self.readers = {}
        self.dma_sem = None
        self.dma_cnt = 0
        self.dma_writers = False


class Prog:
    def __init__(self, nc):
        self.nc = nc
        self.ins = []
        self.last_on = {e: None for e in ENGS}
        self.dma_bufs = []
        self.pending = {e: None for e in ENGS}

    def _emit(self, eng, fn, reads, writes, dma=False):
        iid = len(self.ins)
        deps = set()
        for b in reads:
            if b.dma_writers:
                deps.add(("dma", b, b.dma_cnt))
            elif b.last_w is not None:
                deps.add(("ins", b.last_w))
        for b in writes:
            if b.dma_writers:
                deps.add(("dma", b, b.dma_cnt))
            elif b.last_w is not None:
                deps.add(("ins", b.last_w))
            for r in b.readers.values():
                deps.add(("ins", r))
        if self.pending[eng] is not None:
            deps |= self.pending[eng]
            self.pending[eng] = None
        rec = dict(eng=eng, fn=fn, deps=deps, dma=dma, dst=None, signal=False, cnt=None)
        if dma:
            d = writes[0]
            rec["dst"] = d
            d.dma_cnt += 1
            rec["cnt"] = d.dma_cnt
            d.dma_writers = True
            d.last_w = None
            d.readers = {}
            if d not in self.dma_bufs:
                self.dma_bufs.append(d)
        else:
            for b in writes:
                b.last_w = iid
                b.dma_writers = False
                b.readers = {}
            self.last_on[eng] = iid
        rkey = ("dma", id(writes[0])) if dma else eng
        for b in reads:
            if b not in writes:
                b.readers[rkey] = iid
        self.ins.append(rec)
        return iid

    def op(self, eng, fn, reads=(), writes=()):
        return self._emit(eng, fn, list(reads), list(writes))

    def dma(self, eng, fn, reads, write):
        return self._emit(eng, fn, list(reads), [write], dma=True)

    def barrier(self):
        deps = set()
        for e in ENGS:
            if self.last_on[e] is not None:
                deps.add(("ins", self.last_on[e]))
        for b in self.dma_bufs:
            deps.add(("dma", b, b.dma_cnt))
        for e in ENGS:
            self.pending[e] = set(deps) | (self.pending[e] or set())

    def build(self, final_waits=()):
        nc = self.nc
        ins = self.ins
        for rec in ins:
            for d in rec["deps"]:
                if d[0] == "ins":
                    p = ins[d[1]]
                    if p["dma"]:
                        continue
                    if p["eng"] == "tensor" and rec["eng"] == "tensor":
                        continue
                    p["signal"] = True
        cnt = {e: 0 for e in ENGS}
        for rec in ins:
            if not rec["dma"] and rec["signal"]:
                cnt[rec["eng"]] += 1
                rec["cnt"] = cnt[rec["eng"]]
        self.cnt = cnt
        with contextlib.ExitStack() as st:
            esem = {e: st.enter_context(nc.semaphore("s_" + e)) for e in ENGS}
            for i, b in enumerate(self.dma_bufs):
                b.dma_sem = st.enter_context(nc.semaphore("d%d" % i))
            block = st.enter_context(nc.Block())
            per = {e: [r for r in ins if r["eng"] == e] for e in ENGS}

            def run(engname, eng):
                known = {}
                for rec in per[engname]:
                    waits = {}
                    for d in rec["deps"]:
                        if d[0] == "dma":
                            s, v = d[1].dma_sem, 16 * d[2]
                        else:
                            p = ins[d[1]]
                            if p["dma"]:
                                s, v = p["dst"].dma_sem, 16 * p["cnt"]
                            else:
                                if p["eng"] == "tensor" and engname == "tensor":
                                    continue
                                s, v = esem[p["eng"]], p["cnt"]
                        if known.get(id(s), 0) >= v:
                            continue
                        if waits.get(id(s), (None, 0))[1] < v:
                            waits[id(s)] = (s, v)
                    for s, v in waits.values():
                        eng.wait_ge(s, v)
                        known[id(s)] = v
                    r = rec["fn"](eng)
                    if rec["dma"]:
                        r.then_inc(rec["dst"].dma_sem, 16)
                    elif rec["signal"]:
                        r.then_inc(esem[engname], 1)
                if engname == "sync":
                    for b in final_waits:
                        eng.wait_ge(b.dma_sem, 16 * b.dma_cnt)

            @block.sync
            def _(e):
                run("sync", e)

            @block.scalar
            def _(e):
                run("scalar", e)

            @block.vector
            def _(e):
                run("vector", e)

            @block.gpsimd
            def _(e):
                run("gpsimd", e)

            @block.tensor
            def _(e):
                run("tensor", e)


class T:
    def __init__(self, t, name):
        self.t = t
        self.b = Buf(name)

    def __getitem__(self, k):
        return self.t[k]


class Cfg:
    def __init__(self, L=16384, CTX=256, E=16, DE=1024, dev=False):
        self.L, self.CTX, self.E, self.DE, self.dev = L, CTX, E, DE, dev
        self.stop = None
        self.noscope = False
        self.T1 = L // 128
        self.NB = L // TB
        self.OWN = L // 4
        self.NOB = self.OWN // TB
        self.TG = TB
        self.NG = self.OWN // self.TG
        self.FC = DE // 128
        self.CAP = 2 * L // E
        self.PW = 4128


def const_layout(cfg):
    lay = {}
    off = 0
    for name, w in (("ident", 128), ("ones", 128), ("maskf", 128), ("maskb", 128), ("c128", 128),
                    ("s128", 128), ("reset", TB), ("twc", cfg.T1), ("tws", cfg.T1),
                    ("f1a", 2 * cfg.T1), ("f1b", 2 * cfg.T1), ("sel", cfg.E * 128)):
        lay[name] = (off, w)
        off += w
    return lay, off


def make_consts(cfg):
    lay, ncol = const_layout(cfg)
    c = np.zeros((128, ncol), np.float64)

    def put(name, arr):
        o, w = lay[name]
        c[:arr.shape[0], o:o + w] = arr

    a = np.arange(128)
    put("ident", np.eye(128))
    put("ones", np.ones((128, 128)))
    put("maskf", (a[:, None] <= a[None, :]).astype(np.float64))
    put("maskb", (a[:, None] >= a[None, :]).astype(np.float64))
    ang = 2 * np.pi * np.outer(a, a) / 128.0
    put("c128", np.cos(ang))
    put("s128", np.sin(ang))
    r = np.ones((128, TB))
    r[:, ::128] = 0.0
    put("reset", r)
    T1, L = cfg.T1, cfg.L
    k1 = np.arange(T1)
    nrm = 1.0 / np.sqrt(L * 128.0)
    angt = 2 * np.pi * np.outer(a, k1) / L
    put("twc", np.cos(angt) * nrm)
    put("tws", np.sin(angt) * nrm)
    ang1 = 2 * np.pi * np.outer(k1, k1) / T1
    put("f1a", np.concatenate([np.cos(ang1), -np.sin(ang1)], axis=1))
    put("f1b", np.concatenate([-np.sin(ang1), -np.cos(ang1)], axis=1))
    sel = np.zeros((cfg.E, cfg.E * 128))
    for e in range(cfg.E):
        sel[e, e * 128:(e + 1) * 128] = 1.0
    put("sel", sel)
    return c.astype(np.float32)


def build(cfg):
    L, CTX, E, DE, T1, NB, OWN, NOB = cfg.L, cfg.CTX, cfg.E, cfg.DE, cfg.T1, cfg.NB, cfg.OWN, cfg.NOB
    TG, NG, FC, PW = cfg.TG, cfg.NG, cfg.FC, cfg.PW
    lay, ncol = const_layout(cfg)
    nc = bass.Bass("TRN2", target_bir_lowering=False)
    P = Prog(nc)

    def din(name, shape, dt=F32):
        return nc.dram_tensor(name, list(shape), dt, kind="ExternalInput")

    dbg = "ExternalOutput" if cfg.dev else None

    def dscr(name, shape, dt, out=False):
        if out and dbg:
            return T(nc.dram_tensor(name, list(shape), dt, kind=dbg), name)
        return T(nc.dram_tensor(name, list(shape), dt), name)

    dumps = []

    def dump(name, tl, shape, dt=F32):
        if not cfg.dev:
            return
        dd = T(nc.dram_tensor("dbg_" + name, list(shape), dt, kind="ExternalOutput"), "dbg_" + name)
        P.dma("gpsimd", lambda e: e.dma_start(out=dd.t.ap(), in_=tl[:]), [tl.b], dd.b)
        dumps.append(dd.b)

    xT = din("xT", [D, L])
    ctxT = din("ctxT", [D, CTX])
    cond = din("cond", [128, 32])
    w_ada = din("w_ada", [D, 6 * D])
    b_ada = din("b_ada", [1, 6 * D])
    ncols = din("ncols", [128, 48])
    glag = din("glag", [128, 8])
    w_in = din("w_in", [D, PW])
    wa2 = din("wa2", [16, 1024])
    ba2 = din("ba2", [128, 8])
    w_out = din("w_out", [D, D])
    w_router = din("w_router", [D, E])
    w_eg = din("w_eg", [E * FC * 128, KC * 128])
    w_eu = din("w_eu", [E * FC * 128, KC * 128])
    w_ed = din("w_ed", [E * KC * 128, FC * 128])
    cst_d = din("cst", [128, ncol])
    idx_d = din("idx", [128, NOB], I32)
    outT = T(nc.dram_tensor("outT", [D, OWN], F32, kind="ExternalOutput"), "outT")

    ADA = dscr("ADA", [2, 6 * D], F32, out=True)
    HT = dscr("HT", [NB * 128, KC * TB], BF16, out=True)
    OB = dscr("OB", [NB * 128, 2 * TB], F32)
    QS = dscr("QS", [NB * 128, TB], BF16)
    KS = dscr("KS", [NB * 128, TB], BF16)
    VS = dscr("VS", [NB * 128, 4 * 256], BF16)
    YT = dscr("YT", [NB * 128, KC * TB], BF16, out=True)
    FA = dscr("FA", [128, L], BF16)
    FB = dscr("FB", [128, L], BF16)
    X1 = dscr("X1", [NB * 128, KC * TB], F32, out=True)
    HF = dscr("HF", [NB * 128, KC * TB], BF16, out=True)
    AFS = dscr("AFS", [NB * 128, 4 * E], F32, out=True)

    xT_v = xT.ap().rearrange("(k p) t -> p k t", p=128)
    ctxT_v = ctxT.ap().rearrange("(k p) t -> p k t", p=128)
    w_in_v = w_in.ap().rearrange("(k p) n -> p k n", p=128)
    w_out_v = w_out.ap().rearrange("(k p) n -> p k n", p=128)
    w_ada_v = w_ada.ap().rearrange("(k p) n -> p k n", p=128)
    wr_v = w_router.ap().rearrange("(k p) n -> p k n", p=128)

    top = contextlib.ExitStack()

    def sb(st, name, shape, dt=F32):
        return T(st.enter_context(nc.sbuf_tensor("sb_" + name, list(shape), dt)), name)

    def psb(st, name, dt=F32, n=512):
        return T(st.enter_context(nc.psum_tensor("ps_" + name, [128, n], dt)), name)

    def cs(name):
        o, w = lay[name]
        return slice(o, o + w)

    cst = sb(top, "cst", [128, ncol])
    P.dma("sync", lambda e: e.dma_start(out=cst[:], in_=cst_d[:, :]), [], cst.b)
    cbf = sb(top, "cbf", [128, 128 * 4 + 4 * T1], BF16)
    CB = {"ones": slice(0, 128), "c128": slice(128, 256), "s128": slice(256, 384), "ident": slice(384, 512),
          "f1a": slice(512, 512 + 2 * T1), "f1b": slice(512 + 2 * T1, 512 + 4 * T1)}
    for nm in ("ones", "c128", "s128", "ident", "f1a", "f1b"):
        P.op("vector", lambda e, nm=nm: e.tensor_copy(out=cbf[:, CB[nm]], in_=cst[:, cs(nm)]), [cst.b], [cbf.b])
    mods = sb(top, "mods", [128, 96])
    modc = sb(top, "modc", [128, 32])
    ncl = sb(top, "ncl", [128, 48])
    glg = sb(top, "glg", [128, 8])
    nb2 = sb(top, "nb2", [128, 8])
    wa2f = sb(top, "wa2f", [16, 1024])
    wa2b = sb(top, "wa2b", [16, 1024], BF16)
    G1 = sb(top, "G1", [128, 16])
    G1c = sb(top, "G1c", [128, 16])
    G2 = sb(top, "G2", [128, 16])
    idx = sb(top, "idx", [128, NOB], I32)
    P.dma("sync", lambda e: e.dma_start(out=ncl[:], in_=ncols[:, :]), [], ncl.b)
    P.dma("sync", lambda e: e.dma_start(out=glg[:], in_=glag[:, :]), [], glg.b)
    P.dma("sync", lambda e: e.dma_start(out=nb2[:], in_=ba2[:, :]), [], nb2.b)
    P.dma("sync", lambda e: e.dma_start(out=wa2f[:], in_=wa2[:, :]), [], wa2f.b)
    P.dma("sync", lambda e: e.dma_start(out=idx[:], in_=idx_d[:, :]), [], idx.b)
    P.op("vector", lambda e: e.tensor_scalar(out=nb2[:], in0=nb2[:], scalar1=-1.0, scalar2=None, op0=ALU.mult), [nb2.b], [nb2.b])
    P.op("vector", lambda e: e.tensor_copy(out=wa2b[:], in_=wa2f[:]), [wa2f.b], [wa2b.b])

    def _phase1():
        with (contextlib.nullcontext(top) if cfg.noscope else contextlib.ExitStack()) as st:
            cnd = sb(st, "cnd", [128, 32])
            scd = sb(st, "scd", [128, 32])
            P.dma("sync", lambda e: e.dma_start(out=cnd[:], in_=cond[:, :]), [], cnd.b)
            P.op("scalar", lambda e: e.activation(out=scd[:], in_=cnd[:], func=AF.Silu), [cnd.b], [scd.b])
            wst = [sb(st, "wst%d" % i, [128, 4, 512]) for i in range(4)]
            pr = [psb(st, "pr%d" % i) for i in range(2)]
            rows = [sb(st, "rows%d" % i, [2, 512]) for i in range(2)]
            bad = [sb(st, "bad%d" % i, [2, 512]) for i in range(2)]
            scb = sb(st, "scb", [128, 32], BF16)
            P.op("vector", lambda e: e.tensor_copy(out=scb[:], in_=scd[:]), [scd.b], [scb.b])
            scb_v = scb[:].rearrange("p (c k) -> p c k", k=16)
            wbf = [sb(st, "wbf%d" % i, [128, 4, 512], BF16) for i in range(2)]
            wi = 0
            for n in range(24):
                pt = pr[n % 2]
                bt = bad[n % 2]
                P.dma("sync", lambda e, bt=bt, n=n: e.dma_start(out=bt[:], in_=b_ada[0:1, n * 512:(n + 1) * 512].partition_broadcast(2)), [], bt.b)
                for kq in range(4):
                    w = wst[wi % 4]
                    wi += 1
                    P.dma("sync", lambda e, w=w, n=n, kq=kq: e.dma_start(out=w[:], in_=w_ada_v[:, kq * 4:(kq + 1) * 4, n * 512:(n + 1) * 512]), [], w.b)
                    wb_ = wbf[wi % 2]
                    P.op(("vector", "gpsimd")[wi % 2], lambda e, w=w, wb_=wb_: e.tensor_copy(out=wb_[:], in_=w[:]), [w.b], [wb_.b])
                    for kk in range(4):
                        k = kq * 4 + kk
                        P.op("tensor", lambda e, pt=pt, wb_=wb_, kk=kk, k=k: e.matmul(pt[0:2, :], lhsT=scb_v[:, :, k], rhs=wb_[:, kk, :], start=(k == 0), stop=(k == 15)),
                             [scb.b, wb_.b], [pt.b])
                rw = rows[n % 2]
                P.op("vector", lambda e, rw=rw, pt=pt, bt=bt: e.tensor_tensor(out=rw[:], in0=pt[0:2, :], in1=bt[:], op=ALU.add), [pt.b, bt.b], [rw.b])
                P.dma("gpsimd", lambda e, rw=rw, n=n: e.dma_start(out=ADA[0:2, n * 512:(n + 1) * 512], in_=rw[:]), [rw.b], ADA.b)
            P.dma("sync", lambda e: e.dma_start(out=mods[:], in_=ADA[0:1, :].rearrange("o (j p) -> p (o j)", p=128), allow_slow_non_contiguous=True), [ADA.b], mods.b)
            P.dma("sync", lambda e: e.dma_start(out=modc[:], in_=ADA[1:2, 0:4096].rearrange("o (j p) -> p (o j)", p=128), allow_slow_non_contiguous=True), [ADA.b], modc.b)
            P.op("vector", lambda e: e.scalar_tensor_tensor(out=G1[:], in0=mods[:, 16:32], scalar=1.0, in1=ncl[:, 0:16], op0=ALU.add, op1=ALU.mult), [mods.b, ncl.b], [G1.b])
            P.op("vector", lambda e: e.scalar_tensor_tensor(out=G1c[:], in0=modc[:, 16:32], scalar=1.0, in1=ncl[:, 0:16], op0=ALU.add, op1=ALU.mult), [modc.b, ncl.b], [G1c.b])
            P.op("vector", lambda e: e.scalar_tensor_tensor(out=G2[:], in0=mods[:, 64:80], scalar=1.0, in1=ncl[:, 16:32], op0=ALU.add, op1=ALU.mult), [mods.b, ncl.b], [G2.b])
    _phase1()
    dump('mods', mods, [128, 96])
    dump('modc', modc, [128, 32])
    dump('G1', G1, [128, 16])
    P.barrier()

    def rstd_from_sq(pbank, sq, nk, n, dst, inv_dim):
        for k in range(nk):
            P.op("tensor", lambda e, k=k: e.matmul(pbank[:, 0:n], lhsT=cbf[:, CB["ones"]], rhs=sq[:, k, 0:n], start=(k == 0), stop=(k == nk - 1)),
                 [cbf.b, sq.b], [pbank.b])
        P.op("scalar", lambda e: e.activation(out=dst[:, 0:n], in_=pbank[:, 0:n], func=AF.Ln, scale=inv_dim, bias=EPS), [pbank.b], [dst.b])
        P.op("scalar", lambda e: e.activation(out=dst[:, 0:n], in_=dst[:, 0:n], func=AF.Exp, scale=-0.5), [dst.b], [dst.b])

    hc = sb(top, "hc", [128, KC, CTX], BF16)
    def _phase2():
        with (contextlib.nullcontext(top) if cfg.noscope else contextlib.ExitStack()) as st:
            xs = [sb(st, "xs%d" % i, [128, KC, TB]) for i in range(2)] if not cfg.noscope else [sb(st, "xs0", [128, KC, TB])] * 2
            sq = sb(st, "sq", [128, KC, TB], BF16)
            rs = sb(st, "rs", [128, TB])
            tm = sb(st, "tm", [128, KC, TB])
            hts = [sb(st, "hts%d" % i, [128, KC, TB], BF16) for i in range(2)]
            pb = psb(st, "a0p")

            def front(src_ap, n, xsl, Gc, SHc, dst):
                P.dma("sync", lambda e: e.dma_start(out=xsl[:, :, 0:n], in_=src_ap), [], xsl.b)
                P.op("gpsimd", lambda e: e.tensor_tensor(out=sq[:, :, 0:n], in0=xsl[:, :, 0:n], in1=xsl[:, :, 0:n], op=ALU.mult), [xsl.b], [sq.b])
                rstd_from_sq(pb, sq, KC, n, rs, 1.0 / D)
                if n == CTX:
                    dump('xsl', xsl, [128, KC, TB])
                    dump('rs', rs, [128, TB])
                    dump('sq', sq, [128, KC, TB], BF16)
                P.op("vector", lambda e: e.tensor_tensor(out=tm[:, :, 0:n], in0=xsl[:, :, 0:n], in1=rs[:, 0:n].unsqueeze(1).to_broadcast([128, KC, n]), op=ALU.mult),
                     [xsl.b, rs.b], [tm.b])
                P.op("gpsimd", lambda e: e.tensor_tensor(out=tm[:, :, 0:n], in0=tm[:, :, 0:n], in1=Gc[:, 0:16].unsqueeze(2).to_broadcast([128, KC, n]), op=ALU.mult),
                     [tm.b, Gc.b], [tm.b])
                P.op("vector", lambda e: e.tensor_tensor(out=dst[:, :, 0:n], in0=tm[:, :, 0:n], in1=SHc.unsqueeze(2).to_broadcast([128, KC, n]), op=ALU.add),
                     [tm.b, mods.b, modc.b], [dst.b])

            front(ctxT_v[:, :, :], CTX, xs[0], G1c, modc[:, 0:16], hc)
            for blk in range(NB):
                h = hts[blk % 2]
                front(xT_v[:, :, blk * TB:(blk + 1) * TB], TB, xs[(blk + 1) % 2], G1, mods[:, 0:16], h)
                P.dma("gpsimd", lambda e, h=h, blk=blk: e.dma_start(out=HT[blk * 128:(blk + 1) * 128, :], in_=h[:].rearrange("p k t -> p (k t)")), [h.b], HT.b)
    _phase2()
    P.barrier()
    if cfg.stop == "A0":
        P.build(final_waits=[HT.b, ADA.b] + dumps)
        return nc, P

    def _phase3():
        with (contextlib.nullcontext(top) if cfg.noscope else contextlib.ExitStack()) as st:
            wstg = [sb(st, "wstg%d" % i, [128, KC, 256]) for i in range(2)]
            Wq = sb(st, "Wq", [128, KC, 128], BF16)
            Wk = sb(st, "Wk", [128, KC, 128], BF16)
            Wv = sb(st, "Wv", [128, KC, 256], BF16)
            Wg = sb(st, "Wg", [128, KC, 256], BF16)
            Wz = sb(st, "Wz", [128, KC, 32], BF16)
            hb = [sb(st, "hb%d" % i, [128, KC, TB], BF16) for i in range(2)]
            zt = sb(st, "zt", [16, TB], BF16)
            ex = sb(st, "ex", [128, TB])
            sp = sb(st, "sp", [128, TB])
            cum = sb(st, "cum", [128, TB])
            ea = sb(st, "ea", [128, TB])
            eb = sb(st, "eb", [128, TB])
            ec = sb(st, "ec", [128, TB])
            dec = sb(st, "dec", [128, 4])
            qs = sb(st, "qs", [128, TB], BF16)
            ks = sb(st, "ks", [128, TB], BF16)
            qd = sb(st, "qd", [128, TB], BF16)
            ki = sb(st, "ki", [128, TB], BF16)
            kte = sb(st, "kte", [128, TB])
            kteT = sb(st, "kteT", [128, 4, 128], BF16)
            vt = sb(st, "vt", [128, 4, 256], BF16)
            sg = sb(st, "sg", [128, 2, TB], BF16)
            sT = [sb(st, "sT%d" % i, [128, 128], BF16) for i in range(4)]
            state = sb(st, "state", [128, 256])
            sbf = [sb(st, "sbf%d" % i, [128, 256], BF16) for i in range(2)]
            Sf = sb(st, "Sf", [128, 256])
            Sb = sb(st, "Sb", [128, 256])
            osb = sb(st, "osb", [128, 2, TB])
            obl = sb(st, "obl", [128, 2, TB])
            osq = sb(st, "osq", [128, 2, TB], BF16)
            ors = sb(st, "ors", [128, TB])
            yt = sb(st, "yt", [128, 2, TB], BF16)
            B0, B1, B2, B3, B4, B5, B6, B7 = [psb(st, "gp%d" % i) for i in range(8)]
            b4v = Buf("b4v"); b4kv = Buf("b4kv"); b5s = Buf("b5s"); b5t = Buf("b5t")

            def load_w(dst, c0, n):
                w = wstg[load_w.i % 2]
                load_w.i += 1
                P.dma("sync", lambda e: e.dma_start(out=w[:, :, 0:n], in_=w_in_v[:, :, c0:c0 + n]), [], w.b)
                P.op("vector", lambda e: e.tensor_copy(out=dst[:, :, 0:n], in_=w[:, :, 0:n]), [w.b], [dst.b])
            load_w.i = 0

            def gla_block(h, hsrc, hbuf, n, direction, emit_out, blk):
                nch = n // 128
                dcol = 0 if direction == "f" else 1
                use_cache = emit_out and direction == "f"
                if not use_cache:
                    for k in range(KC):
                        P.op("tensor", lambda e, k=k: e.matmul(B0[:, 0:n], lhsT=Wq[:, k, :], rhs=hsrc[:, k, 0:n], start=(k == 0), stop=(k == KC - 1)), [Wq.b, hbuf], [B0.b])
                    for k in range(KC):
                        P.op("tensor", lambda e, k=k: e.matmul(B1[:, 0:n], lhsT=Wk[:, k, :], rhs=hsrc[:, k, 0:n], start=(k == 0), stop=(k == KC - 1)), [Wk.b, hbuf], [B1.b])
                    qsrc, qsb, ksrc, ksb = B0[:, 0:n], B0.b, B1[:, 0:n], B1.b
                    if emit_out:
                        P.op("scalar", lambda e: e.activation(out=qs[:], in_=B0[:, :], func=AF.Copy), [B0.b], [qs.b])
                        P.op("scalar", lambda e: e.activation(out=ks[:], in_=B1[:, :], func=AF.Copy), [B1.b], [ks.b])
                        P.dma("gpsimd", lambda e: e.dma_start(out=QS[blk * 128:(blk + 1) * 128, :], in_=qs[:]), [qs.b], QS.b)
                        P.dma("gpsimd", lambda e: e.dma_start(out=KS[blk * 128:(blk + 1) * 128, :], in_=ks[:]), [ks.b], KS.b)
                else:
                    P.dma("sync", lambda e: e.dma_start(out=qs[:], in_=QS[blk * 128:(blk + 1) * 128, :]), [QS.b], qs.b)
                    P.dma("sync", lambda e: e.dma_start(out=ks[:], in_=KS[blk * 128:(blk + 1) * 128, :]), [KS.b], ks.b)
                    P.dma("sync", lambda e: e.dma_start(out=vt[:].rearrange("p c v -> p (c v)"), in_=VS[blk * 128:(blk + 1) * 128, :]), [VS.b], vt.b)
                    qsrc, qsb, ksrc, ksb = qs[:, 0:n], qs.b, ks[:, 0:n], ks.b
                for k in range(KC):
                    P.op("tensor", lambda e, k=k: e.matmul(B2[0:16, 0:n], lhsT=Wz[:, k, dcol * 16:(dcol + 1) * 16], rhs=hsrc[:, k, 0:n], start=(k == 0), stop=(k == KC - 1)), [Wz.b, hbuf], [B2.b])
                P.op("vector", lambda e: e.tensor_copy(out=zt[:, 0:n], in_=B2[0:16, 0:n]), [B2.b], [zt.b])
                P.op("tensor", lambda e: e.matmul(B3[:, 0:n], lhsT=wa2b[:, dcol * 512 + h * 128:dcol * 512 + (h + 1) * 128], rhs=zt[:, 0:n], start=True, stop=True), [wa2b.b, zt.b], [B3.b])
                P.op("scalar", lambda e: e.activation(out=ex[:, 0:n], in_=B3[:, 0:n], func=AF.Exp, scale=-1.0, bias=nb2[:, dcol * 4 + h:dcol * 4 + h + 1]), [B3.b, nb2.b], [ex.b])
                P.op("scalar", lambda e: e.activation(out=sp[:, 0:n], in_=ex[:, 0:n], func=AF.Ln, bias=1.0), [ex.b], [sp.b])
                P.op("vector", lambda e: e.tensor_tensor_scan(out=cum[:, 0:n], data0=cst[:, lay["reset"][0]:lay["reset"][0] + n], data1=sp[:, 0:n], initial=0.0, op0=ALU.mult, op1=ALU.add),
                     [cst.b, sp.b], [cum.b])
                cum3 = cum[:, 0:n].rearrange("p (c j) -> p c j", j=128)
                tot = cum3[:, :, 127:128]
                P.op("scalar", lambda e: e.activation(out=dec[:, 0:nch], in_=cum3[:, :, 127], func=AF.Exp, scale=-1.0 / 16), [cum.b], [dec.b])
                if direction == "b":
                    P.op("vector", lambda e: e.tensor_tensor(out=ec[:, 0:n], in0=sp[:, 0:n], in1=cum[:, 0:n], op=ALU.subtract), [sp.b, cum.b], [ec.b])
                    ec3 = ec[:, 0:n].rearrange("p (c j) -> p c j", j=128)
                    P.op("vector", lambda e: e.tensor_tensor(out=ec3, in0=ec3, in1=tot.to_broadcast([128, nch, 128]), op=ALU.add), [ec.b, cum.b], [ec.b])
                    cdir = ec
                else:
                    cdir = cum
                P.op("scalar", lambda e: e.activation(out=ea[:, 0:n], in_=cdir[:, 0:n], func=AF.Exp, scale=-1.0 / 16), [cdir.b], [ea.b])
                P.op("scalar", lambda e: e.activation(out=eb[:, 0:n], in_=cdir[:, 0:n], func=AF.Exp, scale=1.0 / 16), [cdir.b], [eb.b])
                P.op("vector", lambda e: e.scalar_tensor_tensor(out=qd[:, 0:n], in0=qsrc, scalar=128.0 ** -0.5, in1=ea[:, 0:n], op0=ALU.mult, op1=ALU.mult), [qsb, ea.b], [qd.b])
                P.op("vector", lambda e: e.tensor_tensor(out=ki[:, 0:n], in0=ksrc, in1=eb[:, 0:n], op=ALU.mult), [ksb, eb.b], [ki.b])
                ea3 = ea[:, 0:n].rearrange("p (c j) -> p c j", j=128)
                cd3 = cdir[:, 0:n].rearrange("p (c j) -> p c j", j=128)
                P.op("vector", lambda e: e.tensor_tensor(out=ea3, in0=cd3, in1=tot.to_broadcast([128, nch, 128]), op=ALU.subtract), [cdir.b, cum.b, qd.b], [ea.b])
                P.op("scalar", lambda e: e.activation(out=ea[:, 0:n], in_=ea[:, 0:n], func=AF.Exp, scale=1.0 / 16), [ea.b], [ea.b])
                P.op("vector", lambda e: e.tensor_tensor(out=kte[:, 0:n], in0=ksrc, in1=ea[:, 0:n], op=ALU.mult), [ksb, ea.b], [kte.b])
                for c in range(nch):
                    if not use_cache:
                        for k in range(KC):
                            P.op("tensor", lambda e, k=k, c=c: e.matmul(B4[:, 0:256], lhsT=hsrc[:, k, c * 128:(c + 1) * 128], rhs=Wv[:, k, :], start=(k == 0), stop=(k == KC - 1)), [Wv.b, hbuf], [b4v])
                        P.op("scalar", lambda e, c=c: e.activation(out=vt[:, c, :], in_=B4[:, 0:256], func=AF.Copy), [b4v], [vt.b])
                    P.op("tensor", lambda e, c=c: e.transpose(B5[:, 128:256], kte[:, c * 128:(c + 1) * 128], cst[:, cs("ident")]), [kte.b, cst.b], [b5t])
                    P.op("vector", lambda e, c=c: e.tensor_copy(out=kteT[:, c, :], in_=B5[:, 128:256]), [b5t], [kteT.b])
                if emit_out and direction == "b":
                    P.dma("gpsimd", lambda e: e.dma_start(out=VS[blk * 128:(blk + 1) * 128, :], in_=vt[:].rearrange("p c v -> p (c v)")), [vt.b], VS.b)
                if emit_out and direction == "f":
                    for vb in range(2):
                        Bg = (B0, B1)[vb]
                        for k in range(KC):
                            P.op("tensor", lambda e, k=k, vb=vb, Bg=Bg: e.matmul(Bg[:, 0:n], lhsT=Wg[:, k, vb * 128:(vb + 1) * 128], rhs=hsrc[:, k, 0:n], start=(k == 0), stop=(k == KC - 1)), [Wg.b, hbuf], [Bg.b])
                        P.op("scalar", lambda e, vb=vb, Bg=Bg: e.activation(out=sg[:, vb, 0:n], in_=Bg[:, 0:n], func=AF.Silu), [Bg.b], [sg.b])
                order = range(nch) if direction == "f" else range(nch - 1, -1, -1)
                mask = cst[:, cs("maskf")] if direction == "f" else cst[:, cs("maskb")]
                if emit_out:
                    for c in range(nch):
                        csl = slice(c * 128, (c + 1) * 128)
                        P.op("tensor", lambda e, csl=csl: e.matmul(B3[:, csl], lhsT=ki[:, csl], rhs=qd[:, csl], start=True, stop=True), [ki.b, qd.b], [B3.b])
                    for c in range(nch):
                        csl = slice(c * 128, (c + 1) * 128)
                        P.op("vector", lambda e, c=c, csl=csl: e.tensor_tensor(out=sT[c][:], in0=B3[:, csl], in1=mask, op=ALU.mult), [B3.b, cst.b], [sT[c].b])
                for c in order:
                    csl = slice(c * 128, (c + 1) * 128)
                    P.op("tensor", lambda e, c=c: e.matmul(B4[:, 256:512], lhsT=kteT[:, c, :], rhs=vt[:, c, :], start=True, stop=True), [kteT.b, vt.b], [b4kv])
                    if emit_out:
                        s_ = sT[c]
                        for vb in range(2):
                            Bo = (B6, B7)[vb]
                            P.op("tensor", lambda e, vb=vb, c=c, csl=csl, s_=s_, Bo=Bo: e.matmul(Bo[:, csl], lhsT=vt[:, c, vb * 128:(vb + 1) * 128], rhs=s_[:], start=True, stop=False), [vt.b, s_.b], [Bo.b])
                            P.op("tensor", lambda e, vb=vb, csl=csl, Bo=Bo, cur=gla_block.cur: e.matmul(Bo[:, csl], lhsT=cur[:, vb * 128:(vb + 1) * 128], rhs=qd[:, csl], start=False, stop=True), [gla_block.cur.b, qd.b], [Bo.b])
                    P.op("vector", lambda e, c=c: e.scalar_tensor_tensor(out=state[:], in0=state[:], scalar=dec[:, c:c + 1], in1=B4[:, 256:512], op0=ALU.mult, op1=ALU.add), [state.b, dec.b, b4kv], [state.b])
                    nxt = sbf[(gla_block.ci + 1) % 2]
                    gla_block.ci += 1
                    P.op("scalar", lambda e, nxt=nxt: e.activation(out=nxt[:], in_=state[:], func=AF.Copy), [state.b], [nxt.b])
                    gla_block.cur = nxt
                if emit_out:
                    if direction == "b":
                        P.op("vector", lambda e: e.tensor_copy(out=osb[:, 0, :], in_=B6[:, :]), [B6.b], [osb.b])
                        P.op("vector", lambda e: e.tensor_copy(out=osb[:, 1, :], in_=B7[:, :]), [B7.b], [osb.b])
                        P.dma("gpsimd", lambda e: e.dma_start(out=OB[blk * 128:(blk + 1) * 128, :], in_=osb[:].rearrange("p a t -> p (a t)")), [osb.b], OB.b)
                    else:
                        P.dma("sync", lambda e: e.dma_start(out=obl[:].rearrange("p a t -> p (a t)"), in_=OB[blk * 128:(blk + 1) * 128, :]), [OB.b], obl.b)
                        P.op("vector", lambda e: e.tensor_tensor(out=osb[:, 0, :], in0=B6[:, :], in1=obl[:, 0, :], op=ALU.add), [B6.b, obl.b], [osb.b])
                        P.op("vector", lambda e: e.tensor_tensor(out=osb[:, 1, :], in0=B7[:, :], in1=obl[:, 1, :], op=ALU.add), [B7.b, obl.b], [osb.b])
                        P.op("gpsimd", lambda e: e.tensor_tensor(out=osq[:], in0=osb[:], in1=osb[:], op=ALU.mult), [osb.b], [osq.b])
                        rstd_from_sq(B2, osq, 2, TB, ors, 1.0 / 256)
                        P.op("vector", lambda e: e.tensor_tensor(out=osb[:], in0=osb[:], in1=ors[:].unsqueeze(1).to_broadcast([128, 2, TB]), op=ALU.mult), [osb.b, ors.b], [osb.b])
                        for vb in range(2):
                            P.op("vector", lambda e, vb=vb: e.scalar_tensor_tensor(out=yt[:, vb, :], in0=osb[:, vb, :], scalar=glg[:, 2 * h + vb:2 * h + vb + 1], in1=sg[:, vb, :], op0=ALU.mult, op1=ALU.mult),
                                 [osb.b, glg.b, sg.b], [yt.b])
                        P.dma("gpsimd", lambda e: e.dma_start(out=YT[blk * 128:(blk + 1) * 128, 2 * h * TB:(2 * h + 2) * TB], in_=yt[:].rearrange("p a t -> p (a t)")), [yt.b], YT.b)
            gla_block.si = 0
            gla_block.ci = 0
            gla_block.cur = sbf[0]

            def set_state(src):
                if src is None:
                    P.op("vector", lambda e: e.memset(state[:], 0.0), [], [state.b])
                else:
                    P.op("vector", lambda e: e.tensor_copy(out=state[:], in_=src[:]), [src.b], [state.b])
                nxt = sbf[(gla_block.ci + 1) % 2]
                gla_block.ci += 1
                P.op("scalar", lambda e: e.activation(out=nxt[:], in_=state[:], func=AF.Copy), [state.b], [nxt.b])
                gla_block.cur = nxt

            for h in range(4):
                load_w(Wq, h * 128, 128)
                load_w(Wk, 512 + h * 128, 128)
                load_w(Wv, 1024 + h * 256, 256)
                load_w(Wg, 2048 + h * 256, 256)
                load_w(Wz, 3072, 32)
                set_state(None)
                gla_block(h, hc, hc.b, CTX, "f", False, 0)
                P.op("vector", lambda e: e.tensor_copy(out=Sf[:], in_=state[:]), [state.b], [Sf.b])
                set_state(None)
                gla_block(h, hc, hc.b, CTX, "b", False, 0)
                P.op("vector", lambda e: e.tensor_copy(out=Sb[:], in_=state[:]), [state.b], [Sb.b])
                set_state(Sb)
                for i, blk in enumerate(range(NB - 1, -1, -1)):
                    hbt = hb[i % 2]
                    P.dma("sync", lambda e, hbt=hbt, blk=blk: e.dma_start(out=hbt[:].rearrange("p k t -> p (k t)"), in_=HT[blk * 128:(blk + 1) * 128, :]), [HT.b], hbt.b)
                    gla_block(h, hbt, hbt.b, TB, "b", True, blk)
                set_state(Sf)
                for i, blk in enumerate(range(NB)):
                    hbt = hb[i % 2]
                    P.dma("sync", lambda e, hbt=hbt, blk=blk: e.dma_start(out=hbt[:].rearrange("p k t -> p (k t)"), in_=HT[blk * 128:(blk + 1) * 128, :]), [HT.b], hbt.b)
                    gla_block(h, hbt, hbt.b, TB, "f", True, blk)
    _phase3()
    P.barrier()

    if cfg.stop == "A1":
        P.build(final_waits=[HT.b, ADA.b, YT.b] + dumps)
        return nc, P

    NK2 = TB // T1
    def _phase4():
        with (contextlib.nullcontext(top) if cfg.noscope else contextlib.ExitStack()) as st:
            wstg = sb(st, "fwst", [128, KC, 128])
            Wu = sb(st, "Wu", [128, KC, 128], BF16)
            hb = [sb(st, "fhb%d" % i, [128, KC, TB], BF16) for i in range(2)]
            ut = sb(st, "ut", [128, TB], BF16)
            at = [sb(st, "at%d" % i, [128, TB], BF16) for i in range(2)]
            bt_ = [sb(st, "btt%d" % i, [128, TB], BF16) for i in range(2)]
            DA = sb(st, "DA", [T1, 128, 128], BF16)
            DB = sb(st, "DB", [T1, 128, 128], BF16)
            Yall = sb(st, "Yall", [T1, 128, 128], BF16)
            xs1 = [sb(st, "xs1%d" % i, [128, 2, 2, T1]) for i in range(2)]
            tA = [sb(st, "tA%d" % i, [128, 2, T1]) for i in range(2)]
            tB = [sb(st, "tB%d" % i, [128, 2, T1]) for i in range(2)]
            br = [sb(st, "br%d" % i, [128, 2, T1], BF16) for i in range(2)]
            bi = [sb(st, "bi%d" % i, [128, 2, T1], BF16) for i in range(2)]
            yo = [sb(st, "yo%d" % i, [128, TB], BF16) for i in range(2)]
            pu = psb(st, "pu")
            pa = psb(st, "pa")
            pbk = psb(st, "pbk")
            p1 = [psb(st, "p1%d" % i) for i in range(2)]
            p2 = [psb(st, "p2%d" % i) for i in range(2)]
            ptr = T(st.enter_context(nc.psum_tensor("ps_ptr", [128, 1024], BF16)), "ptr")
            twc3 = cst[:, cs("twc")].unsqueeze(1).to_broadcast([128, 2, T1])
            tws3 = cst[:, cs("tws")].unsqueeze(1).to_broadcast([128, 2, T1])
            for gi in range(8):
                P.dma("sync", lambda e, gi=gi: e.dma_start(out=wstg[:], in_=w_in_v[:, :, 3104 + gi * 128:3104 + (gi + 1) * 128]), [], wstg.b)
                P.op("vector", lambda e: e.tensor_copy(out=Wu[:], in_=wstg[:]), [wstg.b], [Wu.b])
                for blk in range(NB):
                    hbt = hb[blk % 2]
                    P.dma("sync", lambda e, hbt=hbt, blk=blk: e.dma_start(out=hbt[:].rearrange("p k t -> p (k t)"), in_=HT[blk * 128:(blk + 1) * 128, :]), [HT.b], hbt.b)
                    for k in range(KC):
                        P.op("tensor", lambda e, k=k, hbt=hbt: e.matmul(pu[:, :], lhsT=Wu[:, k, :], rhs=hbt[:, k, :], start=(k == 0), stop=(k == KC - 1)), [Wu.b, hbt.b], [pu.b])
                    P.op("scalar", lambda e: e.activation(out=ut[:], in_=pu[:, :], func=AF.Copy), [pu.b], [ut.b])
                    a_, b_ = at[blk % 2], bt_[blk % 2]
                    P.op("tensor", lambda e: e.matmul(pa[:, :], lhsT=cbf[:, CB["c128"]], rhs=ut[:], start=True, stop=True), [cbf.b, ut.b], [pa.b])
                    P.op("tensor", lambda e: e.matmul(pbk[:, :], lhsT=cbf[:, CB["s128"]], rhs=ut[:], start=True, stop=True), [cbf.b, ut.b], [pbk.b])
                    P.op("vector", lambda e, a_=a_: e.tensor_copy(out=a_[:], in_=pa[:, :]), [pa.b], [a_.b])
                    P.op("scalar", lambda e, b_=b_: e.activation(out=b_[:], in_=pbk[:, :], func=AF.Copy), [pbk.b], [b_.b])
                    P.dma("gpsimd", lambda e, a_=a_, blk=blk: e.dma_start(out=FA[:, blk * TB:(blk + 1) * TB], in_=a_[:]), [a_.b], FA.b)
                    P.dma("gpsimd", lambda e, b_=b_, blk=blk: e.dma_start(out=FB[:, blk * TB:(blk + 1) * TB], in_=b_[:]), [b_.b], FB.b)
                for m0 in range(0, 128, 16):
                    P.dma("sync", lambda e, m0=m0: e.dma_start(out=DA[:, m0:m0 + 16, :], in_=FA[m0:m0 + 16, :].rearrange("m (a b) -> a m b", b=128)), [FA.b], DA.b)
                    P.dma("sync", lambda e, m0=m0: e.dma_start(out=DB[:, m0:m0 + 16, :], in_=FB[m0:m0 + 16, :].rearrange("m (a b) -> a m b", b=128)), [FB.b], DB.b)
                for mp in range(64):
                    q = mp % 2
                    ps1, ps2 = p1[q], p2[q]
                    for j in range(2):
                        m = mp * 2 + j
                        P.op("tensor", lambda e, m=m, j=j, ps1=ps1: e.matmul(ps1[:, j * 2 * T1:(j + 1) * 2 * T1], lhsT=DA[:, m, :], rhs=cbf[0:T1, CB["f1a"]], start=True, stop=False), [DA.b, cbf.b], [ps1.b])
                        P.op("tensor", lambda e, m=m, j=j, ps1=ps1: e.matmul(ps1[:, j * 2 * T1:(j + 1) * 2 * T1], lhsT=DB[:, m, :], rhs=cbf[0:T1, CB["f1b"]], start=False, stop=True), [DB.b, cbf.b], [ps1.b])
                    x1_, ta, tb, br_, bi_ = xs1[q], tA[q], tB[q], br[q], bi[q]
                    P.op("scalar", lambda e, x1_=x1_, ps1=ps1: e.activation(out=x1_[:].rearrange("p a b c -> p (a b c)"), in_=ps1[:, 0:4 * T1], func=AF.Copy), [ps1.b], [x1_.b])
                    re_, im_ = x1_[:, :, 0, :], x1_[:, :, 1, :]
                    P.op("vector", lambda e, ta=ta, re_=re_: e.tensor_tensor(out=ta[:], in0=re_, in1=twc3, op=ALU.mult), [x1_.b, cst.b], [ta.b])
                    P.op("gpsimd", lambda e, tb=tb, im_=im_: e.tensor_tensor(out=tb[:], in0=im_, in1=tws3, op=ALU.mult), [x1_.b, cst.b], [tb.b])
                    P.op("vector", lambda e, ta=ta, tb=tb, br_=br_: e.tensor_tensor(out=br_[:], in0=ta[:], in1=tb[:], op=ALU.add), [ta.b, tb.b], [br_.b])
                    P.op("gpsimd", lambda e, tb=tb, im_=im_: e.tensor_tensor(out=tb[:], in0=im_, in1=twc3, op=ALU.mult), [x1_.b, cst.b, br_.b], [tb.b])
                    P.op("vector", lambda e, ta=ta, re_=re_: e.tensor_tensor(out=ta[:], in0=re_, in1=tws3, op=ALU.mult), [x1_.b, cst.b, br_.b], [ta.b])
                    P.op("gpsimd", lambda e, ta=ta, tb=tb, bi_=bi_: e.tensor_tensor(out=bi_[:], in0=tb[:], in1=ta[:], op=ALU.subtract), [ta.b, tb.b], [bi_.b])
                    for j in range(2):
                        m = mp * 2 + j
                        P.op("tensor", lambda e, j=j, ps2=ps2, br_=br_: e.matmul(ps2[0:T1, j * 128:(j + 1) * 128], lhsT=br_[:, j, :], rhs=cbf[:, CB["c128"]], start=True, stop=False), [br_.b, cbf.b], [ps2.b])
                        P.op("tensor", lambda e, j=j, ps2=ps2, bi_=bi_: e.matmul(ps2[0:T1, j * 128:(j + 1) * 128], lhsT=bi_[:, j, :], rhs=cbf[:, CB["s128"]], start=False, stop=True), [bi_.b, cbf.b], [ps2.b])
                    P.op("scalar", lambda e, mp=mp, ps2=ps2: e.activation(out=Yall[:, :, mp * 2:mp * 2 + 2].rearrange("p k m -> p m k"), in_=ps2[0:T1, 0:256].rearrange("p (m k) -> p m k", k=128), func=AF.Copy),
                         [ps2.b], [Yall.b])
                for blk in range(NB):
                    yb = yo[blk % 2]
                    for jj in range(NK2):
                        k2 = blk * NK2 + jj
                        P.op("tensor", lambda e, k2=k2, jj=jj: e.transpose(ptr[:, jj * T1:(jj + 1) * T1], Yall[:, k2, :], cbf[0:T1, 384:384 + T1]), [Yall.b, cbf.b], [ptr.b])
                    P.op("vector", lambda e, yb=yb: e.tensor_copy(out=yb[:], in_=ptr[:, 0:TB]), [ptr.b], [yb.b])
                    P.dma("gpsimd", lambda e, yb=yb, blk=blk, gi=gi: e.dma_start(out=YT[blk * 128:(blk + 1) * 128, (8 + gi) * TB:(9 + gi) * TB], in_=yb[:]), [yb.b], YT.b)
    _phase4()
    P.barrier()

    if cfg.stop == "A2":
        P.build(final_waits=[HT.b, ADA.b, YT.b] + dumps)
        return nc, P

    NT = L // 128
    affall = sb(top, "affall", [128, NT, E])
    thr = sb(top, "thr", [128, E])
    def _phase5():
        with (contextlib.nullcontext(top) if cfg.noscope else contextlib.ExitStack()) as st:
            wst = sb(st, "bwst", [128, 1, 2048])
            Wo = sb(st, "Wo", [128, KC, D], BF16)
            wrf = sb(st, "wrf", [128, KC, E])
            wrb = sb(st, "wrb", [128, KC, E], BF16)
            yb = [sb(st, "byb0", [128, KC, TB], BF16)] * 2
            xb = [sb(st, "bxb0", [128, KC, TB])] * 2
            sq = sb(st, "bsq", [128, KC, TB], BF16)
            rs = sb(st, "brs", [128, TB])
            hf = [sb(st, "bhf0", [128, KC, TB], BF16)] * 2
            mx = sb(st, "bmx", [128, 4])
            sm = sb(st, "bsm", [128, 4])
            ee = sb(st, "bee", [128, 4, E])
            po = [psb(st, "po%d" % i) for i in range(2)]
            pq = psb(st, "pq")
            pl = psb(st, "pl")
            for kq in range(KC):
                P.dma("sync", lambda e, kq=kq: e.dma_start(out=wst[:], in_=w_out_v[:, kq:kq + 1, :]), [], wst.b)
                P.op("vector", lambda e, kq=kq: e.tensor_copy(out=Wo[:, kq:kq + 1, :], in_=wst[:]), [wst.b], [Wo.b])
            P.dma("sync", lambda e: e.dma_start(out=wrf[:], in_=wr_v), [], wrf.b)
            P.op("vector", lambda e: e.tensor_copy(out=wrb[:], in_=wrf[:]), [wrf.b], [wrb.b])
            for blk in range(NB):
                y_, x_, h_ = yb[0], xb[0], hf[0]
                x1 = x_
                P.dma("sync", lambda e, y_=y_, blk=blk: e.dma_start(out=y_[:].rearrange("p k t -> p (k t)"), in_=YT[blk * 128:(blk + 1) * 128, :]), [YT.b], y_.b)
                P.dma("sync", lambda e, x_=x_, blk=blk: e.dma_start(out=x_[:], in_=xT_v[:, :, blk * TB:(blk + 1) * TB]), [], x_.b)
                for nb in range(KC):
                    pp = po[nb % 2]
                    for k in range(KC):
                        P.op("tensor", lambda e, pp=pp, k=k, nb=nb, y_=y_: e.matmul(pp[:, :], lhsT=Wo[:, k, nb * 128:(nb + 1) * 128], rhs=y_[:, k, :], start=(k == 0), stop=(k == KC - 1)), [Wo.b, y_.b], [pp.b])
                    P.op("vector", lambda e, pp=pp, nb=nb, x_=x_: e.scalar_tensor_tensor(out=x1[:, nb, :], in0=pp[:, :], scalar=mods[:, 32 + nb:33 + nb], in1=x_[:, nb, :], op0=ALU.mult, op1=ALU.add),
                         [pp.b, mods.b, x_.b], [x1.b])
                P.dma("gpsimd", lambda e, blk=blk: e.dma_start(out=X1[blk * 128:(blk + 1) * 128, :], in_=x1[:].rearrange("p k t -> p (k t)")), [x1.b], X1.b)
                P.op("gpsimd", lambda e: e.tensor_tensor(out=sq[:], in0=x1[:], in1=x1[:], op=ALU.mult), [x1.b], [sq.b])
                rstd_from_sq(pq, sq, KC, TB, rs, 1.0 / D)
                P.op("vector", lambda e: e.tensor_tensor(out=x1[:], in0=x1[:], in1=rs[:].unsqueeze(1).to_broadcast([128, KC, TB]), op=ALU.mult), [x1.b, rs.b], [x1.b])
                P.op("gpsimd", lambda e: e.tensor_tensor(out=x1[:], in0=x1[:], in1=G2[:, 0:16].unsqueeze(2).to_broadcast([128, KC, TB]), op=ALU.mult), [x1.b, G2.b], [x1.b])
                P.op("vector", lambda e, h_=h_: e.tensor_tensor(out=h_[:], in0=x1[:], in1=mods[:, 48:64].unsqueeze(2).to_broadcast([128, KC, TB]), op=ALU.add), [x1.b, mods.b], [h_.b])
                P.dma("gpsimd", lambda e, h_=h_, blk=blk: e.dma_start(out=HF[blk * 128:(blk + 1) * 128, :], in_=h_[:].rearrange("p k t -> p (k t)")), [h_.b], HF.b)
                for s in range(4):
                    for k in range(KC):
                        P.op("tensor", lambda e, s=s, k=k, h_=h_: e.matmul(pl[:, s * E:(s + 1) * E], lhsT=h_[:, k, s * 128:(s + 1) * 128], rhs=wrb[:, k, :], start=(k == 0), stop=(k == KC - 1)), [h_.b, wrb.b], [pl.b])
                pl3 = pl[:, 0:4 * E].rearrange("p (s e) -> p s e", e=E)
                P.op("vector", lambda e: e.tensor_reduce(out=mx[:], in_=pl3, axis=AX.X, op=ALU.max), [pl.b], [mx.b])
                P.op("vector", lambda e: e.tensor_tensor(out=ee[:], in0=pl3, in1=mx[:].unsqueeze(2).to_broadcast([128, 4, E]), op=ALU.subtract), [pl.b, mx.b], [ee.b])
                P.op("scalar", lambda e: e.activation(out=ee[:], in_=ee[:], func=AF.Exp), [ee.b], [ee.b])
                P.op("vector", lambda e: e.tensor_reduce(out=sm[:], in_=ee[:], axis=AX.X, op=ALU.add), [ee.b], [sm.b])
                P.op("vector", lambda e: e.reciprocal(out=sm[:], in_=sm[:]), [sm.b], [sm.b])
                P.op("vector", lambda e, blk=blk: e.tensor_tensor(out=affall[:, blk * 4:(blk + 1) * 4, :], in0=ee[:], in1=sm[:].unsqueeze(2).to_broadcast([128, 4, E]), op=ALU.mult), [ee.b, sm.b], [affall.b])
                P.dma("gpsimd", lambda e, blk=blk: e.dma_start(out=AFS[blk * 128:(blk + 1) * 128, :], in_=affall[:, blk * 4:(blk + 1) * 4, :].rearrange("p s e -> p (s e)")), [affall.b], AFS.b)
    _phase5()
    P.barrier()

    if cfg.stop == "B":
        P.build(final_waits=[HT.b, ADA.b, YT.b, X1.b, AFS.b] + dumps)
        return nc, P

    def _phase6():
        with (contextlib.nullcontext(top) if cfg.noscope else contextlib.ExitStack()) as st:
            mid = sb(st, "mid", [128, E])
            cmpt = sb(st, "cmpt", [128, E, NT])
            pc = sb(st, "pc", [128, E])
            ge = sb(st, "ge", [128, E])
            pc_ps = psb(st, "pcps")
            aff_v = affall[:].rearrange("p t e -> p e t")
            P.op("vector", lambda e: e.memset(thr[:], 0.0), [], [thr.b])
            for it in range(30):
                s_i = 2.0 ** -(it + 1)
                P.op("vector", lambda e, s_i=s_i: e.tensor_scalar(out=mid[:], in0=thr[:], scalar1=s_i, scalar2=None, op0=ALU.add), [thr.b], [mid.b])
                P.op("vector", lambda e: e.tensor_tensor(out=cmpt[:], in0=aff_v, in1=mid[:].unsqueeze(2).to_broadcast([128, E, NT]), op=ALU.is_gt), [affall.b, mid.b], [cmpt.b])
                P.op("vector", lambda e: e.tensor_reduce(out=pc[:], in_=cmpt[:], axis=AX.X, op=ALU.add), [cmpt.b], [pc.b])
                P.op("tensor", lambda e: e.matmul(pc_ps[:, 0:E], lhsT=cst[:, cs("ones")], rhs=pc[:], start=True, stop=True), [cst.b, pc.b], [pc_ps.b])
                P.op("vector", lambda e, s_i=s_i: e.tensor_scalar(out=ge[:], in0=pc_ps[:, 0:E], scalar1=float(cfg.CAP) - 0.5, scalar2=s_i, op0=ALU.is_gt, op1=ALU.mult), [pc_ps.b], [ge.b])
                P.op("vector", lambda e: e.tensor_tensor(out=thr[:], in0=thr[:], in1=ge[:], op=ALU.add), [thr.b, ge.b], [thr.b])
    _phase6()
    P.barrier()

    if cfg.stop == "C":
        P.build(final_waits=[HT.b, ADA.b, YT.b, X1.b, AFS.b] + dumps)
        return nc, P

    S = TG // 128
    def _phase7():
        with (contextlib.nullcontext(top) if cfg.noscope else contextlib.ExitStack()) as st:
            hfT = sb(st, "hfT", [128, KC, TG], BF16)
            acc = sb(st, "acc", [128, KC, TG])
            afo = sb(st, "afo", [128, S, E])
            msk = sb(st, "msk", [128, S, E])
            wgt = sb(st, "wgt", [128, S, E])
            wgT = sb(st, "wgT", [E, TG])
            wb = [sb(st, "wb%d" % i, [128, TG], BF16) for i in range(2)]
            gst = [sb(st, "gst%d" % i, [128, KC, 128]) for i in range(2)]
            ust = [sb(st, "ust%d" % i, [128, KC, 128]) for i in range(2)]
            gbf = [sb(st, "gbf%d" % i, [128, KC, 128], BF16) for i in range(2)]
            ubf = [sb(st, "ubf%d" % i, [128, KC, 128], BF16) for i in range(2)]
            dst_ = [sb(st, "dst%d" % i, [128, FC, 128]) for i in range(2)]
            dbf = [sb(st, "dbf%d" % i, [128, FC, 128], BF16) for i in range(2)]
            sgm = [sb(st, "sgm%d" % i, [128, TG], BF16) for i in range(2)]
            tmu = [sb(st, "tmu%d" % i, [128, TG], BF16) for i in range(2)]
            hid = sb(st, "hid", [128, FC, TG], BF16)
            sq = sb(st, "dsq", [128, KC, TG], BF16)
            rs = sb(st, "drs", [128, TG])
            pg = [psb(st, "pg%d" % i) for i in range(2)]
            pu_ = [psb(st, "pu%d" % i) for i in range(2)]
            py = [psb(st, "py%d" % i) for i in range(2)]
            pw = psb(st, "pw")
            pm = psb(st, "pm")
            X1f = X1.t.ap()
            HFf = HF.t.ap()
            AFf = AFS.t.ap()
            cnt = [0, 0]
            for g in range(NG):
                for jb in range(TG // TB):
                    col = g * (TG // TB) + jb
                    P.dma("gpsimd", lambda e, col=col, jb=jb: e.indirect_dma_start(out=hfT[:].rearrange("p k t -> p (k t)"), out_offset=None, in_=HFf,
                                                                                     in_offset=bass.IndirectOffsetOnAxis(ap=idx[:, col:col + 1], axis=0)), [HF.b, idx.b], hfT.b)
                    P.dma("gpsimd", lambda e, col=col, jb=jb: e.indirect_dma_start(out=acc[:].rearrange("p k t -> p (k t)"), out_offset=None, in_=X1f,
                                                                                     in_offset=bass.IndirectOffsetOnAxis(ap=idx[:, col:col + 1], axis=0)), [X1.b, idx.b], acc.b)
                    P.dma("gpsimd", lambda e, col=col, jb=jb: e.indirect_dma_start(out=afo[:, jb * 4:(jb + 1) * 4, :].rearrange("p s e -> p (s e)"), out_offset=None, in_=AFf,
                                                                                     in_offset=bass.IndirectOffsetOnAxis(ap=idx[:, col:col + 1], axis=0)), [AFS.b, idx.b], afo.b)
                P.op("vector", lambda e: e.tensor_tensor(out=msk[:], in0=afo[:], in1=thr[:].unsqueeze(1).to_broadcast([128, S, E]), op=ALU.is_gt), [afo.b, thr.b], [msk.b])
                P.op("vector", lambda e: e.tensor_tensor(out=wgt[:], in0=afo[:], in1=msk[:], op=ALU.mult), [afo.b, msk.b], [wgt.b])
                for s in range(S):
                    P.op("tensor", lambda e, s=s: e.matmul(pm[0:E, s * 128:(s + 1) * 128], lhsT=wgt[:, s, :], rhs=cst[:, cs("ident")], start=True, stop=True), [wgt.b, cst.b], [pm.b])
                P.op("vector", lambda e: e.tensor_copy(out=wgT[:], in_=pm[0:E, 0:TG]), [pm.b], [wgT.b])
                for ex_ in range(E):
                    wbe = wb[ex_ % 2]
                    so = lay["sel"][0] + ex_ * 128
                    P.op("tensor", lambda e, so=so: e.matmul(pw[:, 0:TG], lhsT=cst[0:E, so:so + 128], rhs=wgT[:], start=True, stop=True), [cst.b, wgT.b], [pw.b])
                    P.op("scalar", lambda e, wbe=wbe: e.activation(out=wbe[:], in_=pw[:, 0:TG], func=AF.Copy), [pw.b], [wbe.b])
                    for fb in range(FC):
                        i = cnt[0] % 2
                        cnt[0] += 1
                        gs, us, gb, ub = gst[i], ust[i], gbf[i], ubf[i]
                        P.dma("sync", lambda e, gs=gs, ex_=ex_, fb=fb: e.dma_start(out=gs[:].rearrange("p k f -> p (k f)"), in_=w_eg[(ex_ * FC + fb) * 128:(ex_ * FC + fb + 1) * 128, :]), [], gs.b)
                        P.dma("sync", lambda e, us=us, ex_=ex_, fb=fb: e.dma_start(out=us[:].rearrange("p k f -> p (k f)"), in_=w_eu[(ex_ * FC + fb) * 128:(ex_ * FC + fb + 1) * 128, :]), [], us.b)
                        P.op("gpsimd", lambda e, gs=gs, gb=gb: e.tensor_copy(out=gb[:], in_=gs[:]), [gs.b], [gb.b])
                        P.op("vector", lambda e, us=us, ub=ub: e.tensor_copy(out=ub[:], in_=us[:]), [us.b], [ub.b])
                        pgi, pui, sgi, tmi = pg[i], pu_[i], sgm[i], tmu[i]
                        for k in range(KC):
                            P.op("tensor", lambda e, k=k, gb=gb, pgi=pgi: e.matmul(pgi[:, 0:TG], lhsT=gb[:, k, :], rhs=hfT[:, k, :], start=(k == 0), stop=(k == KC - 1)), [gb.b, hfT.b], [pgi.b])
                        for k in range(KC):
                            P.op("tensor", lambda e, k=k, ub=ub, pui=pui: e.matmul(pui[:, 0:TG], lhsT=ub[:, k, :], rhs=hfT[:, k, :], start=(k == 0), stop=(k == KC - 1)), [ub.b, hfT.b], [pui.b])
                        P.op("scalar", lambda e, sgi=sgi, pgi=pgi: e.activation(out=sgi[:], in_=pgi[:, 0:TG], func=AF.Silu), [pgi.b], [sgi.b])
                        P.op("vector", lambda e, sgi=sgi, pui=pui, tmi=tmi: e.tensor_tensor(out=tmi[:], in0=pui[:, 0:TG], in1=sgi[:], op=ALU.mult), [pui.b, sgi.b], [tmi.b])
                        P.op("gpsimd", lambda e, tmi=tmi, fb=fb, wbe=wbe: e.tensor_tensor(out=hid[:, fb, :], in0=tmi[:], in1=wbe[:], op=ALU.mult), [tmi.b, wbe.b], [hid.b])
                    for db in range(KC):
                        i = cnt[1] % 2
                        cnt[1] += 1
                        ds_, dbb, pyi = dst_[i], dbf[i], py[i]
                        P.dma("sync", lambda e, ds_=ds_, ex_=ex_, db=db: e.dma_start(out=ds_[:].rearrange("p c d -> p (c d)"), in_=w_ed[(ex_ * KC + db) * 128:(ex_ * KC + db + 1) * 128, :]), [], ds_.b)
                        P.op("gpsimd", lambda e, ds_=ds_, dbb=dbb: e.tensor_copy(out=dbb[:], in_=ds_[:]), [ds_.b], [dbb.b])
                        for fc in range(FC):
                            P.op("tensor", lambda e, fc=fc, dbb=dbb, pyi=pyi: e.matmul(pyi[:, 0:TG], lhsT=dbb[:, fc, :], rhs=hid[:, fc, :], start=(fc == 0), stop=(fc == FC - 1)), [dbb.b, hid.b], [pyi.b])
                        P.op("vector", lambda e, db=db, pyi=pyi: e.scalar_tensor_tensor(out=acc[:, db, :], in0=pyi[:, 0:TG], scalar=mods[:, 80 + db:81 + db], in1=acc[:, db, :], op0=ALU.mult, op1=ALU.add),
                             [pyi.b, mods.b, acc.b], [acc.b])
                P.op("gpsimd", lambda e: e.tensor_tensor(out=sq[:], in0=acc[:], in1=acc[:], op=ALU.mult), [acc.b], [sq.b])
                rstd_from_sq(pw, sq, KC, TG, rs, 1.0 / D)
                P.op("vector", lambda e: e.tensor_tensor(out=acc[:], in0=acc[:], in1=rs[:].unsqueeze(1).to_broadcast([128, KC, TG]), op=ALU.mult), [acc.b, rs.b], [acc.b])
                P.op("gpsimd", lambda e: e.tensor_tensor(out=acc[:], in0=acc[:], in1=ncl[:, 32:48].unsqueeze(2).to_broadcast([128, KC, TG]), op=ALU.mult), [acc.b, ncl.b], [acc.b])
                P.dma("sync", lambda e, g=g: e.dma_start(out=outT.t.ap().rearrange("(k p) t -> p k t", p=128)[:, :, g * TG:(g + 1) * TG], in_=acc[:]), [acc.b], outT.b)
    _phase7()

    finals = [outT.b]
    if cfg.dev:
        finals += [YT.b, X1.b, AFS.b, ADA.b, HT.b] + dumps
    P.build(final_waits=finals)
    top.close()
    return nc, P


def host_inputs(cfg, core, x, c, ctx, c_ctx, w_ada, b_ada, norm1_g, w_in, w_a2_f, b_a2_f, w_a2_b, b_a2_b,
                gla_norm_g, w_out, norm2_g, w_router, w_e_gate, w_e_up, w_e_down, final_norm_g, shared):
    b, r = core // 4, core % 4
    f = lambda a: np.ascontiguousarray(np.asarray(a, dtype=np.float32))
    col = lambda v: f(np.asarray(v).reshape(-1, 128).T)
    m = {}
    m["xT"] = shared["xT"][b]
    m["ctxT"] = shared["ctxT"][b]
    m["cond"] = f(np.concatenate([col(c[b]), col(c_ctx)], axis=1))
    m["w_ada"] = shared["w_ada"]
    m["b_ada"] = shared["b_ada"]
    m["ncols"] = shared["ncols"]
    m["glag"] = shared["glag"]
    m["w_in"] = shared["w_in"]
    m["wa2"] = shared["wa2"]
    m["ba2"] = shared["ba2"]
    m["w_out"] = shared["w_out"]
    m["w_router"] = shared["w_router"]
    m["w_eg"] = shared["w_eg"]
    m["w_eu"] = shared["w_eu"]
    m["w_ed"] = shared["w_ed"]
    m["cst"] = shared["cst"]
    ob0 = r * cfg.NOB
    m["idx"] = np.stack([(ob0 + j) * 128 + np.arange(128) for j in range(cfg.NOB)], axis=1).astype(np.int32)
    return m


def host_shared(cfg, x, c, ctx, c_ctx, w_ada, b_ada, norm1_g, w_in, w_a2_f, b_a2_f, w_a2_b, b_a2_b,
                gla_norm_g, w_out, norm2_g, w_router, w_e_gate, w_e_up, w_e_down, final_norm_g, batches=(0, 1)):
    f = lambda a: np.ascontiguousarray(np.asarray(a, dtype=np.float32))
    col = lambda v: f(np.asarray(v).reshape(-1, 128).T)
    sh = {}
    sh["xT"] = {b: f(np.asarray(x[b]).T) for b in batches}
    sh["ctxT"] = {b: f(np.asarray(ctx[b]).T) for b in batches}
    sh["w_ada"] = f(w_ada[0])
    sh["b_ada"] = f(b_ada[0]).reshape(1, -1)
    sh["ncols"] = f(np.concatenate([col(norm1_g[0]), col(norm2_g[0]), col(final_norm_g)], axis=1))
    sh["glag"] = col(gla_norm_g[0])
    sh["w_in"] = f(w_in[0])
    sh["wa2"] = f(np.concatenate([np.asarray(w_a2_f[0]), np.asarray(w_a2_b[0])], axis=1))
    sh["ba2"] = f(np.concatenate([col(b_a2_f[0]), col(b_a2_b[0])], axis=1))
    sh["w_out"] = f(w_out[0])
    sh["w_router"] = f(w_router[0])
    E_, FC_ = cfg.E, cfg.FC
    slab = lambda w: np.ascontiguousarray(np.asarray(w, dtype=np.float32).reshape(E_, KC, 128, FC_, 128).transpose(0, 3, 2, 1, 4)).reshape(E_ * FC_ * 128, KC * 128)
    sh["w_eg"] = slab(w_e_gate[0])
    sh["w_eu"] = slab(w_e_up[0])
    sh["w_ed"] = np.ascontiguousarray(np.asarray(w_e_down[0], dtype=np.float32).reshape(E_, FC_, 128, KC, 128).transpose(0, 3, 2, 1, 4)).reshape(E_ * KC * 128, FC_ * 128)
    sh["cst"] = make_consts(cfg)
    return sh


def kernel(**inputs):
    cfg = Cfg()
    nc, _ = build(cfg)
    sh = host_shared(cfg, **inputs)
    in_maps = [host_inputs(cfg, core, shared=sh, **inputs) for core in range(8)]
    res = run_bass_kernel_spmd(nc, in_maps, core_ids=list(range(8)))
    out = np.empty((2, cfg.L, D), np.float32)
    for core in range(8):
        b, r = core // 4, core % 4
        out[b, r * cfg.OWN:(r + 1) * cfg.OWN, :] = np.asarray(res.results[core]["outT"]).T
    return out
```
